# Optimizing a Trainium2 kernel written in Bass

```python
import math
import jax, jax.numpy as jnp
from jax import lax
import numpy as np


D_MODEL = 1024
BATCH = 4
SEQ = 8192
DEPTH = 1

N_GLA_HEADS = 4
GLA_DK = 64
GLA_DV = 128
GLA_GATE_RANK = 16
GLA_GATE_NORM = 16.0
GLA_CHUNK = 64
N_DIFF_HEADS = 4
DIFF_DQK = 64
DIFF_DV = 128
Q_BLOCK = 128
NUM_BUCKETS = 32
MAX_DISTANCE = 128
N_EXPERTS = 32
TOP_K = 4
D_FF = D_MODEL
SWIGLU_LIMIT = 7.0
SWIGLU_ALPHA = 1.702
MOE_BLOCK = 256
EPS = 1e-6

GLA_QK_W = N_GLA_HEADS * GLA_DK
GLA_V_W = N_GLA_HEADS * GLA_DV
DIFF_QK_W = N_DIFF_HEADS * 2 * DIFF_DQK
DIFF_V_W = N_DIFF_HEADS * DIFF_DV
MIX_W = GLA_V_W + DIFF_V_W
IN_SPLITS = (GLA_QK_W, GLA_QK_W, GLA_V_W, GLA_V_W, GLA_GATE_RANK, DIFF_QK_W, DIFF_QK_W, DIFF_V_W)
IN_OFFSETS = tuple(int(v) for v in np.cumsum(IN_SPLITS)[:-1])
IN_W = int(sum(IN_SPLITS))

kernel_name = 'hybrid_gla_diffattn_moe_block'


def _rmsnorm(x, g):
    xf = x.astype(jnp.float32)
    y = xf * lax.rsqrt(jnp.mean(xf * xf, axis=-1, keepdims=True) + EPS)
    return (y * g.astype(jnp.float32)).astype(x.dtype)


def _modulate(h, shift, scale):
    return h * (1 + scale[:, None, :]) + shift[:, None, :]


def _t5_bucket(n):
    max_exact = NUM_BUCKETS // 2
    nf = jnp.maximum(n, 1).astype(jnp.float32)
    large = max_exact + (jnp.log(nf / max_exact) / math.log(MAX_DISTANCE / max_exact)
                         * (NUM_BUCKETS - max_exact)).astype(jnp.int32)
    large = jnp.minimum(large, NUM_BUCKETS - 1)
    return jnp.where(n < max_exact, n, large)


def _gla_chunked(q, k, v, g):
    B, H, S, dk = q.shape
    dv = v.shape[-1]
    n = S // GLA_CHUNK
    f32 = jnp.float32
    q, k, v, g = (t.astype(f32).reshape(B, H, n, GLA_CHUNK, t.shape[-1]) for t in (q, k, v, g))
    gc = jnp.cumsum(g, axis=3)
    g_last = gc[:, :, :, -1:, :]
    q_e = q * jnp.exp(gc)
    k_e = k * jnp.exp(-gc)
    causal = jnp.tril(jnp.ones((GLA_CHUNK, GLA_CHUNK), dtype=bool))
    a = jnp.where(causal, jnp.einsum('bhncd,bhnjd->bhncj', q_e, k_e), 0.0)
    o_intra = jnp.einsum('bhncj,bhnje->bhnce', a, v)
    k_s = k * jnp.exp(g_last - gc)
    u = jnp.einsum('bhncd,bhnce->bhnde', k_s, v)
    decay = jnp.exp(g_last[:, :, :, 0, :])

    def step(state, xs):
        u_n, d_n = xs
        return state * d_n[..., None] + u_n, state

    _, s_prev = lax.scan(step, jnp.zeros((B, H, dk, dv), f32),
                         (jnp.moveaxis(u, 2, 0), jnp.moveaxis(decay, 2, 0)))
    s_prev = jnp.moveaxis(s_prev, 0, 2)
    o_inter = jnp.einsum('bhncd,bhnde->bhnce', q_e, s_prev)
    return (o_intra + o_inter).reshape(B, H, S, dv)


def _diff_attention(q, k, v, lam, bias_by_dist):
    B, H, _, S, dqk = q.shape
    nqb = S // Q_BLOCK
    qb = jnp.moveaxis(q.reshape(B, H, 2, nqb, Q_BLOCK, dqk), 3, 0)
    k_pos = jnp.arange(S, dtype=jnp.int32)

    def block(args):
        q_blk, i = args
        q_pos = i * Q_BLOCK + jnp.arange(Q_BLOCK, dtype=jnp.int32)
        dist = q_pos[:, None] - k_pos[None, :]
        bias = bias_by_dist[jnp.clip(dist, 0, S - 1)].astype(jnp.float32)
        bias = bias.reshape(Q_BLOCK, S, H, 2).transpose(2, 3, 0, 1)
        s = jnp.einsum('bhmqd,bhmkd->bhmqk', q_blk, k).astype(jnp.float32) + bias
        s = jnp.where(dist >= 0, s, -jnp.inf)
        p = jax.nn.softmax(s, axis=-1)
        a = p[:, :, 0] - lam * p[:, :, 1]
        return jnp.einsum('bhqk,bhke->bhqe', a.astype(v.dtype), v)

    out = lax.map(block, (qb, jnp.arange(nqb, dtype=jnp.int32)))
    return jnp.moveaxis(out, 0, 2).reshape(B, H, S, v.shape[-1])


def _token_mixer(h, w_in, w_gk_up, b_gk_up, g_gla_out, g_qnorm, g_knorm, lq1, lk1, lq2, lk2,
                 g_subln, w_out, bias_by_dist, lambda_init):
    B, S, _ = h.shape
    f32 = jnp.float32
    proj = h @ w_in
    q_g, k_g, v_g, r_g, gk_lo, q_d, k_d, v_d = jnp.split(proj, IN_OFFSETS, axis=-1)

    def heads(t, nh):
        return t.reshape(B, S, nh, -1).transpose(0, 2, 1, 3)

    gk = jax.nn.log_sigmoid((gk_lo @ w_gk_up + b_gk_up).astype(f32)) / GLA_GATE_NORM
    o_g = _gla_chunked(heads(q_g, N_GLA_HEADS) * (GLA_DK ** -0.5), heads(k_g, N_GLA_HEADS),
                       heads(v_g, N_GLA_HEADS), heads(gk, N_GLA_HEADS))
    o_g = _rmsnorm(o_g, g_gla_out).transpose(0, 2, 1, 3).reshape(B, S, GLA_V_W).astype(h.dtype)
    o_g = o_g * jax.nn.silu(r_g)

    q_d = _rmsnorm(q_d.reshape(B, S, N_DIFF_HEADS, 2, DIFF_DQK), g_qnorm).transpose(0, 2, 3, 1, 4)
    q_d = q_d * (DIFF_DQK ** -0.5)
    k_d = _rmsnorm(k_d.reshape(B, S, N_DIFF_HEADS, 2, DIFF_DQK), g_knorm).transpose(0, 2, 3, 1, 4)
    lam = (jnp.exp(jnp.sum(lq1.astype(f32) * lk1.astype(f32)))
           - jnp.exp(jnp.sum(lq2.astype(f32) * lk2.astype(f32))) + lambda_init)
    o_d = _diff_attention(q_d, k_d, heads(v_d, N_DIFF_HEADS), lam, bias_by_dist)
    o_d = (_rmsnorm(o_d, g_subln) * (1.0 - lambda_init)).transpose(0, 2, 1, 3).reshape(B, S, DIFF_V_W)

    return jnp.concatenate([o_g, o_d.astype(h.dtype)], axis=-1) @ w_out


def _moe(h, w_router, b_router, w_gate_up, b_gate_up, w_down, b_down):
    B, S, D = h.shape
    T = B * S
    A = T * TOP_K
    hf = h.reshape(T, D)
    logits = (hf @ w_router + b_router).astype(jnp.float32)
    top_v, top_e = lax.top_k(logits, TOP_K)
    top_w = jax.nn.softmax(top_v, axis=-1)
    e_flat = top_e.reshape(A).astype(jnp.int32)
    w_flat = top_w.reshape(A)
    tok_flat = jnp.arange(A, dtype=jnp.int32) // TOP_K
    order = jnp.argsort(e_flat, stable=True)
    e_sorted = e_flat[order]
    counts = jnp.zeros((N_EXPERTS,), jnp.int32).at[e_flat].add(1)
    starts = jnp.cumsum(counts) - counts
    padded = ((counts + MOE_BLOCK - 1) // MOE_BLOCK) * MOE_BLOCK
    p_ends = jnp.cumsum(padded)
    p_starts = p_ends - padded
    dest = p_starts[e_sorted] + (jnp.arange(A, dtype=jnp.int32) - starts[e_sorted])
    n_blocks = -(-A // MOE_BLOCK) + N_EXPERTS
    P = n_blocks * MOE_BLOCK
    buf_tok = jnp.zeros((P,), jnp.int32).at[dest].set(tok_flat[order])
    buf_w = jnp.zeros((P,), jnp.float32).at[dest].set(w_flat[order])
    block_e = jnp.minimum(jnp.searchsorted(p_ends, jnp.arange(n_blocks, dtype=jnp.int32) * MOE_BLOCK,
                                           side='right'), N_EXPERTS - 1).astype(jnp.int32)

    def expert_block(args):
        tok_b, w_b, e = args
        xb = hf[tok_b]
        gu = xb @ w_gate_up[e] + b_gate_up[e]
        gate = jnp.minimum(gu[:, :D_FF], SWIGLU_LIMIT)
        up = jnp.clip(gu[:, D_FF:], -SWIGLU_LIMIT, SWIGLU_LIMIT)
        y = (up + 1) * (gate * jax.nn.sigmoid(SWIGLU_ALPHA * gate))
        out = y @ w_down[e] + b_down[e]
        return out * w_b[:, None].astype(out.dtype)

    ys = lax.map(expert_block, (buf_tok.reshape(n_blocks, MOE_BLOCK),
                                buf_w.reshape(n_blocks, MOE_BLOCK), block_e))
    out = jnp.zeros((T, D), h.dtype).at[buf_tok].add(ys.reshape(P, D).astype(h.dtype))
    return out.reshape(B, S, D)


def setup_inputs(seed: int = 0) -> dict:
    key = jax.random.key(seed)
    ks = jax.random.split(key, 26)
    f32 = jnp.float32
    nrm = lambda k, shape, s: jax.random.normal(k, shape, f32) * s
    D, L, E, F = D_MODEL, DEPTH, N_EXPERTS, D_FF
    return {
        'x': nrm(ks[0], (BATCH, SEQ, D), 1.0),
        'c': nrm(ks[1], (BATCH, D), 1.0),
        'rel_bias_table': nrm(ks[2], (NUM_BUCKETS, 2 * N_DIFF_HEADS), 0.5),
        'w_ada': nrm(ks[3], (L, D, 6 * D), 0.5 * D ** -0.5),
        'b_ada': nrm(ks[4], (L, 6 * D), 0.02),
        'g_norm1': 1.0 + nrm(ks[5], (L, D), 0.02),
        'w_in': nrm(ks[6], (L, D, IN_W), D ** -0.5),
        'w_gk_up': nrm(ks[7], (L, GLA_GATE_RANK, GLA_QK_W), GLA_GATE_RANK ** -0.5),
        'b_gk_up': nrm(ks[8], (L, GLA_QK_W), 0.1),
        'g_gla_out': 1.0 + nrm(ks[9], (L, GLA_DV), 0.02),
        'g_qnorm': 1.0 + nrm(ks[10], (L, DIFF_DQK), 0.02),
        'g_knorm': 1.0 + nrm(ks[11], (L, DIFF_DQK), 0.02),
        'lambda_q1': nrm(ks[12], (L, DIFF_DQK), 0.1),
        'lambda_k1': nrm(ks[13], (L, DIFF_DQK), 0.1),
        'lambda_q2': nrm(ks[14], (L, DIFF_DQK), 0.1),
        'lambda_k2': nrm(ks[15], (L, DIFF_DQK), 0.1),
        'g_subln': 1.0 + nrm(ks[16], (L, DIFF_DV), 0.02),
        'w_out': nrm(ks[17], (L, MIX_W, D), MIX_W ** -0.5),
        'g_norm2': 1.0 + nrm(ks[18], (L, D), 0.02),
        'w_router': nrm(ks[19], (L, D, E), D ** -0.5),
        'b_router': nrm(ks[20], (L, E), 0.01),
        'w_gate_up': nrm(ks[21], (L, E, D, 2 * F), D ** -0.5),
        'b_gate_up': nrm(ks[22], (L, E, 2 * F), 0.02),
        'w_down': nrm(ks[23], (L, E, F, D), F ** -0.5),
        'b_down': nrm(ks[24], (L, E, D), 0.02),
    }


def reference(x, c, rel_bias_table, w_ada, b_ada, g_norm1, w_in, w_gk_up, b_gk_up, g_gla_out,
              g_qnorm, g_knorm, lambda_q1, lambda_k1, lambda_q2, lambda_k2, g_subln, w_out,
              g_norm2, w_router, b_router, w_gate_up, b_gate_up, w_down, b_down):
    S = x.shape[1]
    bias_by_dist = rel_bias_table[_t5_bucket(jnp.arange(S, dtype=jnp.int32))]
    cond = jax.nn.silu(c)
    for l in range(DEPTH):
        lambda_init = 0.8 - 0.6 * math.exp(-0.3 * l)
        mod = cond @ w_ada[l] + b_ada[l]
        sh1, sc1, gt1, sh2, sc2, gt2 = jnp.split(mod, 6, axis=-1)
        h = _modulate(_rmsnorm(x, g_norm1[l]), sh1, sc1)
        x = x + gt1[:, None, :] * _token_mixer(
            h, w_in[l], w_gk_up[l], b_gk_up[l], g_gla_out[l], g_qnorm[l], g_knorm[l],
            lambda_q1[l], lambda_k1[l], lambda_q2[l], lambda_k2[l], g_subln[l], w_out[l],
            bias_by_dist, lambda_init)
        h = _modulate(_rmsnorm(x, g_norm2[l]), sh2, sc2)
        x = x + gt2[:, None, :] * _moe(h, w_router[l], b_router[l], w_gate_up[l], b_gate_up[l],
                                       w_down[l], b_down[l])
    return x
```

```python
import math
from contextlib import ExitStack
import numpy as np
import concourse.bass as bass
import concourse.mybir as mybir
from concourse.bass_utils import run_bass_kernel_spmd
from concourse.alu_op_type import AluOpType as ALU

F32 = mybir.dt.float32
BF16 = mybir.dt.bfloat16
AF = mybir.ActivationFunctionType
AX = mybir.AxisListType

D = 1024
S = 8192
HALF = 4096
NT_OWN = 32
NT_K = 64
E = 32
IN_W = 3088
OFF_QG, OFF_KG, OFF_VG, OFF_RG, OFF_GK, OFF_QD, OFF_KD, OFF_VD = 0, 256, 512, 1024, 1536, 1552, 2064, 2576
EPS = 1e-6
NEG = -30000.0
LAMBDA_INIT = 0.8 - 0.6 * math.exp(-0.3 * 0)
NQ = 12
MOE_B = 256
NBLK = (HALF * 4) // MOE_B + E
PSLOTS = NBLK * MOE_B
I32 = mybir.dt.int32

import os
DEBUG = bool(os.environ.get('KDEBUG'))


class H:
    __slots__ = ("w", "r", "psum")

    def __init__(self, psum=False):
        self.w = None
        self.r = {}
        self.psum = psum


class KB:
    def __init__(self, nc):
        self.nc = nc
        self.E = {"pe": nc.tensor, "act": nc.scalar, "dve": nc.vector, "pool": nc.gpsimd, "sp": nc.sync}
        self.sems = []
        self.esem = {}
        self.ecnt = {}
        for n in self.E:
            self.esem[n] = self._newsem("e_" + n)
            self.ecnt[n] = 0
        self.waited = {n: {} for n in self.E}
        self.dq = {}
        for q in ("sp", "pool"):
            self.dq[q] = {"sems": [self._newsem(f"d_{q}{i}") for i in range(NQ)], "cnt": [0] * NQ, "rr": 0}

    def _newsem(self, name):
        s = self.nc.alloc_semaphore(name)
        self.sems.append(s)
        return len(self.sems) - 1

    def _wait(self, en, evs, raw=()):
        need = {}
        own = self.esem[en]
        for ev in list(raw) + list(evs):
            if ev is None:
                continue
            k, v = ev
            if k == own and en == "pe":
                continue
            if need.get(k, 0) < v:
                need[k] = v
        wd = self.waited[en]
        for k, v in need.items():
            if wd.get(k, 0) >= v:
                continue
            self.E[en].wait_ge(self.sems[k], v)
            wd[k] = v

    def _deps(self, r, w, en=None):
        evs = []
        own = self.esem.get(en) if en is not None else None
        for h in w:
            cand = [h.w] + list(h.r.values())
            if h.psum and own is not None:
                cand = [ev for ev in cand if ev is not None and ev[0] != own]
            evs.extend(cand)
        return evs

    @staticmethod
    def _raw(r):
        return [h.w for h in r]

    @staticmethod
    def _mark(ev, r, w):
        for h in r:
            h.r[ev[0]] = ev
        for h in w:
            h.w = ev
            h.r = {}

    def op(self, en, fn, r=(), w=()):
        self._wait(en, self._deps(r, w, en), self._raw(r))
        inst = fn(self.E[en])
        self.ecnt[en] += 1
        inst.then_inc(self.sems[self.esem[en]], 1)
        ev = (self.esem[en], self.ecnt[en])
        self._mark(ev, r, w)
        return ev

    def dma(self, q, out, in_, r=(), w=()):
        d = self.dq[q]
        i = d["rr"]
        d["rr"] = (i + 1) % NQ
        evs = self._deps(r, w)
        if d["cnt"][i] > 0:
            evs.append((d["sems"][i], 16 * d["cnt"][i]))
        self._wait(q, evs, self._raw(r))
        inst = self.E[q].dma_start(out=out, in_=in_)
        d["cnt"][i] += 1
        inst.then_inc(self.sems[d["sems"][i]], 16)
        ev = (d["sems"][i], 16 * d["cnt"][i])
        self._mark(ev, r, w)
        return ev

    def all_events(self):
        evs = [(self.esem[n], self.ecnt[n]) for n in self.E if self.ecnt[n] > 0]
        for q, d in self.dq.items():
            for i in range(NQ):
                if d["cnt"][i] > 0:
                    evs.append((d["sems"][i], 16 * d["cnt"][i]))
        return evs

    def barrier(self):
        evs = self.all_events()
        for en in self.E:
            need = [e for e in evs if e[0] != self.esem[en]]
            self._wait(en, need)

    def final_wait(self):
        self._wait("sp", self.all_events())


def build_program():
    nc = bass.Bass("TRN2", target_bir_lowering=False)
    k = KB(nc)

    def din(name, shape, dt=F32):
        return nc.dram_tensor(name, list(shape), dt, kind="ExternalInput").ap()

    def dscratch(name, shape, dt):
        kind = "ExternalOutput" if (DEBUG and name in ("h2tm_scr", "xs_scr", "ys_scr", "x1_scr")) else "Internal"
        return nc.dram_tensor(name, list(shape), dt, kind=kind).ap()

    x_pre = din("x_pre", [HALF, D])
    x_own = din("x_own", [HALF, D])
    cT_d = din("cT", [128, 8])
    b_adaT_d = din("b_adaT", [128, 48])
    b_ada_row_d = din("b_ada_row", [1, 6 * D])
    g1T_d = din("g1T", [128, 8])
    g2T_d = din("g2T", [128, 8])
    w_ada_d = din("w_ada", [D, 6 * D])
    w_in_d = din("w_in", [D, IN_W])
    w_gk_up_d = din("w_gk_up", [16, 256])
    b_gkT_d = din("b_gkT", [128, 2])
    b_gk_bc_d = din("b_gk_bc", [128, 256])
    ggla_d = din("ggla_col", [128, 1])
    gq_d = din("gq_col", [128, 1])
    gkn_d = din("gk_col", [128, 1])
    lam4_d = din("lam4", [128, 4, 64])
    gsub_d = din("gsub_bc", [128, 128])
    w_out_d = din("w_out", [D, D])
    w_router_d = din("w_router", [D, E])
    b_router_d = din("b_router_bc", [128, E])
    w_gu_d = din("w_gate_up", [E, D, 2 * D])
    b_gu_rows_d = din("b_gu_rows", [E * 128, 16])
    g2_bc_d = din("g2_bc", [128, D])
    ustrict_d = din("ustrict", [128, 128])
    ones_f_d = din("ones_f", [128, 128])
    iota64_d = din("iota64", [128, NBLK])
    pidx_d = din("pidx", [128, 1])
    w_dn_d = din("w_down", [E, D, D])
    b_dn_d = din("b_down", [E, D])
    bias_d = din("bias_tiles", [8, 6, 128, 512])
    cF_d = din("cF", [128, 8])
    cP_d = din("cP", [128, 8])
    flag_d = din("flag", [128, 1])
    ident_d = din("ident", [128, 128])
    maskT_d = din("maskT", [128, 128])
    Lmat_d = din("Lmat", [128, 128])
    keep_d = din("keep", [128, 512])
    blk64_d = din("blk64", [128, 128])
    ones128_d = din("ones128", [128, 128])
    ones_row_d = din("ones_row", [1, 128])

    out_d = nc.dram_tensor("out", [HALF, D], F32, kind="ExternalOutput").ap()
    dbg = {}
    if DEBUG:
        for nm, shp in (("cnt", [128, E]), ("cs", [128, E]), ("pstart", [128, E]), ("be", [128, NBLK]),
                        ("idxf", [128, NT_OWN * 4]), ("wk", [128, NT_OWN * 4]), ("lg", [128, NT_OWN * E]),
                        ("wts", [128, NT_OWN * E]), ("msk", [128, NT_OWN * E])):
            dbg[nm] = nc.dram_tensor("dbg_" + nm, shp, F32, kind="ExternalOutput").ap()
        for nm, shp, dt_ in (("xT", [128, 8, 512], BF16), ("wgu", [128, 8, 2 * D], BF16), ("wdn", [128, 8, D], BF16),
                             ("bgu", [128, 16], F32), ("bdn", [128, D], F32), ("yT", [128, 8, 512], BF16),
                             ("bidx", [128, 3 * NBLK], I32)):
            dbg[nm] = nc.dram_tensor("dbg_" + nm, shp, dt_, kind="ExternalOutput").ap()

    hT_d = dscratch("hT_scr", [8, 128, S], BF16)
    h2tm_d = dscratch("h2tm_scr", [HALF, D], BF16)
    xs_d = dscratch("xs_scr", [PSLOTS, D], BF16)
    ys_d = dscratch("ys_scr", [PSLOTS, D], F32)
    wgu_bf_d = dscratch("wgu_bf_scr", [E * 128, 8 * 2 * D], BF16)
    wdn_bf_d = dscratch("wdn_bf_scr", [E * 128, 8 * D], BF16)
    x1_d = dscratch("x1_scr", [HALF, D], F32)

    top = ExitStack()

    uid = [0]

    def sb(es, name, shape, dt=F32):
        uid[0] += 1
        return es.enter_context(nc.sbuf_tensor(f"s{uid[0]}_{name}", list(shape), dt)).ap()

    PS = [top.enter_context(nc.psum_tensor(f"ps{i}", [128, 512], F32)).ap() for i in range(8)]
    PH = [H(psum=True) for _ in range(8)]

    def const(name, src, shape, dt=F32, q="sp"):
        t = sb(top, name, shape, dt)
        h = H()
        k.dma(q, t, src, w=[h])
        return t, h

    ident_f, h_ident_f = const("ident_f", ident_d, [128, 128])
    ident_b, h_ident_b = const("ident_b", ident_d, [128, 128], BF16, q="pool")
    maskT, h_maskT = const("maskT", maskT_d, [128, 128])
    Lmat, h_Lmat = const("Lmat", Lmat_d, [128, 128])
    keep, h_keep = const("keep", keep_d, [128, 512])
    blk64, h_blk64 = const("blk64", blk64_d, [128, 128], BF16, q="pool")
    ones128, h_ones128 = const("ones128", ones128_d, [128, 128], BF16, q="pool")
    ones_row, h_ones_row = const("ones_row", ones_row_d, [1, 128])
    cT, h_cT = const("cT", cT_d, [128, 8])
    b_adaT, h_b_adaT = const("b_adaT", b_adaT_d, [128, 48])
    g1T, h_g1T = const("g1T", g1T_d, [128, 8])
    g2T, h_g2T = const("g2T", g2T_d, [128, 8])
    w_gk_up = sb(top, "w_gk_up_pad", [128, 256]); h_w_gk_up = H()
    k.op("dve", lambda e: e.memset(w_gk_up, 0.0), w=[h_w_gk_up])
    k.dma("sp", w_gk_up[0:16, :], w_gk_up_d, w=[h_w_gk_up])
    b_gkT, h_b_gkT = const("b_gkT", b_gkT_d, [128, 2])
    b_gk_bc, h_b_gk_bc = const("b_gk_bc", b_gk_bc_d, [128, 256])
    ggla, h_ggla = const("ggla", ggla_d, [128, 1])
    gq, h_gq = const("gq", gq_d, [128, 1])
    gkn, h_gkn = const("gkn", gkn_d, [128, 1])
    lam4, h_lam4 = const("lam4", lam4_d, [128, 4, 64])
    gsub, h_gsub = const("gsub", gsub_d, [128, 128])
    b_router, h_b_router = const("b_router", b_router_d, [128, E])
    cF0, h_cF0 = const("cF", cF_d, [128, 8])
    cP0, h_cP0 = const("cP", cP_d, [128, 8])
    SHIFT = 8.0
    cF = sb(top, "cF_sh", [128, 8]); h_cF = H()
    cP = sb(top, "cP_sh", [128, 8]); h_cP = H()
    k.op("dve", lambda e: e.tensor_scalar(cF, cF0, -SHIFT, None, ALU.add), r=[h_cF0], w=[h_cF])
    k.op("dve", lambda e: e.tensor_scalar(cP, cP0, -SHIFT, None, ALU.add), r=[h_cP0], w=[h_cP])
    flag, h_flag = const("flag", flag_d, [128, 1])
    ustrict, h_ustrict = const("ustrict", ustrict_d, [128, 128])
    ones_f, h_ones_f = const("ones_f", ones_f_d, [128, 128])
    iota64, h_iota64 = const("iota64", iota64_d, [128, NBLK])
    lane_id, h_pidx = const("lane_id", pidx_d, [128, 1])

    a1T = sb(top, "a1T", [128, 8]); h_a1T = H()
    sh1T = sb(top, "sh1T", [128, 8]); h_sh1T = H()
    gt2_bc = sb(top, "gt2_bc", [128, D]); h_gt2 = H()
    wts = sb(top, "wts", [128, NT_OWN, E]); h_wts = H()
    lg_all = sb(top, "lg_all", [128, NT_OWN, E]); h_lg_all = H()
    top8_all = sb(top, "top8_all", [128, NT_OWN, 8]); h_top8_all = H()
    msk_all = sb(top, "msk_all", [128, NT_OWN, E]); h_msk_all = H()
    idxf_all = sb(top, "idxf_all", [128, NT_OWN, 4]); h_idxf_all = H()
    wk_all = sb(top, "wk_all", [128, NT_OWN, 4]); h_wk_all = H()
    idx_all = sb(top, "idx_all", [128, NT_OWN * 4], I32); h_idx_all = H()
    blk_idx = sb(top, "blk_idx", [128, 3 * NBLK], I32); h_blk_idx = H()
    modbc_d = dscratch("modbc_scr", [3, 128, D], F32)
    neg_bgkT = sb(top, "neg_bgkT", [128, 2]); h_neg_bgkT = H()
    gq8 = sb(top, "gq8", [128, 1]); h_gq8 = H()
    neglam = sb(top, "neglam", [128, 1]); h_neglam = H()
    gsub8 = sb(top, "gsub8", [128, 128]); h_gsub8 = H()
    eps_col = sb(top, "eps_col", [128, 1]); h_eps = H()
    one_col = sb(top, "one_col", [128, 1]); h_one = H()
    k.op("dve", lambda e: e.memset(eps_col, EPS), w=[h_eps])
    k.op("dve", lambda e: e.memset(one_col, 1.0), w=[h_one])

    def act(out, in_, func, r, w, bias=None, scale=None):
        kw = {}
        if bias is not None:
            kw["bias"] = bias
        if scale is not None:
            kw["scale"] = scale
        return k.op("act", lambda e: e.activation(out=out, in_=in_, func=func, **kw), r=r, w=w)

    def mm(out, lhsT, rhs, start, stop, r, bank, skip=False):
        kw = {"skip_group_check": True} if skip else {}
        return k.op("pe", lambda e: e.matmul(out, lhsT, rhs, start=start, stop=stop, **kw), r=r, w=[PH[bank]])

    def rstd_from_ss(out_col, ss_col, scale, r_h, w_h, tmp, h_tmp):
        act(tmp, ss_col, AF.Ln, r=[r_h, h_eps], w=[h_tmp], bias=eps_col[0:tmp.shape[0], :], scale=scale)
        act(out_col, tmp, AF.Exp, r=[h_tmp], w=[w_h], scale=-0.5)

    with ExitStack() as es:
        cond = sb(es, "cond", [128, 8]); h_cond = H()
        act(cond, cT, AF.Silu, r=[h_cT], w=[h_cond])
        wa = [sb(es, f"wa{i}", [128, 8, D]) for i in range(2)]
        h_wa = [H(), H()]
        modT = sb(es, "modT", [128, 2, 8]); h_modT = H()
        rows = sb(es, "rows", [1, 4, D]); h_rows = H()
        brow = sb(es, "brow", [1, 4, D]); h_brow = H()
        g2bc = sb(es, "g2bc", [128, D]); h_g2bc = H()
        gt1_bc = sb(es, "gt1_bc0", [128, D]); h_gt1 = H()
        a2_bc = sb(es, "a2_bc0", [128, D]); h_a2bc = H()
        sh2_bc = sb(es, "sh2_bc0", [128, D]); h_sh2bc = H()
        k.dma("sp", g2bc, g2_bc_d, w=[h_g2bc])
        row_slot = {2: 0, 5: 1, 3: 2, 4: 3}
        for s6_, ri_ in row_slot.items():
            k.dma("sp", brow[:, ri_, :], b_ada_row_d[:, s6_ * D:(s6_ + 1) * D], w=[h_brow])
        fm_slot = {0: 0, 1: 1}
        for s6 in range(6):
            wb, hb = wa[s6 % 2], h_wa[s6 % 2]
            k.dma("sp", wb, w_ada_d[:, s6 * D:(s6 + 1) * D].rearrange("(kc p) n -> p kc n", p=128), w=[hb])
            if s6 in fm_slot:
                sl = fm_slot[s6]
                for nt in range(8):
                    for kc in range(8):
                        mm(PS[0][:, nt:nt + 1], wb[:, kc, nt * 128:(nt + 1) * 128], cond[:, kc:kc + 1],
                           start=(kc == 0), stop=(kc == 7), r=[hb, h_cond], bank=0, skip=True)
                k.op("dve", lambda e: e.tensor_tensor(out=modT[:, sl, :], in0=PS[0][:, 0:8],
                                                      in1=b_adaT[:, s6 * 8:(s6 + 1) * 8], op=ALU.add),
                     r=[h_b_adaT], w=[PH[0], h_modT])
            else:
                ri = row_slot[s6]
                for half in range(2):
                    for kc in range(8):
                        mm(PS[1][0:1, :], cond[:, kc:kc + 1], wb[:, kc, half * 512:(half + 1) * 512],
                           start=(kc == 0), stop=(kc == 7), r=[hb, h_cond], bank=1)
                    k.op("dve", lambda e: e.tensor_tensor(out=rows[:, ri, half * 512:(half + 1) * 512],
                                                          in0=PS[1][0:1, :],
                                                          in1=brow[:, ri, half * 512:(half + 1) * 512], op=ALU.add),
                         r=[h_brow], w=[PH[1], h_rows])
                dst, hd_ = {2: (gt1_bc, h_gt1), 5: (gt2_bc, h_gt2), 3: (sh2_bc, h_sh2bc), 4: (a2_bc, h_a2bc)}[s6]
                for half in range(2):
                    mm(PS[2], ones_row, rows[:, ri, half * 512:(half + 1) * 512], start=True, stop=True,
                       r=[h_ones_row, h_rows], bank=2)
                    k.op("dve", lambda e: e.tensor_copy(dst[:, half * 512:(half + 1) * 512], PS[2]),
                         w=[PH[2], hd_])
        k.op("dve", lambda e: e.scalar_tensor_tensor(out=a1T, in0=modT[:, 1, :], scalar=1.0, in1=g1T,
                                                     op0=ALU.add, op1=ALU.mult), r=[h_modT, h_g1T], w=[h_a1T])
        k.op("dve", lambda e: e.tensor_copy(sh1T, modT[:, 0, :]), r=[h_modT], w=[h_sh1T])
        k.op("dve", lambda e: e.scalar_tensor_tensor(out=a2_bc, in0=a2_bc, scalar=1.0, in1=g2bc,
                                                     op0=ALU.add, op1=ALU.mult), r=[h_a2bc, h_g2bc], w=[h_a2bc])
        k.dma("sp", modbc_d[0], gt1_bc, r=[h_gt1])
        k.dma("sp", modbc_d[1], a2_bc, r=[h_a2bc])
        k.dma("sp", modbc_d[2], sh2_bc, r=[h_sh2bc])
        k.op("dve", lambda e: e.tensor_scalar(neg_bgkT, b_gkT, -1.0, None, ALU.mult), r=[h_b_gkT], w=[h_neg_bgkT])
        k.op("dve", lambda e: e.tensor_scalar(gq8, gq, 0.125, None, ALU.mult), r=[h_gq], w=[h_gq8])
        k.op("dve", lambda e: e.tensor_scalar(gsub8, gsub, 1.0 - LAMBDA_INIT, None, ALU.mult), r=[h_gsub], w=[h_gsub8])
        lp = sb(es, "lp", [128, 2, 64]); h_lp = H()
        ls = sb(es, "ls", [128, 2]); h_ls = H()
        le = sb(es, "le", [128, 2]); h_le = H()
        k.op("dve", lambda e: e.tensor_tensor(out=lp[:, 0, :], in0=lam4[:, 0, :], in1=lam4[:, 1, :], op=ALU.mult),
             r=[h_lam4], w=[h_lp])
        k.op("dve", lambda e: e.tensor_tensor(out=lp[:, 1, :], in0=lam4[:, 2, :], in1=lam4[:, 3, :], op=ALU.mult),
             r=[h_lam4], w=[h_lp])
        k.op("dve", lambda e: e.tensor_reduce(out=ls, in_=lp, axis=AX.X, op=ALU.add), r=[h_lp], w=[h_ls])
        act(le, ls, AF.Exp, r=[h_ls], w=[h_le])
        k.op("dve", lambda e: e.scalar_tensor_tensor(out=neglam, in0=le[:, 1:2], scalar=-LAMBDA_INIT, in1=le[:, 0:1],
                                                     op0=ALU.add, op1=ALU.subtract), r=[h_le], w=[h_neglam])
        k.barrier()

    all_casts = []
    for ex_ in range(E):
        all_casts.append((wgu_bf_d[ex_ * 128:(ex_ + 1) * 128, :].rearrange("p (kc f) -> p kc f", kc=8),
                          w_gu_d[ex_].rearrange("(kc p) f -> p kc f", p=128)))
        all_casts.append((wdn_bf_d[ex_ * 128:(ex_ + 1) * 128, :].rearrange("p (kc f) -> p kc f", kc=8),
                          w_dn_d[ex_].rearrange("(kc p) f -> p kc f", p=128)))

    def issue_casts(n):
        for _ in range(min(n, len(all_casts))):
            dst_, src_ = all_casts.pop(0)
            k.dma("pool", dst_, src_)

    with ExitStack() as es:
        xb = [sb(es, f"xb{i}", [128, D]) for i in range(3)]
        h_xb = [H() for _ in range(3)]
        sq = sb(es, "sq_junk", [128, D], BF16); h_sq = H()
        ss = [sb(es, f"ss{i}", [128, 1]) for i in range(2)]; h_ss = [H(), H()]
        sd = [sb(es, f"sd{i}", [128, 1]) for i in range(2)]; h_sd = [H(), H()]
        rs = [sb(es, f"rs{i}", [128, 1]) for i in range(2)]; h_rs = [H(), H()]
        xn = [sb(es, f"xn{i}", [128, D], BF16) for i in range(2)]; h_xn = [H(), H()]
        hT = [sb(es, f"hTst{i}", [128, 8, 512], BF16) for i in range(2)]; h_hT = [H(), H()]; h_hTa = [H(), H()]
        psb = [PS[0].bitcast(BF16), PS[1].bitcast(BF16)]
        def p1_stage1(i):
            src = x_pre if i < 32 else x_own
            ti = i % 32
            b3 = i % 3
            b2 = i % 2
            k.dma("sp", xb[b3], src[ti * 128:(ti + 1) * 128, :], w=[h_xb[b3]])
            act(sq, xb[b3], AF.Square, r=[h_xb[b3]], w=[h_sq])
            k.op("dve", lambda e: e.tensor_reduce(out=ss[b2], in_=sq, axis=AX.X, op=ALU.add), r=[h_sq], w=[h_ss[b2]])
            rstd_from_ss(rs[b2], ss[b2], 1.0 / D, h_ss[b2], h_rs[b2], sd[b2], h_sd[b2])
            k.op("dve", lambda e: e.tensor_scalar(xn[b2], xb[b3], rs[b2], None, ALU.mult),
                 r=[h_xb[b3], h_rs[b2]], w=[h_xn[b2]])

        def p1_stage2(i):
            b2 = i % 2
            sbi = i // 4
            hb = (sbi % 2)
            bd, ba = 2 * b2, 2 * b2 + 1
            pvd = PS[bd].bitcast(BF16)
            pva = PS[ba].bitcast(BF16)
            for kc in range(8):
                pv_, bank_ = (pvd, bd) if kc < 4 else (pva, ba)
                k.op("pe", lambda e: e.transpose(pv_[:, (kc % 4) * 128:(kc % 4 + 1) * 128], xn[b2][:, kc * 128:(kc + 1) * 128],
                                                 ident_b), r=[h_xn[b2], h_ident_b], w=[PH[bank_]])
            for kc in range(4):
                dst = hT[hb][:, kc, (i % 4) * 128:(i % 4 + 1) * 128]
                k.op("dve", lambda e: e.tensor_scalar(dst, pvd[:, kc * 128:(kc + 1) * 128], a1T[:, kc:kc + 1],
                                                      sh1T[:, kc:kc + 1], ALU.mult, ALU.add),
                     r=[h_a1T, h_sh1T], w=[PH[bd], h_hT[hb]])
            for kc in range(4, 8):
                dst = hT[hb][:, kc, (i % 4) * 128:(i % 4 + 1) * 128]
                act(dst, pva[:, (kc - 4) * 128:(kc - 3) * 128], AF.Identity, r=[h_a1T, h_sh1T], w=[PH[ba], h_hTa[hb]],
                    bias=sh1T[:, kc:kc + 1], scale=a1T[:, kc:kc + 1])
            if i % 4 == 3:
                k.dma("pool", hT_d[:, :, sbi * 512:(sbi + 1) * 512].rearrange("k p t -> p k t"), hT[hb], r=[h_hT[hb], h_hTa[hb]])

        p1_stage1(0)
        for i in range(NT_K):
            if i + 1 < NT_K:
                p1_stage1(i + 1)
            p1_stage2(i)
            if i % 8 == 7:
                issue_casts(1)
        k.barrier()

    mix_es = ExitStack()
    mixT = sb(mix_es, "mixT", [128, 8, HALF], BF16)
    h_mixT = H()

    def load_w(es, name, col0, ncols, q="pool"):
        t = sb(es, name, [128, 8, ncols], BF16)
        h = H()
        k.dma(q, t, w_in_d[:, col0:col0 + ncols].rearrange("(kc p) n -> p kc n", p=128), w=[h])
        return t, h

    for P in range(2):
        with ExitStack() as es:
            wq, h_wq = load_w(es, "g_wq", OFF_QG + P * 128, 128)
            wk, h_wk = load_w(es, "g_wk", OFF_KG + P * 128, 128)
            wv, h_wv = load_w(es, "g_wv", OFF_VG + P * 256, 256)
            wr, h_wr = load_w(es, "g_wr", OFF_RG + P * 256, 256)
            wgk = sb(es, "g_wgk_pad", [128, 8, 128], BF16); h_wgk = H()
            k.op("dve", lambda e: e.memset(wgk, 0.0), w=[h_wgk])
            k.dma("pool", wgk[:, :, 0:16], w_in_d[:, OFF_GK:OFF_GK + 16].rearrange("(kc p) n -> p kc n", p=128), w=[h_wgk])
            hTb = [sb(es, f"g_hT{i}", [128, 8, 512], BF16) for i in range(2)]; h_hTb = [H(), H()]
            gkT2 = [sb(es, f"g_gkT{i}", [128, 512]) for i in range(2)]; h_gkT2 = [H(), H()]
            e1 = sb(es, "g_e1", [128, 512]); h_e1 = H()
            spf = sb(es, "g_spf", [128, 512]); h_spf = H()
            cum = sb(es, "g_cum", [128, 512]); h_cum = H()
            Eq2 = [sb(es, f"g_Eq{i}", [128, 512]) for i in range(2)]; h_Eq2 = [H(), H()]
            Ek = sb(es, "g_Ek", [128, 512]); h_Ek = H()
            keT2 = [sb(es, f"g_keT{i}", [128, 512], BF16) for i in range(2)]; h_keT2 = [H(), H()]
            qeTz2 = [[sb(es, f"g_qeTz{j}_{i}", [128, 512], BF16) for i in range(2)] for j in range(2)]; h_qeT2 = [H(), H()]
            for j_ in range(2):
                k.op("dve", lambda e: e.memset(qeTz2[j_][0][64:128, :], 0.0), w=[h_qeT2[j_]])
                k.op("dve", lambda e: e.memset(qeTz2[j_][1][0:64, :], 0.0), w=[h_qeT2[j_]])
            rsT2 = [sb(es, f"g_rsT{i}", [128, 2, 512]) for i in range(2)]; h_rsT2 = [H(), H()]
            vtm2 = [sb(es, f"g_vtm{i}", [128, 256], BF16) for i in range(2)]; h_vtm2 = [H(), H()]
            ks2 = [sb(es, f"g_ks{i}", [128, 128], BF16) for i in range(2)]; h_ks2 = [H(), H()]
            AT2 = [sb(es, f"g_AT{i}", [128, 128], BF16) for i in range(2)]; h_AT2 = [H(), H()]
            sqo2 = [sb(es, f"g_sqo{i}", [128, 128], BF16) for i in range(2)]; h_sqo2 = [H(), H()]
            sdo2 = [sb(es, f"g_sdo{i}", [128, 128]) for i in range(2)]; h_sdo2 = [H(), H()]
            rso2 = [sb(es, f"g_rso{i}", [128, 128]) for i in range(2)]; h_rso2 = [H(), H()]
            t12 = [sb(es, f"g_t1{i}", [128, 128]) for i in range(2)]; h_t12 = [H(), H()]
            gtm = sb(es, "g_gtm", [128, 128]); h_gtm = H()
            e1t = sb(es, "g_e1t", [128, 128]); h_e1t = H()
            spt = sb(es, "g_spt", [128, 128]); h_spt = H()
            Es = sb(es, "g_Es", [128, 128]); h_Es = H()
            ks = sb(es, "g_ks", [128, 128], BF16); h_ks = H()
            AT = sb(es, "g_AT", [128, 128], BF16); h_AT = H()
            Sf = sb(es, "g_Sf", [128, 128]); h_Sf = H()
            Sb = sb(es, "g_Sb", [128, 128], BF16); h_Sb = H()
            sqo = sb(es, "g_sqo", [128, 128], BF16); h_sqo = H()
            sdo = sb(es, "g_sdo", [128, 128]); h_sdo = H()
            rso = sb(es, "g_rso", [128, 128]); h_rso = H()
            t1 = sb(es, "g_t1", [128, 128]); h_t1 = H()
            k.op("dve", lambda e: e.memset(Sf, 0.0), w=[h_Sf])
            k.op("dve", lambda e: e.memset(Sb, 0.0), w=[h_Sb])
            def fm(sbi):
                own = sbi >= 8
                fb = sbi % 2
                hTc, h_hTc = hTb[fb], h_hTb[fb]
                k.dma("sp", hTc, hT_d[:, :, sbi * 512:(sbi + 1) * 512].rearrange("k p t -> p k t"), w=[h_hTc])
                for kc in range(8):
                    mm(PS[0], wgk[:, kc, :], hTc[:, kc, :], start=(kc == 0), stop=(kc == 7), r=[h_wgk, h_hTc], bank=0)
                k.op("dve", lambda e: e.tensor_copy(gkT2[fb], PS[0]), w=[PH[0], h_gkT2[fb]])
                mm(PS[1], w_gk_up[:, P * 128:(P + 1) * 128], gkT2[fb], start=True, stop=True, r=[h_w_gk_up, h_gkT2[fb]], bank=1)
                act(e1, PS[1], AF.Exp, r=[h_neg_bgkT], w=[PH[1], h_e1], bias=neg_bgkT[:, P:P + 1], scale=-1.0)
                act(spf, e1, AF.Ln, r=[h_e1, h_one], w=[h_spf], bias=one_col)
                k.op("dve", lambda e: e.tensor_tensor_scan(out=cum, data0=keep, data1=spf, initial=0.0,
                                                           op0=ALU.mult, op1=ALU.add), r=[h_keep, h_spf], w=[h_cum])
                act(Eq2[fb], cum, AF.Exp, r=[h_cum], w=[h_Eq2[fb]], scale=-1.0 / 16.0)
                act(Ek, cum, AF.Exp, r=[h_cum], w=[h_Ek], scale=1.0 / 16.0)
                for kc in range(8):
                    mm(PS[2], wk[:, kc, :], hTc[:, kc, :], start=(kc == 0), stop=(kc == 7), r=[h_wk, h_hTc], bank=2)
                k.op("dve", lambda e: e.tensor_tensor(out=keT2[fb], in0=PS[2], in1=Ek, op=ALU.mult),
                     r=[h_Ek], w=[PH[2], h_keT2[fb]])
                if own:
                    for kc in range(8):
                        mm(PS[0], wq[:, kc, :], hTc[:, kc, :], start=(kc == 0), stop=(kc == 7), r=[h_wq, h_hTc], bank=0)
                    for hq in range(2):
                        pr = slice(hq * 64, (hq + 1) * 64)
                        k.op("dve", lambda e: e.scalar_tensor_tensor(out=qeTz2[fb][hq][pr, :], in0=PS[0][pr, :], scalar=0.125,
                                                                     in1=Eq2[fb][pr, :], op0=ALU.mult, op1=ALU.mult),
                             r=[h_Eq2[fb]], w=[PH[0], h_qeT2[fb]])
                    for hd in range(2):
                        for kc in range(8):
                            mm(PS[1 + hd], wr[:, kc, hd * 128:(hd + 1) * 128], hTc[:, kc, :], start=(kc == 0), stop=(kc == 7),
                               r=[h_wr, h_hTc], bank=1 + hd)
                        act(rsT2[fb][:, hd, :], PS[1 + hd], AF.Silu, r=[], w=[PH[1 + hd], h_rsT2[fb]])

            fm(0)
            for sbi in range(16):
                issue_casts(1)
                own = sbi >= 8
                fb = sbi % 2
                hTc, h_hTc = hTb[fb], h_hTb[fb]
                gkT, h_gkT = gkT2[fb], h_gkT2[fb]
                Eq, h_Eq = Eq2[fb], h_Eq2[fb]
                keT, h_keT = keT2[fb], h_keT2[fb]
                qeTz, h_qeT = qeTz2[fb], h_qeT2[fb]
                rsT, h_rsT = rsT2[fb], h_rsT2[fb]
                if sbi == 8:
                    k.op("dve", lambda e: e.tensor_scalar(Sf, Sf, flag, None, ALU.mult), r=[h_flag, h_Sf], w=[h_Sf])
                    act(Sb, Sf, AF.Copy, r=[h_Sf], w=[h_Sb])
                BB = (7, 3)

                def stage_a(t):
                    tok = slice(t * 128, (t + 1) * 128)
                    ab = t % 2
                    for kc in range(8):
                        mm(PS[4][:, 0:128], hTc[:, kc, tok], wk[:, kc, :], start=(kc == 0), stop=(kc == 7),
                           r=[h_wk, h_hTc], bank=4)
                    for kc in range(8):
                        mm(PS[5][:, 0:256], hTc[:, kc, tok], wv[:, kc, :], start=(kc == 0), stop=(kc == 7),
                           r=[h_wv, h_hTc], bank=5)
                    act(vtm2[ab], PS[5][:, 0:256], AF.Copy, r=[], w=[PH[5], h_vtm2[ab]])
                    mm(PS[6][:, 0:128], gkT[:, tok], w_gk_up[:, P * 128:(P + 1) * 128], start=True, stop=True,
                       r=[h_gkT, h_w_gk_up], bank=6)
                    k.op("dve", lambda e: e.tensor_tensor(out=gtm, in0=PS[6][:, 0:128],
                                                          in1=b_gk_bc[:, P * 128:(P + 1) * 128], op=ALU.add),
                         r=[h_b_gk_bc], w=[PH[6], h_gtm])
                    act(e1t, gtm, AF.Exp, r=[h_gtm], w=[h_e1t], scale=-1.0)
                    act(spt, e1t, AF.Ln, r=[h_e1t, h_one], w=[h_spt], bias=one_col)
                    mm(PS[6][:, 128:256], Lmat, spt, start=True, stop=True, r=[h_Lmat, h_spt], bank=6)
                    act(Es, PS[6][:, 128:256], AF.Exp, r=[], w=[PH[6], h_Es])
                    k.op("dve", lambda e: e.tensor_tensor(out=ks2[ab], in0=PS[4][:, 0:128], in1=Es, op=ALU.mult),
                         r=[h_Es], w=[PH[4], h_ks2[ab]])

                def stage_b(t):
                    tok = slice(t * 128, (t + 1) * 128)
                    ab = t % 2
                    vt, h_vt, kst, h_kst = vtm2[ab], h_vtm2[ab], ks2[ab], h_ks2[ab]
                    if own:
                        gt = (sbi - 8) * 512 + t * 128
                        R = [slice(0, 64), slice(64, 128)]
                        for hd in range(2):
                            mm(PS[BB[hd]][:, 0:128], keT[:, tok], qeTz[hd][:, tok], start=True, stop=True,
                               r=[h_keT, h_qeT], bank=BB[hd])
                        for hd in range(2):
                            k.op("dve", lambda e: e.tensor_tensor(out=AT2[hd], in0=PS[BB[hd]][:, 0:128], in1=maskT, op=ALU.mult),
                                 r=[h_maskT], w=[PH[BB[hd]], h_AT2[hd]])
                        for hd in range(2):
                            mm(PS[BB[hd]][:, 128:256], vt[:, hd * 128:(hd + 1) * 128], AT2[hd], start=True, stop=False,
                               r=[h_vt, h_AT2[hd]], bank=BB[hd], skip=True)
                            mm(PS[BB[hd]][:, 128:256], Sb, qeTz[hd][:, tok], start=False, stop=True,
                               r=[h_Sb, h_qeT], bank=BB[hd], skip=True)
                        for hd in range(2):
                            act(sqo2[hd], PS[BB[hd]][:, 128:256], AF.Square, r=[], w=[PH[BB[hd]], h_sqo2[hd]])
                        for hd in range(2):
                            mm(PS[BB[hd]][:, 256:384], ones128, sqo2[hd], start=True, stop=True, r=[h_ones128, h_sqo2[hd]],
                               bank=BB[hd], skip=True)
                        for hd in range(2):
                            act(sdo2[hd], PS[BB[hd]][:, 256:384], AF.Ln, r=[h_eps], w=[PH[BB[hd]], h_sdo2[hd]], bias=eps_col)
                        for hd in range(2):
                            act(rso2[hd], sdo2[hd], AF.Exp, r=[h_sdo2[hd]], w=[h_rso2[hd]], scale=-0.5)
                        for hd in range(2):
                            k.op("dve", lambda e: e.scalar_tensor_tensor(out=t12[hd], in0=PS[BB[hd]][:, 128:256], scalar=ggla,
                                                                         in1=rso2[hd], op0=ALU.mult, op1=ALU.mult),
                                 r=[h_ggla, h_rso2[hd]], w=[PH[BB[hd]], h_t12[hd]])
                        for hd in range(2):
                            k.op("dve", lambda e: e.tensor_tensor(out=mixT[:, 2 * P + hd, gt:gt + 128], in0=t12[hd],
                                                                  in1=rsT[:, hd, tok], op=ALU.mult),
                                 r=[h_t12[hd], h_rsT], w=[h_mixT])
                    mm(PS[6][:, 256:512], kst, vt, start=True, stop=True, r=[h_kst, h_vt], bank=6, skip=True)
                    for hd in range(2):
                        pr = slice(hd * 64, (hd + 1) * 64)
                        dcol = Eq[pr, t * 128 + 127:t * 128 + 128]
                        k.op("dve", lambda e: e.scalar_tensor_tensor(out=Sf[pr, :], in0=Sf[pr, :], scalar=dcol,
                                                                     in1=PS[6][pr, 256 + hd * 128:256 + (hd + 1) * 128],
                                                                     op0=ALU.mult, op1=ALU.add), r=[h_Eq, h_Sf], w=[PH[6], h_Sf])
                    act(Sb, Sf, AF.Copy, r=[h_Sf], w=[h_Sb])

                stage_a(0)
                for t in range(4):
                    if t + 1 < 4:
                        stage_a(t + 1)
                    if t == 2 and sbi + 1 < 16:
                        fm(sbi + 1)
                    stage_b(t)
            k.barrier()

    with ExitStack() as es:
        KT = sb(es, "a_KT", [128, S], BF16); h_KT = H()
        QTz = [sb(es, f"a_QT{i}", [128, HALF], BF16) for i in range(2)]; h_QT = H()
        k.op("dve", lambda e: e.memset(QTz[0][64:128, :], 0.0), w=[h_QT])
        k.op("dve", lambda e: e.memset(QTz[1][0:64, :], 0.0), w=[h_QT])
        Vg = sb(es, "a_V", [128, NT_K, 130], BF16); h_V = H()
        k.op("dve", lambda e: e.memset(Vg, 1.0), w=[h_V])
        hTb = [sb(es, f"a_hT{i}", [128, 8, 512], BF16) for i in range(2)]; h_hTb = [H(), H()]
        bt = sb(es, "a_bias", [128, 2, 6, 512]); h_bt = H()
        sqk2 = [sb(es, f"a_sqk{i}", [128, 512], BF16) for i in range(2)]; h_sqk2 = [H(), H()]
        sdk2 = [sb(es, f"a_sdk{i}", [128, 512]) for i in range(2)]; h_sdk2 = [H(), H()]
        PT = [sb(es, f"a_PT{i}", [128, 512], BF16) for i in range(4)]; h_PT = [H() for _ in range(4)]
        stmp0 = sb(es, "a_stmp0", [128, 512]); h_stmp0 = H()
        stmp = [stmp0, stmp0]; h_stmp = [h_stmp0, h_stmp0]
        vT_sb = sb(es, "a_vT", [128, 512], BF16); h_vT_sb = H()
        on0 = sb(es, "a_on0", [128, 4, 128]); h_on0 = H()
        od_single = sb(es, "a_od", [128, 4, 128]); h_od_single = H()
        od2 = [od_single, od_single]; h_od2 = [h_od_single, h_od_single]
        yb4 = [sb(es, f"a_yb{i}", [128, 128], BF16) for i in range(4)]; h_yb4 = [H() for _ in range(4)]
        pending = []
        rl = sb(es, "a_rl", [128, 4]); h_rl = H()
        nrl = sb(es, "a_nrl", [128, 4]); h_nrl = H()
        sqd = sb(es, "a_sqd", [128, 128]); h_sqd = H()
        ssd = sb(es, "a_ssd", [128, 1]); h_ssd = H()
        sdd = sb(es, "a_sdd", [128, 1]); h_sdd = H()
        rsd = sb(es, "a_rsd", [128, 1]); h_rsd = H()
        wqd = sb(es, "a_wq", [128, 8, 128], BF16); h_wqd = H()
        wkd = sb(es, "a_wk", [128, 8, 128], BF16); h_wkd = H()
        wvd = sb(es, "a_wv", [128, 8, 128], BF16); h_wvd = H()
        for hd in range(4):
            for wt_, hw_, off_ in ((wqd, h_wqd, OFF_QD), (wkd, h_wkd, OFF_KD), (wvd, h_wvd, OFF_VD)):
                k.dma("pool", wt_, w_in_d[:, off_ + hd * 128:off_ + (hd + 1) * 128].rearrange("(kc p) n -> p kc n", p=128),
                      w=[hw_])
            k.dma("sp", bt, bias_d[2 * hd:2 * hd + 2].rearrange("m s p q -> p m s q"), w=[h_bt])
            cast_jobs = [all_casts.pop(0) for _ in range(min(6, len(all_casts)))]
            if hd == 3:
                cast_jobs += all_casts
                all_casts = []
            units_p = []
            for sbi in range(16):
                units_p.append((sbi, 0))
                if sbi >= 8:
                    units_p.append((sbi, 1))
            loaded = set()

            def ensure_hT(sbi):
                if sbi not in loaded:
                    loaded.add(sbi)
                    hb_ = sbi % 2
                    k.dma("sp", hTb[hb_], hT_d[:, :, sbi * 512:(sbi + 1) * 512].rearrange("k p t -> p k t"), w=[h_hTb[hb_]])

            def proj_a(u):
                sbi, which = units_p[u]
                ensure_hT(sbi)
                hTc, h_hTc = hTb[sbi % 2], h_hTb[sbi % 2]
                wsel, h_wsel = (wkd, h_wkd) if which == 0 else (wqd, h_wqd)
                pa = u % 3
                for kc in range(8):
                    mm(PS[pa], wsel[:, kc, :], hTc[:, kc, :], start=(kc == 0), stop=(kc == 7), r=[h_wsel, h_hTc], bank=pa)
                act(sqk2[u % 2], PS[pa], AF.Square, r=[], w=[PH[pa], h_sqk2[u % 2]])

            def proj_b(u):
                sbi, which = units_p[u]
                gcol, h_gcol = (gkn, h_gkn) if which == 0 else (gq8, h_gq8)
                pa = u % 3
                pm = 3 if u % 2 == 0 else 6
                sq_, h_sq_ = sqk2[u % 2], h_sqk2[u % 2]
                sd_, h_sd_ = sdk2[u % 2], h_sdk2[u % 2]
                mm(PS[pm], blk64, sq_, start=True, stop=True, r=[h_blk64, h_sq_], bank=pm)
                act(sd_, PS[pm], AF.Ln, r=[h_eps], w=[PH[pm], h_sd_], bias=eps_col)
                act(sd_, sd_, AF.Exp, r=[h_sd_], w=[h_sd_], scale=-0.5)
                if which == 0:
                    dst = KT[:, sbi * 512:(sbi + 1) * 512]
                    k.op("dve", lambda e: e.scalar_tensor_tensor(out=dst, in0=PS[pa], scalar=gcol, in1=sd_,
                                                                 op0=ALU.mult, op1=ALU.mult),
                         r=[h_gcol, h_sd_], w=[PH[pa], h_KT])
                else:
                    for mq in range(2):
                        pr = slice(mq * 64, (mq + 1) * 64)
                        dst = QTz[mq][pr, (sbi - 8) * 512:(sbi - 7) * 512]
                        k.op("dve", lambda e: e.scalar_tensor_tensor(out=dst, in0=PS[pa][pr, :], scalar=gcol[pr, :],
                                                                     in1=sd_[pr, :], op0=ALU.mult, op1=ALU.mult),
                             r=[h_gcol, h_sd_], w=[PH[pa], h_QT])

            def proj_v(sbi):
                hTc, h_hTc = hTb[sbi % 2], h_hTb[sbi % 2]
                for kc in range(8):
                    mm(PS[4], wvd[:, kc, :], hTc[:, kc, :], start=(kc == 0), stop=(kc == 7), r=[h_wvd, h_hTc], bank=4)
                act(vT_sb, PS[4], AF.Copy, r=[], w=[PH[4], h_vT_sb])
                pvv = PS[5].bitcast(BF16)
                for t in range(4):
                    k.op("pe", lambda e: e.transpose(pvv[:, t * 128:(t + 1) * 128], vT_sb[:, t * 128:(t + 1) * 128], ident_b),
                         r=[h_vT_sb, h_ident_b], w=[PH[5]])
                k.op("dve", lambda e: e.tensor_copy(Vg[:, sbi * 4:sbi * 4 + 4, 0:128],
                                                    pvv[:, 0:512].rearrange("p (t e) -> p t e", t=4)), w=[PH[5], h_V])

            proj_a(0)
            for u in range(len(units_p)):
                sbi_u, which_u = units_p[u]
                last_of_sb = (u + 1 == len(units_p)) or (units_p[u + 1][0] != sbi_u)
                if last_of_sb:
                    proj_v(sbi_u)
                if u + 1 < len(units_p):
                    proj_a(u + 1)
                proj_b(u)
            k._wait("pool", [], [h_QT.w, h_V.w])
            for dst_, src_ in cast_jobs:
                k.dma("pool", dst_, src_)
            SB = (0, 1, 6)
            LOOK = 2
            pidx = 0
            stream = []
            for j_ in range(8):
                for m_ in range(2):
                    for g_ in range(32 + 4 * j_ + 4):
                        stream.append((j_, m_, g_))

            def emit_S(si):
                j_, m_, g_ = stream[si]
                sbank_ = SB[si % 3]
                mm(PS[sbank_], KT[:, g_ * 128:(g_ + 1) * 128], QTz[m_][:, j_ * 512:(j_ + 1) * 512],
                   start=True, stop=True, r=[h_KT, h_QT], bank=sbank_)

            for si in range(LOOK):
                emit_S(si)
            sbase = 0
            prev_pv = [None]
            for j in range(8):
                for m in range(2):
                    hm = 2 * hd + m
                    ob = (2, 3) if m == 0 else (4, 5)
                    nkeys = 32 + 4 * j + 4
                    base = sbase
                    sbase += nkeys
                    for g in range(nkeys):
                        if g == 12 and pending:
                            pending.pop(0)()
                        if base + g + LOOK < len(stream):
                            emit_S(base + g + LOOK)
                        sbank = SB[(base + g) % 3]
                        pb = pidx % 4
                        pidx += 1
                        tile_id = None
                        qb_min = 0
                        if g == 31 and j == 0:
                            tile_id = 5
                        elif j > 0 and g == 32 + 4 * j - 1:
                            tile_id = 4
                        elif g >= 32 + 4 * j:
                            tile_id = g - (32 + 4 * j)
                            qb_min = tile_id
                        if tile_id is None:
                            bcol, h_bcol = (cP, h_cP) if g < 32 else (cF, h_cF)
                            act(PT[pb], PS[sbank], AF.Exp, r=[h_bcol], w=[PH[sbank], h_PT[pb]],
                                bias=bcol[:, hm:hm + 1])
                        else:
                            st_, h_st = stmp[g % 2], h_stmp[g % 2]
                            k.op("dve", lambda e: e.scalar_tensor_tensor(out=st_, in0=PS[sbank], scalar=-SHIFT,
                                                                         in1=bt[:, m, tile_id, :], op0=ALU.add, op1=ALU.add),
                                 r=[h_bt], w=[PH[sbank], h_st])
                            act(PT[pb], st_, AF.Exp, r=[h_st], w=[h_PT[pb]])
                        def pv_closure(g=g, pb=pb, qb_min=qb_min, ob=ob, nkeys=nkeys):
                            for qb in range(qb_min, 4):
                                bnk = ob[0] if qb < 3 else ob[1]
                                c0 = (qb % 3) * 130
                                first_in_bank = (g == 0) and (qb == 0 or qb == 3)
                                mm(PS[bnk][:, c0:c0 + 129], PT[pb][:, qb * 128:(qb + 1) * 128], Vg[:, g, 0:129],
                                   start=first_in_bank, stop=(g == nkeys - 1), r=[h_PT[pb], h_V], bank=bnk, skip=True)

                        if prev_pv[0] is not None:
                            prev_pv[0]()
                        prev_pv[0] = pv_closure
                        if g == nkeys - 1:
                            prev_pv[0]()
                            prev_pv[0] = None
                    for qb in range(4):
                        bnk = ob[0] if qb < 3 else ob[1]
                        c0 = (qb % 3) * 130
                        k.op("dve", lambda e: e.reciprocal(rl[:, qb:qb + 1], PS[bnk][:, c0 + 128:c0 + 129]),
                             w=[PH[bnk], h_rl])
                        if m == 0:
                            k.op("dve", lambda e: e.tensor_scalar(on0[:, qb, :], PS[bnk][:, c0:c0 + 128],
                                                                  rl[:, qb:qb + 1], None, ALU.mult),
                                 r=[h_rl], w=[PH[bnk], h_on0])
                        else:
                            k.op("dve", lambda e: e.tensor_scalar(nrl[:, qb:qb + 1], rl[:, qb:qb + 1], neglam, None,
                                                                  ALU.mult), r=[h_rl, h_neglam], w=[h_nrl])
                            k.op("dve", lambda e: e.scalar_tensor_tensor(out=od2[j % 2][:, qb, :], in0=PS[bnk][:, c0:c0 + 128],
                                                                         scalar=nrl[:, qb:qb + 1], in1=on0[:, qb, :],
                                                                         op0=ALU.mult, op1=ALU.add),
                                 r=[h_nrl, h_on0], w=[PH[bnk], h_od2[j % 2]])
                    if m == 1:
                        def epilogue(j=j, hd=hd):
                            for qb in range(4):
                                k.op("dve", lambda e: e.tensor_tensor(out=sqd, in0=od2[j % 2][:, qb, :], in1=od2[j % 2][:, qb, :], op=ALU.mult),
                                     r=[h_od2[j % 2]], w=[h_sqd])
                                k.op("dve", lambda e: e.tensor_reduce(out=ssd, in_=sqd, axis=AX.X, op=ALU.add),
                                     r=[h_sqd], w=[h_ssd])
                                rstd_from_ss(rsd, ssd, 1.0 / 128.0, h_ssd, h_rsd, sdd, h_sdd)
                                k.op("dve", lambda e: e.scalar_tensor_tensor(out=yb4[qb], in0=od2[j % 2][:, qb, :], scalar=rsd, in1=gsub8,
                                                                             op0=ALU.mult, op1=ALU.mult),
                                     r=[h_od2[j % 2], h_rsd, h_gsub8], w=[h_yb4[qb]])
                            pv = PS[7].bitcast(BF16)
                            for qb in range(4):
                                k.op("pe", lambda e: e.transpose(pv[:, qb * 128:(qb + 1) * 128], yb4[qb], ident_b),
                                     r=[h_yb4[qb], h_ident_b], w=[PH[7]])
                            q0 = j * 512
                            k.op("dve", lambda e: e.tensor_copy(mixT[:, 4 + hd, q0:q0 + 512], pv[:, 0:512]),
                                 w=[PH[7], h_mixT])
                        pending.append(epilogue)
            while pending:
                pending.pop(0)()
        k.barrier()

    with ExitStack() as es:
        wo_f = sb(es, "wo_f", [128, 8, D]); h_wo_f = H()
        wo = sb(es, "wo", [128, 8, D], BF16); h_wo = H()
        wr_f = sb(es, "wr_f", [128, 8, E]); h_wr_f = H()
        gt1_bc = sb(es, "gt1_bc", [128, D]); h_gt1 = H()
        a2_bc = sb(es, "a2_bc", [128, D]); h_a2bc = H()
        sh2_bc = sb(es, "sh2_bc", [128, D]); h_sh2bc = H()
        k.dma("sp", gt1_bc, modbc_d[0], w=[h_gt1])
        k.dma("sp", a2_bc, modbc_d[1], w=[h_a2bc])
        k.dma("sp", sh2_bc, modbc_d[2], w=[h_sh2bc])
        k.dma("sp", wo_f, w_out_d.rearrange("(kc p) n -> p kc n", p=128), w=[h_wo_f])
        k.dma("sp", wr_f, w_router_d.rearrange("(kc p) n -> p kc n", p=128), w=[h_wr_f])
        for kc in range(8):
            k.op("dve", lambda e: e.tensor_tensor(out=wo[:, kc, :], in0=wo_f[:, kc, :], in1=gt1_bc, op=ALU.mult),
                 r=[h_wo_f, h_gt1], w=[h_wo])
        xb = [sb(es, f"p4_x{i}", [128, D]) for i in range(2)]; h_xb = [H(), H()]
        x1 = [sb(es, f"p4_x1{i}", [128, D]) for i in range(2)]; h_x1 = [H(), H()]
        sq = sb(es, "p4_sq", [128, D], BF16); h_sq = H()
        ss = sb(es, "p4_ss", [128, 1]); h_ss = H()
        sd = sb(es, "p4_sd", [128, 1]); h_sd = H()
        rs = sb(es, "p4_rs", [128, 1]); h_rs = H()
        h2t = sb(es, "p4_h2t", [128, D]); h_h2t = H()
        h2tb = [sb(es, f"p4_h2tb{i}", [128, D], BF16) for i in range(2)]; h_h2tb = [H(), H()]
        h2f = sb(es, "p4_h2f", [128, 8, 128]); h_h2f = H()
        nmx = sb(es, "p4_nmx", [128, 1]); h_nmx = H()
        ex = sb(es, "p4_ex", [128, E]); h_ex = H()
        exm = sb(es, "p4_exm", [128, E]); h_exm = H()
        den = sb(es, "p4_den", [128, 1]); h_den = H()
        rden = sb(es, "p4_rden", [128, 1]); h_rden = H()
        def p4_stage1(t):
            b2 = t % 2
            tok = slice(t * 128, (t + 1) * 128)
            k.dma("sp", xb[b2], x_own[tok, :], w=[h_xb[b2]])
            for half in range(2):
                for kc in range(8):
                    mm(PS[half], mixT[:, kc, tok], wo[:, kc, half * 512:(half + 1) * 512], start=(kc == 0), stop=(kc == 7),
                       r=[h_mixT, h_wo], bank=half)
                k.op("dve", lambda e: e.tensor_tensor(out=x1[b2][:, half * 512:(half + 1) * 512], in0=PS[half],
                                                      in1=xb[b2][:, half * 512:(half + 1) * 512], op=ALU.add),
                     r=[h_xb[b2]], w=[PH[half], h_x1[b2]])
            k.dma("pool", x1_d[tok, :], x1[b2], r=[h_x1[b2]])

        h2t2 = [h2t, sb(es, "p4_h2t_b", [128, D])]; h_h2t2 = [h_h2t, H()]

        def p4_stage2a(t):
            b2 = t % 2
            tok = slice(t * 128, (t + 1) * 128)
            ht, h_ht = h2t2[b2], h_h2t2[b2]
            act(sq, x1[b2], AF.Square, r=[h_x1[b2]], w=[h_sq])
            k.op("dve", lambda e: e.tensor_reduce(out=ss, in_=sq, axis=AX.X, op=ALU.add), r=[h_sq], w=[h_ss])
            rstd_from_ss(rs, ss, 1.0 / D, h_ss, h_rs, sd, h_sd)
            k.op("dve", lambda e: e.scalar_tensor_tensor(out=ht, in0=x1[b2], scalar=rs, in1=a2_bc, op0=ALU.mult,
                                                         op1=ALU.mult), r=[h_x1[b2], h_rs, h_a2bc], w=[h_ht])
            k.op("dve", lambda e: e.tensor_tensor(out=ht, in0=ht, in1=sh2_bc, op=ALU.add),
                 r=[h_ht, h_sh2bc], w=[h_ht])
            act(h2tb[b2], ht, AF.Copy, r=[h_ht], w=[h_h2tb[b2]])
            k.dma("pool", h2tm_d[tok, :], h2tb[b2], r=[h_h2tb[b2]])

        def p4_stage2b(t):
            b2 = t % 2
            ht, h_ht = h2t2[b2], h_h2t2[b2]
            for kc in range(8):
                bank = 2 + kc // 4
                k.op("pe", lambda e: e.transpose(PS[bank][:, (kc % 4) * 128:(kc % 4 + 1) * 128],
                                                 ht[:, kc * 128:(kc + 1) * 128], ident_f),
                     r=[h_ht, h_ident_f], w=[PH[bank]])
            for hf in range(2):
                act(h2f[:, hf * 4:(hf + 1) * 4, :], PS[2 + hf].rearrange("p (k t) -> p k t", k=4), AF.Copy, r=[],
                    w=[PH[2 + hf], h_h2f])
            for kc in range(8):
                mm(PS[4][:, 0:E], h2f[:, kc, :], wr_f[:, kc, :], start=(kc == 0), stop=(kc == 7), r=[h_h2f, h_wr_f], bank=4)
            lg = lg_all[:, t, :]
            top8 = top8_all[:, t, :]
            msk = msk_all[:, t, :]
            k.op("dve", lambda e: e.tensor_tensor(out=lg, in0=PS[4][:, 0:E], in1=b_router, op=ALU.add),
                 r=[h_b_router], w=[PH[4], h_lg_all])
            k.op("dve", lambda e: e.max(out=top8, in_=lg), r=[h_lg_all], w=[h_top8_all])
            k.op("dve", lambda e: e.tensor_scalar(nmx, top8[:, 0:1], -1.0, None, ALU.mult), r=[h_top8_all], w=[h_nmx])
            k.op("dve", lambda e: e.tensor_scalar(msk, lg, top8[:, 3:4], None, ALU.is_ge), r=[h_lg_all, h_top8_all],
                 w=[h_msk_all])
            act(ex, lg, AF.Exp, r=[h_lg_all, h_nmx], w=[h_ex], bias=nmx)
            k.op("dve", lambda e: e.tensor_tensor(out=exm, in0=ex, in1=msk, op=ALU.mult), r=[h_ex, h_msk_all], w=[h_exm])
            k.op("dve", lambda e: e.tensor_reduce(out=den, in_=exm, axis=AX.X, op=ALU.add), r=[h_exm], w=[h_den])
            k.op("dve", lambda e: e.reciprocal(rden, den), r=[h_den], w=[h_rden])
            k.op("dve", lambda e: e.tensor_scalar(wts[:, t, :], exm, rden, None, ALU.mult), r=[h_exm, h_rden], w=[h_wts])

        p4_stage1(0)
        p4_stage2a(0)
        for t in range(NT_OWN):
            if t + 1 < NT_OWN:
                p4_stage1(t + 1)
            p4_stage2b(t)
            if t + 1 < NT_OWN:
                p4_stage2a(t + 1)
        k.barrier()
    mix_es.close()

    with ExitStack() as es:
        cnt = sb(es, "r_cnt", [128, E]); h_cnt = H()
        nblk = sb(es, "r_nblk", [128, E]); h_nblk = H()
        ones32 = sb(es, "r_ones32", [128, E]); h_ones32 = H()
        cs = sb(es, "r_cs", [128, E]); h_cs = H()
        pstart = sb(es, "r_pstart", [128, E]); h_pstart = H()
        be = sb(es, "r_be", [128, NBLK]); h_be = H()
        bidxf = sb(es, "r_bidxf", [128, 3, NBLK]); h_bidxf = H()
        dest = sb(es, "r_dest", [128, E]); h_dest = H()
        eq = sb(es, "r_eq", [128, E]); h_eq = H()
        pr = sb(es, "r_pr", [128, E]); h_pr = H()
        h2r = [sb(es, f"r_h2r{i}", [128, D], BF16) for i in range(3)]; h_h2r = [H() for _ in range(3)]
        for t in range(NT_OWN):
            mm(PS[0][:, 0:E], ones_f, msk_all[:, t, :], start=(t == 0), stop=(t == NT_OWN - 1), r=[h_ones_f, h_msk_all], bank=0)
        k.op("dve", lambda e: e.tensor_copy(cnt, PS[0][:, 0:E]), w=[PH[0], h_cnt])
        k.op("dve", lambda e: e.memset(nblk, 0.0), w=[h_nblk])
        k.op("dve", lambda e: e.memset(ones32, 1.0), w=[h_ones32])
        for j in range(HALF // MOE_B):
            k.op("dve", lambda e: e.scalar_tensor_tensor(out=nblk, in0=cnt, scalar=float(j * MOE_B), in1=nblk,
                                                         op0=ALU.is_gt, op1=ALU.add), r=[h_cnt, h_nblk], w=[h_nblk])
        k.op("dve", lambda e: e.tensor_tensor_scan(out=cs, data0=ones32, data1=nblk, initial=0.0, op0=ALU.mult,
                                                   op1=ALU.add), r=[h_ones32, h_nblk], w=[h_cs])
        k.op("dve", lambda e: e.tensor_tensor(out=pstart, in0=cs, in1=nblk, op=ALU.subtract), r=[h_cs, h_nblk], w=[h_pstart])
        k.op("dve", lambda e: e.tensor_scalar(pstart, pstart, float(MOE_B), None, ALU.mult), r=[h_pstart], w=[h_pstart])
        k.op("dve", lambda e: e.memset(be, 0.0), w=[h_be])
        for ex_ in range(E):
            k.op("dve", lambda e: e.scalar_tensor_tensor(out=be, in0=iota64, scalar=cs[:, ex_:ex_ + 1], in1=be,
                                                         op0=ALU.is_ge, op1=ALU.add), r=[h_iota64, h_cs, h_be], w=[h_be])
        k.op("dve", lambda e: e.tensor_scalar(bidxf[:, 0, :], be, 1024.0, lane_id, ALU.mult, ALU.add), r=[h_be, h_pidx], w=[h_bidxf])
        k.op("dve", lambda e: e.tensor_scalar(bidxf[:, 1, :], be, 128.0, lane_id, ALU.mult, ALU.add), r=[h_be, h_pidx], w=[h_bidxf])
        k.op("dve", lambda e: e.tensor_copy(bidxf[:, 2, :], be), r=[h_be], w=[h_bidxf])
        k.op("dve", lambda e: e.tensor_copy(blk_idx, bidxf.rearrange("p a b -> p (a b)")), r=[h_bidxf], w=[h_blk_idx])
        dest_all = sb(es, "r_dest_all", [128, NT_OWN, E]); h_dest_all = H()
        eq_all = sb(es, "r_eq_all", [128, NT_OWN * 4, E]); h_eq_all = H()
        pr_all = sb(es, "r_pr_all", [128, NT_OWN * 4, E]); h_pr_all = H()
        base_all = sb(es, "r_base_all", [128, NT_OWN, E]); h_base_all = H()
        for t in range(NT_OWN):
            mm(PS[1 + t // 16][:, (t % 16) * E:(t % 16 + 1) * E], ustrict, msk_all[:, t, :], start=(t % 16 == 0), stop=True,
               r=[h_ustrict, h_msk_all], bank=1 + t // 16, skip=True)
        for t in range(NT_OWN):
            mm(PS[3 + t // 16][:, (t % 16) * E:(t % 16 + 1) * E], ones_f, msk_all[:, t, :], start=(t % 16 == 0), stop=True,
               r=[h_ones_f, h_msk_all], bank=3 + t // 16, skip=True)
        k.op("dve", lambda e: e.tensor_copy(base_all[:, 0, :], pstart), r=[h_pstart], w=[h_base_all])
        for t in range(1, NT_OWN):
            tp = t - 1
            k.op("dve", lambda e: e.tensor_tensor(out=base_all[:, t, :], in0=PS[3 + tp // 16][:, (tp % 16) * E:(tp % 16 + 1) * E],
                                                  in1=base_all[:, t - 1, :], op=ALU.add), r=[h_base_all],
                 w=[PH[3 + tp // 16], h_base_all])
        for hf in range(2):
            k.op("dve", lambda e: e.tensor_tensor(out=dest_all[:, hf * 16:(hf + 1) * 16, :],
                                                  in0=PS[1 + hf].rearrange("p (t e) -> p t e", e=E),
                                                  in1=base_all[:, hf * 16:(hf + 1) * 16, :], op=ALU.add),
                 r=[h_base_all], w=[PH[1 + hf], h_dest_all])
        for t in range(NT_OWN):
            for kk in range(4):
                k.op("dve", lambda e: e.tensor_scalar(eq_all[:, 4 * t + kk, :], lg_all[:, t, :], top8_all[:, t, kk:kk + 1], None,
                                                      ALU.is_equal), r=[h_lg_all, h_top8_all], w=[h_eq_all])
        for t in range(NT_OWN):
            for kk in range(4):
                k.op("dve", lambda e: e.tensor_tensor(out=pr_all[:, 4 * t + kk, :], in0=eq_all[:, 4 * t + kk, :],
                                                      in1=dest_all[:, t, :], op=ALU.mult), r=[h_eq_all, h_dest_all], w=[h_pr_all])
        k.op("dve", lambda e: e.tensor_reduce(out=idxf_all.rearrange("p a b -> p (a b)"), in_=pr_all, axis=AX.X, op=ALU.add),
             r=[h_pr_all], w=[h_idxf_all])
        for t in range(NT_OWN):
            for kk in range(4):
                k.op("dve", lambda e: e.tensor_tensor(out=pr_all[:, 4 * t + kk, :], in0=eq_all[:, 4 * t + kk, :],
                                                      in1=wts[:, t, :], op=ALU.mult), r=[h_eq_all, h_wts, h_idxf_all], w=[h_pr_all])
        k.op("dve", lambda e: e.tensor_reduce(out=wk_all.rearrange("p a b -> p (a b)"), in_=pr_all, axis=AX.X, op=ALU.add),
             r=[h_pr_all], w=[h_wk_all])
        k.op("dve", lambda e: e.tensor_copy(idx_all, idxf_all.rearrange("p a b -> p (a b)")), r=[h_idxf_all], w=[h_idx_all])
        if DEBUG:
            k.dma("sp", dbg["cnt"], cnt, r=[h_cnt])
            k.dma("sp", dbg["cs"], cs, r=[h_cs])
            k.dma("sp", dbg["pstart"], pstart, r=[h_pstart])
            k.dma("sp", dbg["be"], be, r=[h_be])
            k.dma("sp", dbg["idxf"], idxf_all.rearrange("p a b -> p (a b)"), r=[h_idxf_all])
            k.dma("sp", dbg["wk"], wk_all.rearrange("p a b -> p (a b)"), r=[h_wk_all])
            k.dma("sp", dbg["lg"], lg_all.rearrange("p a b -> p (a b)"), r=[h_lg_all])
            k.dma("sp", dbg["wts"], wts.rearrange("p a b -> p (a b)"), r=[h_wts])
            k.dma("sp", dbg["msk"], msk_all.rearrange("p a b -> p (a b)"), r=[h_msk_all])
        h_xs = H()
        reg_bounds = nc.gpsimd.to_reg(PSLOTS - 1)
        reg_bw = nc.gpsimd.to_reg(E * D - 1)
        reg_bb = nc.gpsimd.to_reg(E * 128 - 1)
        reg_be = nc.gpsimd.to_reg(E - 1)
        for t in range(NT_OWN):
            b3 = t % 3
            k.dma("sp", h2r[b3], h2tm_d[t * 128:(t + 1) * 128, :], w=[h_h2r[b3]])
            for kk in range(4):
                k._wait("pool", [], [h_h2r[b3].w, h_idx_all.w])
                d = k.dq["pool"]
                i = d["rr"]; d["rr"] = (i + 1) % NQ
                if d["cnt"][i] > 0:
                    k._wait("pool", [(d["sems"][i], 16 * d["cnt"][i])])
                inst = nc.gpsimd.indirect_dma_start(
                    out=xs_d, out_offset=bass.IndirectOffsetOnAxis(ap=idx_all[:, t * 4 + kk:t * 4 + kk + 1], axis=0),
                    in_=h2r[b3], in_offset=None, bounds_check=reg_bounds, oob_is_err=False)
                d["cnt"][i] += 1
                inst.then_inc(k.sems[d["sems"][i]], 16)
                ev = (d["sems"][i], 16 * d["cnt"][i])
                k._mark(ev, [h_h2r[b3], h_idx_all], [h_xs])
        k.barrier()

    def igather(out, src, idx_ap, r, w, element_offset=0, bounds=None):
        d = k.dq["pool"]
        i = d["rr"]; d["rr"] = (i + 1) % NQ
        evs = k._deps(r, w, "pool")
        if d["cnt"][i] > 0:
            evs.append((d["sems"][i], 16 * d["cnt"][i]))
        k._wait("pool", evs, k._raw(r))
        kw = {}
        if bounds is not None:
            kw = {"bounds_check": bounds, "oob_is_err": False}
        inst = nc.gpsimd.indirect_dma_start(out=out, out_offset=None, in_=src,
                                            in_offset=bass.IndirectOffsetOnAxis(ap=idx_ap, axis=0),
                                            element_offset=element_offset, **kw)
        d["cnt"][i] += 1
        inst.then_inc(k.sems[d["sems"][i]], 16)
        ev = (d["sems"][i], 16 * d["cnt"][i])
        k._mark(ev, r, w)
        return ev

    w_gu_rows = wgu_bf_d
    w_dn_rows = wdn_bf_d

    NSUB = MOE_B // 128
    with ExitStack() as es:
        wgu = [sb(es, f"m_wgu{i}", [128, 8, 2 * D], BF16) for i in range(2)]; h_wgu = [[H() for _ in range(8)] for _ in range(2)]
        wdn = [sb(es, f"m_wdn{i}", [128, 8, D], BF16) for i in range(2)]; h_wdn = [[H() for _ in range(8)] for _ in range(2)]
        bgu = [sb(es, f"m_bgu{i}", [128, 16]) for i in range(2)]; h_bgu = [H(), H()]
        bgu1 = [sb(es, f"m_bgu1{i}", [128, 8]) for i in range(2)]; h_bgu1 = [H(), H()]
        bdn = [sb(es, f"m_bdn{i}", [128, D]) for i in range(2)]; h_bdn = [H(), H()]
        xsb = sb(es, "m_xs", [128, NSUB, D], BF16); h_xsb = H()
        xT = [sb(es, f"m_xT{i}", [128, 8, MOE_B], BF16) for i in range(2)]; h_xT = [H(), H()]; h_xTd = [H(), H()]
        yT = [sb(es, f"m_yT{i}", [128, 8, MOE_B], BF16) for i in range(2)]; h_yT = [H(), H()]
        gate = [sb(es, f"m_gate{i}", [128, MOE_B]) for i in range(2)]; h_gate = [H(), H()]
        sig = [sb(es, f"m_sig{i}", [128, MOE_B]) for i in range(2)]; h_sig = [H(), H()]
        upc = [sb(es, f"m_upc{i}", [128, MOE_B]) for i in range(2)]; h_upc = [H(), H()]
        gs = [sb(es, f"m_gs{i}", [128, MOE_B]) for i in range(2)]; h_gs = [H(), H()]
        ysb = [sb(es, f"m_ys{i}", [128, D]) for i in range(2)]; h_ysb = [H(), H()]
        h_ys = H()

        def load_block_weights(bi):
            sl = bi % 2
            ib = blk_idx[:, NBLK + bi:NBLK + bi + 1]
            igather(wgu[sl].rearrange("p k f -> p (k f)"), w_gu_rows, ib, r=[h_blk_idx], w=h_wgu[sl], bounds=reg_bb)
            igather(wdn[sl].rearrange("p k f -> p (k f)"), w_dn_rows, ib, r=[h_blk_idx], w=h_wdn[sl], bounds=reg_bb)
            igather(bgu[sl], b_gu_rows_d, blk_idx[:, NBLK + bi:NBLK + bi + 1], r=[h_blk_idx], w=[h_bgu[sl]], bounds=reg_bb)
            igather(bdn[sl], b_dn_d, blk_idx[:, 2 * NBLK + bi:2 * NBLK + bi + 1], r=[h_blk_idx], w=[h_bdn[sl]], bounds=reg_be)

        xsb2 = [xsb, sb(es, "m_xs2", [128, NSUB, D], BF16)]; h_xsb2 = [h_xsb, H()]

        def load_xs(bi):
            sl = bi % 2
            k.dma("pool", xsb2[sl], xs_d[bi * MOE_B:(bi + 1) * MOE_B, :].rearrange("(s p) d -> p s d", p=128), w=[h_xsb2[sl]])

        def transposes(bi):
            sl = bi % 2
            for s4 in range(NSUB):
                bank = 4 + s4 % 2
                pv = PS[bank].bitcast(BF16)
                for kc in range(8):
                    k.op("pe", lambda e: e.transpose(pv[:, kc * 128:(kc + 1) * 128], xsb2[sl][:, s4, kc * 128:(kc + 1) * 128], ident_b),
                         r=[h_xsb2[sl], h_ident_b], w=[PH[bank]])
                if s4 % 2 == 0:
                    act(xT[sl][:, :, s4 * 128:(s4 + 1) * 128], pv.rearrange("p (k t) -> p k t", k=8), AF.Copy, r=[],
                        w=[PH[bank], h_xT[sl]])
                else:
                    k.op("dve", lambda e: e.tensor_copy(xT[sl][:, :, s4 * 128:(s4 + 1) * 128], pv.rearrange("p (k t) -> p k t", k=8)),
                         w=[PH[bank], h_xTd[sl]])

        load_block_weights(0)
        load_xs(0)
        transposes(0)
        it = 0
        ysi = 0
        for bi in range(NBLK):
            sl = bi % 2
            if bi + 1 < NBLK:
                load_block_weights(bi + 1)
                load_xs(bi + 1)
            k.op("dve", lambda e: e.tensor_scalar(bgu1[sl], bgu[sl][:, 8:16], 1.0, None, ALU.add), r=[h_bgu[sl]], w=[h_bgu1[sl]])
            for ft in range(8):
                i2 = it % 2
                it += 1
                gb, ub = (0, 1) if i2 == 0 else (2, 3)
                for kc in range(8):
                    mm(PS[gb][:, 0:MOE_B], wgu[sl][:, kc, ft * 128:(ft + 1) * 128], xT[sl][:, kc, :], start=(kc == 0), stop=(kc == 7),
                       r=[h_wgu[sl][kc], h_xT[sl], h_xTd[sl]], bank=gb)
                for kc in range(8):
                    mm(PS[ub][:, 0:MOE_B], wgu[sl][:, kc, D + ft * 128:D + (ft + 1) * 128], xT[sl][:, kc, :], start=(kc == 0),
                       stop=(kc == 7), r=[h_wgu[sl][kc], h_xT[sl], h_xTd[sl]], bank=ub)
                k.op("dve", lambda e: e.tensor_scalar(gate[i2], PS[gb][:, 0:MOE_B], bgu[sl][:, ft:ft + 1], 7.0, ALU.add, ALU.min),
                     r=[h_bgu[sl]], w=[PH[gb], h_gate[i2]])
                act(sig[i2], gate[i2], AF.Sigmoid, r=[h_gate[i2]], w=[h_sig[i2]], scale=1.702)
                k.op("dve", lambda e: e.tensor_scalar(upc[i2], PS[ub][:, 0:MOE_B], bgu1[sl][:, ft:ft + 1], 8.0, ALU.add, ALU.min),
                     r=[h_bgu1[sl]], w=[PH[ub], h_upc[i2]])
                k.op("dve", lambda e: e.tensor_tensor(out=gs[i2], in0=gate[i2], in1=sig[i2], op=ALU.mult),
                     r=[h_gate[i2], h_sig[i2]], w=[h_gs[i2]])
                k.op("dve", lambda e: e.scalar_tensor_tensor(out=yT[sl][:, ft, :], in0=upc[i2], scalar=-6.0, in1=gs[i2],
                                                             op0=ALU.max, op1=ALU.mult), r=[h_gs[i2], h_upc[i2]], w=[h_yT[sl]])
            if bi + 1 < NBLK:
                transposes(bi + 1)
            for s4 in range(NSUB):
                yb2 = ysi % 2
                ysi += 1
                for half in range(2):
                    bank = 6 + half
                    for fc in range(8):
                        mm(PS[bank], yT[sl][:, fc, s4 * 128:(s4 + 1) * 128], wdn[sl][:, fc, half * 512:(half + 1) * 512],
                           start=(fc == 0), stop=False, r=[h_yT[sl], h_wdn[sl][fc]], bank=bank)
                    mm(PS[bank], ones_row, bdn[sl][0:1, half * 512:(half + 1) * 512], start=False, stop=True,
                       r=[h_ones_row, h_bdn[sl]], bank=bank)
                    act(ysb[yb2][:, half * 512:(half + 1) * 512], PS[bank], AF.Copy, r=[], w=[PH[bank], h_ysb[yb2]])
                r0 = bi * MOE_B + s4 * 128
                k.dma("sp", ys_d[r0:r0 + 128, :], ysb[yb2], r=[h_ysb[yb2]], w=[h_ys])
        k.barrier()

    with ExitStack() as es:
        gr = [[sb(es, f"c_g{i}_{kk}", [128, D]) for kk in range(4)] for i in range(2)]
        h_gr = [[H() for _ in range(4)] for _ in range(2)]
        x1b = [sb(es, f"c_x1{i}", [128, D]) for i in range(2)]; h_x1b = [H(), H()]
        acc = [sb(es, f"c_acc{i}", [128, D]) for i in range(2)]; h_acc = [H(), H()]
        for t in range(NT_OWN):
            b2 = t % 2
            tok = slice(t * 128, (t + 1) * 128)
            k.dma("sp", x1b[b2], x1_d[tok, :], w=[h_x1b[b2]])
            for kk in range(4):
                igather(gr[b2][kk], ys_d, idx_all[:, t * 4 + kk:t * 4 + kk + 1], r=[h_idx_all, h_ys], w=[h_gr[b2][kk]], bounds=reg_bounds)
            k.op("dve", lambda e: e.tensor_scalar(acc[b2], gr[b2][0], wk_all[:, t, 0:1], None, ALU.mult),
                 r=[h_gr[b2][0], h_wk_all], w=[h_acc[b2]])
            for kk in range(1, 4):
                k.op("dve", lambda e: e.scalar_tensor_tensor(out=acc[b2], in0=gr[b2][kk], scalar=wk_all[:, t, kk:kk + 1],
                                                             in1=acc[b2], op0=ALU.mult, op1=ALU.add),
                     r=[h_gr[b2][kk], h_wk_all, h_acc[b2]], w=[h_acc[b2]])
            k.op("dve", lambda e: e.tensor_tensor(out=acc[b2], in0=acc[b2], in1=gt2_bc, op=ALU.mult),
                 r=[h_acc[b2], h_gt2], w=[h_acc[b2]])
            k.op("dve", lambda e: e.tensor_tensor(out=acc[b2], in0=acc[b2], in1=x1b[b2], op=ALU.add),
                 r=[h_acc[b2], h_x1b[b2]], w=[h_acc[b2]])
            k.dma("sp", out_d[tok, :], acc[b2], r=[h_acc[b2]])
        k.barrier()
    k.final_wait()
    top.close()
    return nc


def _t5_bucket(n):
    max_exact = 16
    nf = np.maximum(n, 1).astype(np.float32)
    large = max_exact + (np.log(nf / np.float32(max_exact)) / np.float32(math.log(128 / 16))
                         * np.float32(32 - max_exact)).astype(np.int32)
    large = np.minimum(large, 31)
    return np.where(n < max_exact, n, large)


def _bias_tiles(table, half):
    tiles = np.empty((8, 6, 128, 512), np.float32)
    kk = np.arange(128)[:, None]
    qq = np.arange(512)[None, :]
    for i in range(4):
        d = qq - (i * 128 + kk)
        b = _t5_bucket(np.maximum(d, 0))
        for hm in range(8):
            tiles[hm, i] = np.where(d >= 0, table[b, hm], np.float32(NEG))
    d = qq + 128 - kk
    b = _t5_bucket(d)
    for hm in range(8):
        tiles[hm, 4] = table[b, hm]
        tiles[hm, 5] = table[b, hm] if half == 1 else np.float32(NEG)
    return tiles


_NC_CACHE = {}


def kernel(x, c, rel_bias_table, w_ada, b_ada, g_norm1, w_in, w_gk_up, b_gk_up, g_gla_out, g_qnorm, g_knorm,
           lambda_q1, lambda_k1, lambda_q2, lambda_k2, g_subln, w_out, g_norm2, w_router, b_router,
           w_gate_up, b_gate_up, w_down, b_down):
    f = lambda a: np.ascontiguousarray(np.asarray(a, dtype=np.float32))
    x, c, table = f(x), f(c), f(rel_bias_table)
    w_ada, b_ada, w_in = f(w_ada)[0], f(b_ada)[0], f(w_in)[0]
    w_gk_up, b_gk_up = f(w_gk_up)[0], f(b_gk_up)[0]
    w_out, w_router, b_router = f(w_out)[0], f(w_router)[0], f(b_router)[0]
    w_gate_up, b_gate_up, w_down, b_down = f(w_gate_up)[0], f(b_gate_up)[0], f(w_down)[0], f(b_down)[0]
    colT = lambda v: f(v.reshape(-1, 128).T)
    rep = lambda v, n=128: f(np.broadcast_to(v[None, :], (n, v.shape[0])))

    ii = np.arange(128)
    shared = {
        "b_adaT": colT(b_ada), "b_ada_row": f(b_ada[None, :]),
        "g1T": colT(f(g_norm1)[0]), "g2T": colT(f(g_norm2)[0]),
        "w_ada": w_ada, "w_in": w_in, "w_gk_up": w_gk_up,
        "b_gkT": colT(b_gk_up), "b_gk_bc": rep(b_gk_up),
        "ggla_col": f(f(g_gla_out)[0][:, None]),
        "gq_col": f(np.tile(f(g_qnorm)[0], 2)[:, None]), "gk_col": f(np.tile(f(g_knorm)[0], 2)[:, None]),
        "lam4": f(np.broadcast_to(np.stack([f(lambda_q1)[0], f(lambda_k1)[0], f(lambda_q2)[0], f(lambda_k2)[0]])[None],
                                  (128, 4, 64))),
        "gsub_bc": rep(f(g_subln)[0]),
        "w_out": w_out, "w_router": w_router, "b_router_bc": rep(b_router),
        "w_gate_up": w_gate_up, "b_gu_rows": f(b_gate_up.reshape(E, 16, 128).transpose(0, 2, 1).reshape(E * 128, 16)),
        "g2_bc": rep(f(g_norm2)[0]),
        "ustrict": (ii[:, None] < ii[None, :]).astype(np.float32),
        "ones_f": np.ones((128, 128), np.float32),
        "iota64": f(np.broadcast_to(np.arange(NBLK, dtype=np.float32)[None, :], (128, NBLK))),
        "pidx": f(ii.astype(np.float32)[:, None]),
        "w_down": w_down, "b_down": b_down,
        "cF": rep(table[31]),
        "ident": np.eye(128, dtype=np.float32),
        "maskT": (ii[:, None] <= ii[None, :]).astype(np.float32),
        "Lmat": np.where(ii[:, None] > ii[None, :], np.float32(-1.0 / 16.0), np.float32(0.0)).astype(np.float32),
        "keep": f(np.tile(np.where(ii == 0, 0.0, 1.0).astype(np.float32), 4)[None, :].repeat(128, 0)),
        "blk64": f(np.kron(np.eye(2, dtype=np.float32), np.full((64, 64), 1.0 / 64.0, np.float32))),
        "ones128": np.full((128, 128), 1.0 / 128.0, np.float32),
        "ones_row": np.ones((1, 128), np.float32),
    }
    tiles = [_bias_tiles(table, 0), _bias_tiles(table, 1)]
    in_maps = []
    for core in range(8):
        b, half = core // 2, core % 2
        m = dict(shared)
        m["x_pre"] = f(x[b, 0:HALF])
        m["x_own"] = f(x[b, half * HALF:(half + 1) * HALF])
        m["cT"] = colT(c[b])
        m["bias_tiles"] = tiles[half]
        m["cP"] = shared["cF"] if half == 1 else np.full((128, 8), NEG, np.float32)
        m["flag"] = np.full((128, 1), float(half), np.float32)
        in_maps.append(m)
    if "nc" not in _NC_CACHE:
        _NC_CACHE["nc"] = build_program()
    nc = _NC_CACHE["nc"]
    res = run_bass_kernel_spmd(nc, in_maps, core_ids=list(range(8)))
    out = np.empty((4, S, D), np.float32)
    for core in range(8):
        b, half = core // 2, core % 2
        out[b, half * HALF:(half + 1) * HALF] = res.results[core]["out"]
    kernel.last_results = res.results
    return out
```

```python
import math
from contextlib import ExitStack
import numpy as np
import concourse.bass as bass
import concourse.mybir as mybir
from concourse.bass_utils import run_bass_kernel_spmd
from concourse.alu_op_type import AluOpType as ALU

F32 = mybir.dt.float32
BF16 = mybir.dt.bfloat16
AF = mybir.ActivationFunctionType
AX = mybir.AxisListType

D = 1024
S = 8192
HALF = 4096
NT_OWN = 32
NT_K = 64
E = 32
IN_W = 3088
OFF_QG, OFF_KG, OFF_VG, OFF_RG, OFF_GK, OFF_QD, OFF_KD, OFF_VD = 0, 256, 512, 1024, 1536, 1552, 2064, 2576
EPS = 1e-6
NEG = -30000.0
LAMBDA_INIT = 0.8 - 0.6 * math.exp(-0.3 * 0)
NQ = 12
MOE_B = 256
NBLK = (HALF * 4 - E) // MOE_B + E
PSLOTS = NBLK * MOE_B
I32 = mybir.dt.int32

import os
DEBUG = bool(os.environ.get('KDEBUG'))


class H:
    __slots__ = ("w", "r", "psum")

    def __init__(self, psum=False):
        self.w = None
        self.r = {}
        self.psum = psum


class KB:
    def __init__(self, nc):
        self.nc = nc
        self.E = {"pe": nc.tensor, "act": nc.scalar, "dve": nc.vector, "pool": nc.gpsimd, "sp": nc.sync}
        self.sems = []
        self.esem = {}
        self.ecnt = {}
        for n in self.E:
            self.esem[n] = self._newsem("e_" + n)
            self.ecnt[n] = 0
        self.waited = {n: {} for n in self.E}
        self.dq = {}
        for q in ("sp", "pool"):
            self.dq[q] = {"sems": [self._newsem(f"d_{q}{i}") for i in range(NQ)], "cnt": [0] * NQ, "rr": 0}

    def _newsem(self, name):
        s = self.nc.alloc_semaphore(name)
        self.sems.append(s)
        return len(self.sems) - 1

    def _wait(self, en, evs, raw=()):
        need = {}
        own = self.esem[en]
        for ev in list(raw) + list(evs):
            if ev is None:
                continue
            k, v = ev
            if k == own and en == "pe":
                continue
            if need.get(k, 0) < v:
                need[k] = v
        wd = self.waited[en]
        for k, v in need.items():
            if wd.get(k, 0) >= v:
                continue
            self.E[en].wait_ge(self.sems[k], v)
            wd[k] = v

    def _deps(self, r, w, en=None):
        evs = []
        own = self.esem.get(en) if en is not None else None
        for h in w:
            cand = [h.w] + list(h.r.values())
            if h.psum and own is not None:
                cand = [ev for ev in cand if ev is not None and ev[0] != own]
            evs.extend(cand)
        return evs

    @staticmethod
    def _raw(r):
        return [h.w for h in r]

    @staticmethod
    def _mark(ev, r, w):
        for h in r:
            h.r[ev[0]] = ev
        for h in w:
            h.w = ev
            h.r = {}

    def op(self, en, fn, r=(), w=()):
        self._wait(en, self._deps(r, w, en), self._raw(r))
        inst = fn(self.E[en])
        self.ecnt[en] += 1
        inst.then_inc(self.sems[self.esem[en]], 1)
        ev = (self.esem[en], self.ecnt[en])
        self._mark(ev, r, w)
        return ev

    def dma(self, q, out, in_, r=(), w=()):
        d = self.dq[q]
        i = d["rr"]
        d["rr"] = (i + 1) % NQ
        evs = self._deps(r, w)
        if d["cnt"][i] > 0:
            evs.append((d["sems"][i], 16 * d["cnt"][i]))
        self._wait(q, evs, self._raw(r))
        inst = self.E[q].dma_start(out=out, in_=in_)
        d["cnt"][i] += 1
        inst.then_inc(self.sems[d["sems"][i]], 16)
        ev = (d["sems"][i], 16 * d["cnt"][i])
        self._mark(ev, r, w)
        return ev

    def all_events(self):
        evs = [(self.esem[n], self.ecnt[n]) for n in self.E if self.ecnt[n] > 0]
        for q, d in self.dq.items():
            for i in range(NQ):
                if d["cnt"][i] > 0:
                    evs.append((d["sems"][i], 16 * d["cnt"][i]))
        return evs

    def barrier(self):
        evs = self.all_events()
        for en in self.E:
            need = [e for e in evs if e[0] != self.esem[en]]
            self._wait(en, need)

    def final_wait(self):
        self._wait("sp", self.all_events())


def build_program():
    nc = bass.Bass("TRN2", target_bir_lowering=False)
    k = KB(nc)

    def din(name, shape, dt=F32):
        return nc.dram_tensor(name, list(shape), dt, kind="ExternalInput").ap()

    def dscratch(name, shape, dt):
        kind = "ExternalOutput" if (DEBUG and name in ("h2tm_scr", "xs_scr", "ys_scr", "x1_scr")) else "Internal"
        return nc.dram_tensor(name, list(shape), dt, kind=kind).ap()

    x_pre = din("x_pre", [HALF, D])
    x_own = din("x_own", [HALF, D])
    cT_d = din("cT", [128, 8])
    b_adaT_d = din("b_adaT", [128, 48])
    b_ada_row_d = din("b_ada_row", [1, 6 * D])
    g1T_d = din("g1T", [128, 8])
    g2T_d = din("g2T", [128, 8])
    w_ada_d = din("w_ada", [D, 6 * D])
    w_in_d = din("w_in", [D, IN_W])
    w_gk_up_d = din("w_gk_up", [16, 256])
    b_gkT_d = din("b_gkT", [128, 2])
    b_gk_bc_d = din("b_gk_bc", [128, 256])
    ggla_d = din("ggla_col", [128, 1])
    gq_d = din("gq_col", [128, 1])
    gkn_d = din("gk_col", [128, 1])
    lam4_d = din("lam4", [128, 4, 64])
    gsub_d = din("gsub_bc", [128, 128])
    w_out_d = din("w_out", [D, D])
    w_router_d = din("w_router", [D, E])
    b_router_d = din("b_router_bc", [128, E])
    w_gu_d = din("w_gate_up", [E, D, 2 * D])
    b_gu_rows_d = din("b_gu_rows", [E * 128, 16])
    g2_bc_d = din("g2_bc", [128, D])
    ustrict_d = din("ustrict", [128, 128])
    ones_f_d = din("ones_f", [128, 128])
    iota64_d = din("iota64", [128, NBLK])
    pidx_d = din("pidx", [128, 1])
    w_dn_d = din("w_down", [E, D, D])
    b_dn_d = din("b_down", [E, D])
    bias_d = din("bias_tiles", [8, 6, 128, 512])
    cF_d = din("cF", [128, 8])
    cP_d = din("cP", [128, 8])
    flag_d = din("flag", [128, 1])
    ident_d = din("ident", [128, 128])
    maskT_d = din("maskT", [128, 128])
    Lmat_d = din("Lmat", [128, 128])
    keep_d = din("keep", [128, 512])
    blk64_d = din("blk64", [128, 128])
    ones128_d = din("ones128", [128, 128])
    ones_row_d = din("ones_row", [1, 128])

    out_d = nc.dram_tensor("out", [HALF, D], F32, kind="ExternalOutput").ap()
    dbg = {}
    if DEBUG:
        for nm, shp in (("cnt", [128, E]), ("cs", [128, E]), ("pstart", [128, E]), ("be", [128, NBLK]),
                        ("idxf", [128, NT_OWN * 4]), ("wk", [128, NT_OWN * 4]), ("lg", [128, NT_OWN * E]),
                        ("wts", [128, NT_OWN * E]), ("msk", [128, NT_OWN * E])):
            dbg[nm] = nc.dram_tensor("dbg_" + nm, shp, F32, kind="ExternalOutput").ap()
        for nm, shp, dt_ in (("xT", [128, 8, 512], BF16), ("wgu", [128, 8, 2 * D], BF16), ("wdn", [128, 8, D], BF16),
                             ("bgu", [128, 16], F32), ("bdn", [128, D], F32), ("yT", [128, 8, 512], BF16),
                             ("bidx", [128, 3 * NBLK], I32)):
            dbg[nm] = nc.dram_tensor("dbg_" + nm, shp, dt_, kind="ExternalOutput").ap()

    hT_d = dscratch("hT_scr", [8, 128, S], BF16)
    h2tm_d = dscratch("h2tm_scr", [HALF, D], BF16)
    xs_d = dscratch("xs_scr", [PSLOTS, D], BF16)
    ys_d = dscratch("ys_scr", [PSLOTS, D], F32)
    wgu_bf_d = dscratch("wgu_bf_scr", [E * 128, 8 * 2 * D], BF16)
    wdn_bf_d = dscratch("wdn_bf_scr", [E * 128, 8 * D], BF16)
    x1_d = dscratch("x1_scr", [HALF, D], F32)

    top = ExitStack()

    uid = [0]

    def sb(es, name, shape, dt=F32):
        uid[0] += 1
        return es.enter_context(nc.sbuf_tensor(f"s{uid[0]}_{name}", list(shape), dt)).ap()

    PS = [top.enter_context(nc.psum_tensor(f"ps{i}", [128, 512], F32)).ap() for i in range(8)]
    PH = [H(psum=True) for _ in range(8)]

    def const(name, src, shape, dt=F32, q="sp"):
        t = sb(top, name, shape, dt)
        h = H()
        k.dma(q, t, src, w=[h])
        return t, h

    ident_f, h_ident_f = const("ident_f", ident_d, [128, 128])
    ident_b, h_ident_b = const("ident_b", ident_d, [128, 128], BF16, q="pool")
    maskT, h_maskT = const("maskT", maskT_d, [128, 128])
    Lmat, h_Lmat = const("Lmat", Lmat_d, [128, 128])
    keep, h_keep = const("keep", keep_d, [128, 512])
    blk64, h_blk64 = const("blk64", blk64_d, [128, 128], BF16, q="pool")
    ones128, h_ones128 = const("ones128", ones128_d, [128, 128], BF16, q="pool")
    ones_row, h_ones_row = const("ones_row", ones_row_d, [1, 128])
    cT, h_cT = const("cT", cT_d, [128, 8])
    b_adaT, h_b_adaT = const("b_adaT", b_adaT_d, [128, 48])
    g1T, h_g1T = const("g1T", g1T_d, [128, 8])
    g2T, h_g2T = const("g2T", g2T_d, [128, 8])
    w_gk_up = sb(top, "w_gk_up_pad", [128, 256]); h_w_gk_up = H()
    k.op("dve", lambda e: e.memset(w_gk_up, 0.0), w=[h_w_gk_up])
    k.dma("sp", w_gk_up[0:16, :], w_gk_up_d, w=[h_w_gk_up])
    b_gkT, h_b_gkT = const("b_gkT", b_gkT_d, [128, 2])
    b_gk_bc, h_b_gk_bc = const("b_gk_bc", b_gk_bc_d, [128, 256])
    ggla, h_ggla = const("ggla", ggla_d, [128, 1])
    gq, h_gq = const("gq", gq_d, [128, 1])
    gkn, h_gkn = const("gkn", gkn_d, [128, 1])
    lam4, h_lam4 = const("lam4", lam4_d, [128, 4, 64])
    gsub, h_gsub = const("gsub", gsub_d, [128, 128])
    b_router, h_b_router = const("b_router", b_router_d, [128, E])
    cF0, h_cF0 = const("cF", cF_d, [128, 8])
    cP0, h_cP0 = const("cP", cP_d, [128, 8])
    SHIFT = 8.0
    cF = sb(top, "cF_sh", [128, 8]); h_cF = H()
    cP = sb(top, "cP_sh", [128, 8]); h_cP = H()
    k.op("dve", lambda e: e.tensor_scalar(cF, cF0, -SHIFT, None, ALU.add), r=[h_cF0], w=[h_cF])
    k.op("dve", lambda e: e.tensor_scalar(cP, cP0, -SHIFT, None, ALU.add), r=[h_cP0], w=[h_cP])
    flag, h_flag = const("flag", flag_d, [128, 1])
    ustrict, h_ustrict = const("ustrict", ustrict_d, [128, 128])
    ones_f, h_ones_f = const("ones_f", ones_f_d, [128, 128])
    iota64, h_iota64 = const("iota64", iota64_d, [128, NBLK])
    lane_id, h_pidx = const("lane_id", pidx_d, [128, 1])

    a1T = sb(top, "a1T", [128, 8]); h_a1T = H()
    sh1T = sb(top, "sh1T", [128, 8]); h_sh1T = H()
    gt2_bc = sb(top, "gt2_bc", [128, D]); h_gt2 = H()
    wts = sb(top, "wts", [128, NT_OWN, E]); h_wts = H()
    lg_all = sb(top, "lg_all", [128, NT_OWN, E]); h_lg_all = H()
    top8_all = sb(top, "top8_all", [128, NT_OWN, 8]); h_top8_all = H()
    msk_all = sb(top, "msk_all", [128, NT_OWN, E]); h_msk_all = H()
    idxf_all = sb(top, "idxf_all", [128, NT_OWN, 4]); h_idxf_all = H()
    wk_all = sb(top, "wk_all", [128, NT_OWN, 4]); h_wk_all = H()
    idx_all = sb(top, "idx_all", [128, NT_OWN * 4], I32); h_idx_all = H()
    blk_idx = sb(top, "blk_idx", [128, 3 * NBLK], I32); h_blk_idx = H()
    modbc_d = dscratch("modbc_scr", [3, 128, D], F32)
    neg_bgkT = sb(top, "neg_bgkT", [128, 2]); h_neg_bgkT = H()
    gq8 = sb(top, "gq8", [128, 1]); h_gq8 = H()
    neglam = sb(top, "neglam", [128, 1]); h_neglam = H()
    gsub8 = sb(top, "gsub8", [128, 128]); h_gsub8 = H()
    eps_col = sb(top, "eps_col", [128, 1]); h_eps = H()
    one_col = sb(top, "one_col", [128, 1]); h_one = H()
    k.op("dve", lambda e: e.memset(eps_col, EPS), w=[h_eps])
    k.op("dve", lambda e: e.memset(one_col, 1.0), w=[h_one])

    def act(out, in_, func, r, w, bias=None, scale=None):
        kw = {}
        if bias is not None:
            kw["bias"] = bias
        if scale is not None:
            kw["scale"] = scale
        return k.op("act", lambda e: e.activation(out=out, in_=in_, func=func, **kw), r=r, w=w)

    def mm(out, lhsT, rhs, start, stop, r, bank, skip=False):
        kw = {"skip_group_check": True} if skip else {}
        return k.op("pe", lambda e: e.matmul(out, lhsT, rhs, start=start, stop=stop, **kw), r=r, w=[PH[bank]])

    def rstd_from_ss(out_col, ss_col, scale, r_h, w_h, tmp, h_tmp):
        act(tmp, ss_col, AF.Ln, r=[r_h, h_eps], w=[h_tmp], bias=eps_col[0:tmp.shape[0], :], scale=scale)
        act(out_col, tmp, AF.Exp, r=[h_tmp], w=[w_h], scale=-0.5)

    with ExitStack() as es:
        cond = sb(es, "cond", [128, 8]); h_cond = H()
        act(cond, cT, AF.Silu, r=[h_cT], w=[h_cond])
        wa = [sb(es, f"wa{i}", [128, 8, D]) for i in range(2)]
        h_wa = [H(), H()]
        modT = sb(es, "modT", [128, 2, 8]); h_modT = H()
        rows = sb(es, "rows", [1, 4, D]); h_rows = H()
        brow = sb(es, "brow", [1, 4, D]); h_brow = H()
        g2bc = sb(es, "g2bc", [128, D]); h_g2bc = H()
        gt1_bc = sb(es, "gt1_bc0", [128, D]); h_gt1 = H()
        a2_bc = sb(es, "a2_bc0", [128, D]); h_a2bc = H()
        sh2_bc = sb(es, "sh2_bc0", [128, D]); h_sh2bc = H()
        k.dma("sp", g2bc, g2_bc_d, w=[h_g2bc])
        row_slot = {2: 0, 5: 1, 3: 2, 4: 3}
        for s6_, ri_ in row_slot.items():
            k.dma("sp", brow[:, ri_, :], b_ada_row_d[:, s6_ * D:(s6_ + 1) * D], w=[h_brow])
        fm_slot = {0: 0, 1: 1}
        for s6 in range(6):
            wb, hb = wa[s6 % 2], h_wa[s6 % 2]
            k.dma("sp", wb, w_ada_d[:, s6 * D:(s6 + 1) * D].rearrange("(kc p) n -> p kc n", p=128), w=[hb])
            if s6 in fm_slot:
                sl = fm_slot[s6]
                for nt in range(8):
                    for kc in range(8):
                        mm(PS[0][:, nt:nt + 1], wb[:, kc, nt * 128:(nt + 1) * 128], cond[:, kc:kc + 1],
                           start=(kc == 0), stop=(kc == 7), r=[hb, h_cond], bank=0, skip=True)
                k.op("dve", lambda e: e.tensor_tensor(out=modT[:, sl, :], in0=PS[0][:, 0:8],
                                                      in1=b_adaT[:, s6 * 8:(s6 + 1) * 8], op=ALU.add),
                     r=[h_b_adaT], w=[PH[0], h_modT])
            else:
                ri = row_slot[s6]
                for half in range(2):
                    for kc in range(8):
                        mm(PS[1][0:1, :], cond[:, kc:kc + 1], wb[:, kc, half * 512:(half + 1) * 512],
                           start=(kc == 0), stop=(kc == 7), r=[hb, h_cond], bank=1)
                    k.op("dve", lambda e: e.tensor_tensor(out=rows[:, ri, half * 512:(half + 1) * 512],
                                                          in0=PS[1][0:1, :],
                                                          in1=brow[:, ri, half * 512:(half + 1) * 512], op=ALU.add),
                         r=[h_brow], w=[PH[1], h_rows])
                dst, hd_ = {2: (gt1_bc, h_gt1), 5: (gt2_bc, h_gt2), 3: (sh2_bc, h_sh2bc), 4: (a2_bc, h_a2bc)}[s6]
                for half in range(2):
                    mm(PS[2], ones_row, rows[:, ri, half * 512:(half + 1) * 512], start=True, stop=True,
                       r=[h_ones_row, h_rows], bank=2)
                    k.op("dve", lambda e: e.tensor_copy(dst[:, half * 512:(half + 1) * 512], PS[2]),
                         w=[PH[2], hd_])
        k.op("dve", lambda e: e.scalar_tensor_tensor(out=a1T, in0=modT[:, 1, :], scalar=1.0, in1=g1T,
                                                     op0=ALU.add, op1=ALU.mult), r=[h_modT, h_g1T], w=[h_a1T])
        k.op("dve", lambda e: e.tensor_copy(sh1T, modT[:, 0, :]), r=[h_modT], w=[h_sh1T])
        k.op("dve", lambda e: e.scalar_tensor_tensor(out=a2_bc, in0=a2_bc, scalar=1.0, in1=g2bc,
                                                     op0=ALU.add, op1=ALU.mult), r=[h_a2bc, h_g2bc], w=[h_a2bc])
        k.dma("sp", modbc_d[0], gt1_bc, r=[h_gt1])
        k.dma("sp", modbc_d[1], a2_bc, r=[h_a2bc])
        k.dma("sp", modbc_d[2], sh2_bc, r=[h_sh2bc])
        k.op("dve", lambda e: e.tensor_scalar(neg_bgkT, b_gkT, -1.0, None, ALU.mult), r=[h_b_gkT], w=[h_neg_bgkT])
        k.op("dve", lambda e: e.tensor_scalar(gq8, gq, 0.125, None, ALU.mult), r=[h_gq], w=[h_gq8])
        k.op("dve", lambda e: e.tensor_scalar(gsub8, gsub, 1.0 - LAMBDA_INIT, None, ALU.mult), r=[h_gsub], w=[h_gsub8])
        lp = sb(es, "lp", [128, 2, 64]); h_lp = H()
        ls = sb(es, "ls", [128, 2]); h_ls = H()
        le = sb(es, "le", [128, 2]); h_le = H()
        k.op("dve", lambda e: e.tensor_tensor(out=lp[:, 0, :], in0=lam4[:, 0, :], in1=lam4[:, 1, :], op=ALU.mult),
             r=[h_lam4], w=[h_lp])
        k.op("dve", lambda e: e.tensor_tensor(out=lp[:, 1, :], in0=lam4[:, 2, :], in1=lam4[:, 3, :], op=ALU.mult),
             r=[h_lam4], w=[h_lp])
        k.op("dve", lambda e: e.tensor_reduce(out=ls, in_=lp, axis=AX.X, op=ALU.add), r=[h_lp], w=[h_ls])
        act(le, ls, AF.Exp, r=[h_ls], w=[h_le])
        k.op("dve", lambda e: e.scalar_tensor_tensor(out=neglam, in0=le[:, 1:2], scalar=-LAMBDA_INIT, in1=le[:, 0:1],
                                                     op0=ALU.add, op1=ALU.subtract), r=[h_le], w=[h_neglam])
        k.barrier()

    all_casts = []
    for ex_ in range(E):
        all_casts.append((wgu_bf_d[ex_ * 128:(ex_ + 1) * 128, :].rearrange("p (kc f) -> p kc f", kc=8),
                          w_gu_d[ex_].rearrange("(kc p) f -> p kc f", p=128)))
        all_casts.append((wdn_bf_d[ex_ * 128:(ex_ + 1) * 128, :].rearrange("p (kc f) -> p kc f", kc=8),
                          w_dn_d[ex_].rearrange("(kc p) f -> p kc f", p=128)))

    def issue_casts(n):
        for _ in range(min(n, len(all_casts))):
            dst_, src_ = all_casts.pop(0)
            k.dma("pool", dst_, src_)

    with ExitStack() as es:
        xb = [sb(es, f"xb{i}", [128, D]) for i in range(3)]
        h_xb = [H() for _ in range(3)]
        sq = sb(es, "sq_junk", [128, D], BF16); h_sq = H()
        ss = [sb(es, f"ss{i}", [128, 1]) for i in range(2)]; h_ss = [H(), H()]
        sd = [sb(es, f"sd{i}", [128, 1]) for i in range(2)]; h_sd = [H(), H()]
        rs = [sb(es, f"rs{i}", [128, 1]) for i in range(2)]; h_rs = [H(), H()]
        xn = [sb(es, f"xn{i}", [128, D], BF16) for i in range(2)]; h_xn = [H(), H()]
        hT = [sb(es, f"hTst{i}", [128, 8, 512], BF16) for i in range(2)]; h_hT = [H(), H()]; h_hTa = [H(), H()]
        psb = [PS[0].bitcast(BF16), PS[1].bitcast(BF16)]
        def p1_stage1(i):
            src = x_pre if i < 32 else x_own
            ti = i % 32
            b3 = i % 3
            b2 = i % 2
            k.dma("sp", xb[b3], src[ti * 128:(ti + 1) * 128, :], w=[h_xb[b3]])
            act(sq, xb[b3], AF.Square, r=[h_xb[b3]], w=[h_sq])
            k.op("dve", lambda e: e.tensor_reduce(out=ss[b2], in_=sq, axis=AX.X, op=ALU.add), r=[h_sq], w=[h_ss[b2]])
            rstd_from_ss(rs[b2], ss[b2], 1.0 / D, h_ss[b2], h_rs[b2], sd[b2], h_sd[b2])
            k.op("dve", lambda e: e.tensor_scalar(xn[b2], xb[b3], rs[b2], None, ALU.mult),
                 r=[h_xb[b3], h_rs[b2]], w=[h_xn[b2]])

        def p1_stage2(i):
            b2 = i % 2
            sbi = i // 4
            hb = (sbi % 2)
            bd, ba = 2 * b2, 2 * b2 + 1
            pvd = PS[bd].bitcast(BF16)
            pva = PS[ba].bitcast(BF16)
            for kc in range(8):
                pv_, bank_ = (pvd, bd) if kc < 4 else (pva, ba)
                k.op("pe", lambda e: e.transpose(pv_[:, (kc % 4) * 128:(kc % 4 + 1) * 128], xn[b2][:, kc * 128:(kc + 1) * 128],
                                                 ident_b), r=[h_xn[b2], h_ident_b], w=[PH[bank_]])
            for kc in range(4):
                dst = hT[hb][:, kc, (i % 4) * 128:(i % 4 + 1) * 128]
                k.op("dve", lambda e: e.tensor_scalar(dst, pvd[:, kc * 128:(kc + 1) * 128], a1T[:, kc:kc + 1],
                                                      sh1T[:, kc:kc + 1], ALU.mult, ALU.add),
                     r=[h_a1T, h_sh1T], w=[PH[bd], h_hT[hb]])
            for kc in range(4, 8):
                dst = hT[hb][:, kc, (i % 4) * 128:(i % 4 + 1) * 128]
                act(dst, pva[:, (kc - 4) * 128:(kc - 3) * 128], AF.Identity, r=[h_a1T, h_sh1T], w=[PH[ba], h_hTa[hb]],
                    bias=sh1T[:, kc:kc + 1], scale=a1T[:, kc:kc + 1])
            if i % 4 == 3:
                k.dma("pool", hT_d[:, :, sbi * 512:(sbi + 1) * 512].rearrange("k p t -> p k t"), hT[hb], r=[h_hT[hb], h_hTa[hb]])

        p1_stage1(0)
        for i in range(NT_K):
            if i + 1 < NT_K:
                p1_stage1(i + 1)
            p1_stage2(i)
            if i % 8 == 7:
                issue_casts(1)
        k.barrier()

    mix_es = ExitStack()
    mixT = sb(mix_es, "mixT", [128, 8, HALF], BF16)
    h_mixT = H()

    def load_w(es, name, col0, ncols, q="pool"):
        t = sb(es, name, [128, 8, ncols], BF16)
        h = H()
        k.dma(q, t, w_in_d[:, col0:col0 + ncols].rearrange("(kc p) n -> p kc n", p=128), w=[h])
        return t, h

    for P in range(2):
        with ExitStack() as es:
            wq, h_wq = load_w(es, "g_wq", OFF_QG + P * 128, 128)
            wk, h_wk = load_w(es, "g_wk", OFF_KG + P * 128, 128)
            wv, h_wv = load_w(es, "g_wv", OFF_VG + P * 256, 256)
            wr, h_wr = load_w(es, "g_wr", OFF_RG + P * 256, 256)
            wgk = sb(es, "g_wgk_pad", [128, 8, 128], BF16); h_wgk = H()
            k.op("dve", lambda e: e.memset(wgk, 0.0), w=[h_wgk])
            k.dma("pool", wgk[:, :, 0:16], w_in_d[:, OFF_GK:OFF_GK + 16].rearrange("(kc p) n -> p kc n", p=128), w=[h_wgk])
            hTb = [sb(es, f"g_hT{i}", [128, 8, 512], BF16) for i in range(2)]; h_hTb = [H(), H()]
            gkT2 = [sb(es, f"g_gkT{i}", [128, 512]) for i in range(2)]; h_gkT2 = [H(), H()]
            e1 = sb(es, "g_e1", [128, 512]); h_e1 = H()
            spf = sb(es, "g_spf", [128, 512]); h_spf = H()
            cum = sb(es, "g_cum", [128, 512]); h_cum = H()
            Eq2 = [sb(es, f"g_Eq{i}", [128, 512]) for i in range(2)]; h_Eq2 = [H(), H()]
            Ek = sb(es, "g_Ek", [128, 512]); h_Ek = H()
            keT2 = [sb(es, f"g_keT{i}", [128, 512], BF16) for i in range(2)]; h_keT2 = [H(), H()]
            qeTz2 = [[sb(es, f"g_qeTz{j}_{i}", [128, 512], BF16) for i in range(2)] for j in range(2)]; h_qeT2 = [H(), H()]
            for j_ in range(2):
                k.op("dve", lambda e: e.memset(qeTz2[j_][0][64:128, :], 0.0), w=[h_qeT2[j_]])
                k.op("dve", lambda e: e.memset(qeTz2[j_][1][0:64, :], 0.0), w=[h_qeT2[j_]])
            rsT2 = [sb(es, f"g_rsT{i}", [128, 2, 512]) for i in range(2)]; h_rsT2 = [H(), H()]
            vtm2 = [sb(es, f"g_vtm{i}", [128, 256], BF16) for i in range(2)]; h_vtm2 = [H(), H()]
            ks2 = [sb(es, f"g_ks{i}", [128, 128], BF16) for i in range(2)]; h_ks2 = [H(), H()]
            AT2 = [sb(es, f"g_AT{i}", [128, 128], BF16) for i in range(2)]; h_AT2 = [H(), H()]
            sqo2 = [sb(es, f"g_sqo{i}", [128, 128], BF16) for i in range(2)]; h_sqo2 = [H(), H()]
            sdo2 = [sb(es, f"g_sdo{i}", [128, 128]) for i in range(2)]; h_sdo2 = [H(), H()]
            rso2 = [sb(es, f"g_rso{i}", [128, 128]) for i in range(2)]; h_rso2 = [H(), H()]
            t12 = [sb(es, f"g_t1{i}", [128, 128]) for i in range(2)]; h_t12 = [H(), H()]
            gtm = sb(es, "g_gtm", [128, 128]); h_gtm = H()
            e1t = sb(es, "g_e1t", [128, 128]); h_e1t = H()
            spt = sb(es, "g_spt", [128, 128]); h_spt = H()
            Es = sb(es, "g_Es", [128, 128]); h_Es = H()
            ks = sb(es, "g_ks", [128, 128], BF16); h_ks = H()
            AT = sb(es, "g_AT", [128, 128], BF16); h_AT = H()
            Sf = sb(es, "g_Sf", [128, 128]); h_Sf = H()
            Sb = sb(es, "g_Sb", [128, 128], BF16); h_Sb = H()
            sqo = sb(es, "g_sqo", [128, 128], BF16); h_sqo = H()
            sdo = sb(es, "g_sdo", [128, 128]); h_sdo = H()
            rso = sb(es, "g_rso", [128, 128]); h_rso = H()
            t1 = sb(es, "g_t1", [128, 128]); h_t1 = H()
            k.op("dve", lambda e: e.memset(Sf, 0.0), w=[h_Sf])
            k.op("dve", lambda e: e.memset(Sb, 0.0), w=[h_Sb])
            def fm(sbi):
                own = sbi >= 8
                fb = sbi % 2
                hTc, h_hTc = hTb[fb], h_hTb[fb]
                k.dma("sp", hTc, hT_d[:, :, sbi * 512:(sbi + 1) * 512].rearrange("k p t -> p k t"), w=[h_hTc])
                for kc in range(8):
                    mm(PS[0], wgk[:, kc, :], hTc[:, kc, :], start=(kc == 0), stop=(kc == 7), r=[h_wgk, h_hTc], bank=0)
                k.op("dve", lambda e: e.tensor_copy(gkT2[fb], PS[0]), w=[PH[0], h_gkT2[fb]])
                mm(PS[1], w_gk_up[:, P * 128:(P + 1) * 128], gkT2[fb], start=True, stop=True, r=[h_w_gk_up, h_gkT2[fb]], bank=1)
                act(e1, PS[1], AF.Exp, r=[h_neg_bgkT], w=[PH[1], h_e1], bias=neg_bgkT[:, P:P + 1], scale=-1.0)
                act(spf, e1, AF.Ln, r=[h_e1, h_one], w=[h_spf], bias=one_col)
                k.op("dve", lambda e: e.tensor_tensor_scan(out=cum, data0=keep, data1=spf, initial=0.0,
                                                           op0=ALU.mult, op1=ALU.add), r=[h_keep, h_spf], w=[h_cum])
                act(Eq2[fb], cum, AF.Exp, r=[h_cum], w=[h_Eq2[fb]], scale=-1.0 / 16.0)
                act(Ek, cum, AF.Exp, r=[h_cum], w=[h_Ek], scale=1.0 / 16.0)
                for kc in range(8):
                    mm(PS[2], wk[:, kc, :], hTc[:, kc, :], start=(kc == 0), stop=(kc == 7), r=[h_wk, h_hTc], bank=2)
                k.op("dve", lambda e: e.tensor_tensor(out=keT2[fb], in0=PS[2], in1=Ek, op=ALU.mult),
                     r=[h_Ek], w=[PH[2], h_keT2[fb]])
                if own:
                    for kc in range(8):
                        mm(PS[0], wq[:, kc, :], hTc[:, kc, :], start=(kc == 0), stop=(kc == 7), r=[h_wq, h_hTc], bank=0)
                    for hq in range(2):
                        pr = slice(hq * 64, (hq + 1) * 64)
                        k.op("dve", lambda e: e.scalar_tensor_tensor(out=qeTz2[fb][hq][pr, :], in0=PS[0][pr, :], scalar=0.125,
                                                                     in1=Eq2[fb][pr, :], op0=ALU.mult, op1=ALU.mult),
                             r=[h_Eq2[fb]], w=[PH[0], h_qeT2[fb]])
                    for hd in range(2):
                        for kc in range(8):
                            mm(PS[1 + hd], wr[:, kc, hd * 128:(hd + 1) * 128], hTc[:, kc, :], start=(kc == 0), stop=(kc == 7),
                               r=[h_wr, h_hTc], bank=1 + hd)
                        act(rsT2[fb][:, hd, :], PS[1 + hd], AF.Silu, r=[], w=[PH[1 + hd], h_rsT2[fb]])

            fm(0)
            for sbi in range(16):
                issue_casts(1)
                own = sbi >= 8
                fb = sbi % 2
                hTc, h_hTc = hTb[fb], h_hTb[fb]
                gkT, h_gkT = gkT2[fb], h_gkT2[fb]
                Eq, h_Eq = Eq2[fb], h_Eq2[fb]
                keT, h_keT = keT2[fb], h_keT2[fb]
                qeTz, h_qeT = qeTz2[fb], h_qeT2[fb]
                rsT, h_rsT = rsT2[fb], h_rsT2[fb]
                if sbi == 8:
                    k.op("dve", lambda e: e.tensor_scalar(Sf, Sf, flag, None, ALU.mult), r=[h_flag, h_Sf], w=[h_Sf])
                    act(Sb, Sf, AF.Copy, r=[h_Sf], w=[h_Sb])
                BB = (7, 3)

                def stage_a(t):
                    tok = slice(t * 128, (t + 1) * 128)
                    ab = t % 2
                    for kc in range(8):
                        mm(PS[4][:, 0:128], hTc[:, kc, tok], wk[:, kc, :], start=(kc == 0), stop=(kc == 7),
                           r=[h_wk, h_hTc], bank=4)
                    for kc in range(8):
                        mm(PS[5][:, 0:256], hTc[:, kc, tok], wv[:, kc, :], start=(kc == 0), stop=(kc == 7),
                           r=[h_wv, h_hTc], bank=5)
                    act(vtm2[ab], PS[5][:, 0:256], AF.Copy, r=[], w=[PH[5], h_vtm2[ab]])
                    mm(PS[6][:, 0:128], gkT[:, tok], w_gk_up[:, P * 128:(P + 1) * 128], start=True, stop=True,
                       r=[h_gkT, h_w_gk_up], bank=6)
                    k.op("dve", lambda e: e.tensor_tensor(out=gtm, in0=PS[6][:, 0:128],
                                                          in1=b_gk_bc[:, P * 128:(P + 1) * 128], op=ALU.add),
                         r=[h_b_gk_bc], w=[PH[6], h_gtm])
                    act(e1t, gtm, AF.Exp, r=[h_gtm], w=[h_e1t], scale=-1.0)
                    act(spt, e1t, AF.Ln, r=[h_e1t, h_one], w=[h_spt], bias=one_col)
                    mm(PS[6][:, 128:256], Lmat, spt, start=True, stop=True, r=[h_Lmat, h_spt], bank=6)
                    act(Es, PS[6][:, 128:256], AF.Exp, r=[], w=[PH[6], h_Es])
                    k.op("dve", lambda e: e.tensor_tensor(out=ks2[ab], in0=PS[4][:, 0:128], in1=Es, op=ALU.mult),
                         r=[h_Es], w=[PH[4], h_ks2[ab]])

                def stage_b(t):
                    tok = slice(t * 128, (t + 1) * 128)
                    ab = t % 2
                    vt, h_vt, kst, h_kst = vtm2[ab], h_vtm2[ab], ks2[ab], h_ks2[ab]
                    if own:
                        gt = (sbi - 8) * 512 + t * 128
                        R = [slice(0, 64), slice(64, 128)]
                        for hd in range(2):
                            mm(PS[BB[hd]][:, 0:128], keT[:, tok], qeTz[hd][:, tok], start=True, stop=True,
                               r=[h_keT, h_qeT], bank=BB[hd])
                        for hd in range(2):
                            k.op("dve", lambda e: e.tensor_tensor(out=AT2[hd], in0=PS[BB[hd]][:, 0:128], in1=maskT, op=ALU.mult),
                                 r=[h_maskT], w=[PH[BB[hd]], h_AT2[hd]])
                        for hd in range(2):
                            mm(PS[BB[hd]][:, 128:256], vt[:, hd * 128:(hd + 1) * 128], AT2[hd], start=True, stop=False,
                               r=[h_vt, h_AT2[hd]], bank=BB[hd], skip=True)
                            mm(PS[BB[hd]][:, 128:256], Sb, qeTz[hd][:, tok], start=False, stop=True,
                               r=[h_Sb, h_qeT], bank=BB[hd], skip=True)
                        for hd in range(2):
                            act(sqo2[hd], PS[BB[hd]][:, 128:256], AF.Square, r=[], w=[PH[BB[hd]], h_sqo2[hd]])
                        for hd in range(2):
                            mm(PS[BB[hd]][:, 256:384], ones128, sqo2[hd], start=True, stop=True, r=[h_ones128, h_sqo2[hd]],
                               bank=BB[hd], skip=True)
                        for hd in range(2):
                            act(sdo2[hd], PS[BB[hd]][:, 256:384], AF.Ln, r=[h_eps], w=[PH[BB[hd]], h_sdo2[hd]], bias=eps_col)
                        for hd in range(2):
                            act(rso2[hd], sdo2[hd], AF.Exp, r=[h_sdo2[hd]], w=[h_rso2[hd]], scale=-0.5)
                        for hd in range(2):
                            k.op("dve", lambda e: e.scalar_tensor_tensor(out=t12[hd], in0=PS[BB[hd]][:, 128:256], scalar=ggla,
                                                                         in1=rso2[hd], op0=ALU.mult, op1=ALU.mult),
                                 r=[h_ggla, h_rso2[hd]], w=[PH[BB[hd]], h_t12[hd]])
                        for hd in range(2):
                            k.op("dve", lambda e: e.tensor_tensor(out=mixT[:, 2 * P + hd, gt:gt + 128], in0=t12[hd],
                                                                  in1=rsT[:, hd, tok], op=ALU.mult),
                                 r=[h_t12[hd], h_rsT], w=[h_mixT])
                    mm(PS[6][:, 256:512], kst, vt, start=True, stop=True, r=[h_kst, h_vt], bank=6, skip=True)
                    for hd in range(2):
                        pr = slice(hd * 64, (hd + 1) * 64)
                        dcol = Eq[pr, t * 128 + 127:t * 128 + 128]
                        k.op("dve", lambda e: e.scalar_tensor_tensor(out=Sf[pr, :], in0=Sf[pr, :], scalar=dcol,
                                                                     in1=PS[6][pr, 256 + hd * 128:256 + (hd + 1) * 128],
                                                                     op0=ALU.mult, op1=ALU.add), r=[h_Eq, h_Sf], w=[PH[6], h_Sf])
                    act(Sb, Sf, AF.Copy, r=[h_Sf], w=[h_Sb])

                stage_a(0)
                for t in range(4):
                    if t + 1 < 4:
                        stage_a(t + 1)
                    if t == 2 and sbi + 1 < 16:
                        fm(sbi + 1)
                    stage_b(t)
            k.barrier()

    with ExitStack() as es:
        KT = sb(es, "a_KT", [128, S], BF16); h_KT = H()
        QTz = [sb(es, f"a_QT{i}", [128, HALF], BF16) for i in range(2)]; h_QT = H()
        k.op("dve", lambda e: e.memset(QTz[0][64:128, :], 0.0), w=[h_QT])
        k.op("dve", lambda e: e.memset(QTz[1][0:64, :], 0.0), w=[h_QT])
        Vg = sb(es, "a_V", [128, NT_K, 130], BF16); h_V = H()
        k.op("dve", lambda e: e.memset(Vg, 1.0), w=[h_V])
        hTb = [sb(es, f"a_hT{i}", [128, 8, 512], BF16) for i in range(2)]; h_hTb = [H(), H()]
        bt = sb(es, "a_bias", [128, 2, 6, 512]); h_bt = H()
        sqk2 = [sb(es, f"a_sqk{i}", [128, 512], BF16) for i in range(2)]; h_sqk2 = [H(), H()]
        sdk2 = [sb(es, f"a_sdk{i}", [128, 512]) for i in range(2)]; h_sdk2 = [H(), H()]
        PT = [sb(es, f"a_PT{i}", [128, 512], BF16) for i in range(4)]; h_PT = [H() for _ in range(4)]
        stmp0 = sb(es, "a_stmp0", [128, 512]); h_stmp0 = H()
        stmp = [stmp0, stmp0]; h_stmp = [h_stmp0, h_stmp0]
        vT_sb = sb(es, "a_vT", [128, 512], BF16); h_vT_sb = H()
        on0 = sb(es, "a_on0", [128, 4, 128]); h_on0 = H()
        od_single = sb(es, "a_od", [128, 4, 128]); h_od_single = H()
        od2 = [od_single, od_single]; h_od2 = [h_od_single, h_od_single]
        yb4 = [sb(es, f"a_yb{i}", [128, 128], BF16) for i in range(4)]; h_yb4 = [H() for _ in range(4)]
        pending = []
        rl = sb(es, "a_rl", [128, 4]); h_rl = H()
        nrl = sb(es, "a_nrl", [128, 4]); h_nrl = H()
        sqd = sb(es, "a_sqd", [128, 128]); h_sqd = H()
        ssd = sb(es, "a_ssd", [128, 1]); h_ssd = H()
        sdd = sb(es, "a_sdd", [128, 1]); h_sdd = H()
        rsd = sb(es, "a_rsd", [128, 1]); h_rsd = H()
        wqd = sb(es, "a_wq", [128, 8, 128], BF16); h_wqd = H()
        wkd = sb(es, "a_wk", [128, 8, 128], BF16); h_wkd = H()
        wvd = sb(es, "a_wv", [128, 8, 128], BF16); h_wvd = H()
        for hd in range(4):
            for wt_, hw_, off_ in ((wqd, h_wqd, OFF_QD), (wkd, h_wkd, OFF_KD), (wvd, h_wvd, OFF_VD)):
                k.dma("pool", wt_, w_in_d[:, off_ + hd * 128:off_ + (hd + 1) * 128].rearrange("(kc p) n -> p kc n", p=128),
                      w=[hw_])
            k.dma("sp", bt, bias_d[2 * hd:2 * hd + 2].rearrange("m s p q -> p m s q"), w=[h_bt])
            cast_jobs = [all_casts.pop(0) for _ in range(min(6, len(all_casts)))]
            if hd == 3:
                cast_jobs += all_casts
                all_casts = []
            units_p = []
            for sbi in range(16):
                units_p.append((sbi, 0))
                if sbi >= 8:
                    units_p.append((sbi, 1))
            loaded = set()

            def ensure_hT(sbi):
                if sbi not in loaded:
                    loaded.add(sbi)
                    hb_ = sbi % 2
                    k.dma("sp", hTb[hb_], hT_d[:, :, sbi * 512:(sbi + 1) * 512].rearrange("k p t -> p k t"), w=[h_hTb[hb_]])

            def proj_a(u):
                sbi, which = units_p[u]
                ensure_hT(sbi)
                hTc, h_hTc = hTb[sbi % 2], h_hTb[sbi % 2]
                wsel, h_wsel = (wkd, h_wkd) if which == 0 else (wqd, h_wqd)
                pa = u % 3
                for kc in range(8):
                    mm(PS[pa], wsel[:, kc, :], hTc[:, kc, :], start=(kc == 0), stop=(kc == 7), r=[h_wsel, h_hTc], bank=pa)
                act(sqk2[u % 2], PS[pa], AF.Square, r=[], w=[PH[pa], h_sqk2[u % 2]])

            def proj_b(u):
                sbi, which = units_p[u]
                gcol, h_gcol = (gkn, h_gkn) if which == 0 else (gq8, h_gq8)
                pa = u % 3
                pm = 3 if u % 2 == 0 else 6
                sq_, h_sq_ = sqk2[u % 2], h_sqk2[u % 2]
                sd_, h_sd_ = sdk2[u % 2], h_sdk2[u % 2]
                mm(PS[pm], blk64, sq_, start=True, stop=True, r=[h_blk64, h_sq_], bank=pm)
                act(sd_, PS[pm], AF.Ln, r=[h_eps], w=[PH[pm], h_sd_], bias=eps_col)
                act(sd_, sd_, AF.Exp, r=[h_sd_], w=[h_sd_], scale=-0.5)
                if which == 0:
                    dst = KT[:, sbi * 512:(sbi + 1) * 512]
                    k.op("dve", lambda e: e.scalar_tensor_tensor(out=dst, in0=PS[pa], scalar=gcol, in1=sd_,
                                                                 op0=ALU.mult, op1=ALU.mult),
                         r=[h_gcol, h_sd_], w=[PH[pa], h_KT])
                else:
                    for mq in range(2):
                        pr = slice(mq * 64, (mq + 1) * 64)
                        dst = QTz[mq][pr, (sbi - 8) * 512:(sbi - 7) * 512]
                        k.op("dve", lambda e: e.scalar_tensor_tensor(out=dst, in0=PS[pa][pr, :], scalar=gcol[pr, :],
                                                                     in1=sd_[pr, :], op0=ALU.mult, op1=ALU.mult),
                             r=[h_gcol, h_sd_], w=[PH[pa], h_QT])

            def proj_v(sbi):
                hTc, h_hTc = hTb[sbi % 2], h_hTb[sbi % 2]
                for kc in range(8):
                    mm(PS[4], wvd[:, kc, :], hTc[:, kc, :], start=(kc == 0), stop=(kc == 7), r=[h_wvd, h_hTc], bank=4)
                act(vT_sb, PS[4], AF.Copy, r=[], w=[PH[4], h_vT_sb])
                pvv = PS[5].bitcast(BF16)
                for t in range(4):
                    k.op("pe", lambda e: e.transpose(pvv[:, t * 128:(t + 1) * 128], vT_sb[:, t * 128:(t + 1) * 128], ident_b),
                         r=[h_vT_sb, h_ident_b], w=[PH[5]])
                k.op("dve", lambda e: e.tensor_copy(Vg[:, sbi * 4:sbi * 4 + 4, 0:128],
                                                    pvv[:, 0:512].rearrange("p (t e) -> p t e", t=4)), w=[PH[5], h_V])

            proj_a(0)
            for u in range(len(units_p)):
                sbi_u, which_u = units_p[u]
                last_of_sb = (u + 1 == len(units_p)) or (units_p[u + 1][0] != sbi_u)
                if last_of_sb:
                    proj_v(sbi_u)
                if u + 1 < len(units_p):
                    proj_a(u + 1)
                proj_b(u)
            k._wait("pool", [], [h_QT.w, h_V.w])
            for dst_, src_ in cast_jobs:
                k.dma("pool", dst_, src_)
            SB = (0, 1, 6)
            LOOK = 2
            pidx = 0
            stream = []
            for j_ in range(8):
                for m_ in range(2):
                    for g_ in range(32 + 4 * j_ + 4):
                        stream.append((j_, m_, g_))

            def emit_S(si):
                j_, m_, g_ = stream[si]
                sbank_ = SB[si % 3]
                mm(PS[sbank_], KT[:, g_ * 128:(g_ + 1) * 128], QTz[m_][:, j_ * 512:(j_ + 1) * 512],
                   start=True, stop=True, r=[h_KT, h_QT], bank=sbank_)

            for si in range(LOOK):
                emit_S(si)
            sbase = 0
            prev_pv = [None]
            for j in range(8):
                for m in range(2):
                    hm = 2 * hd + m
                    ob = (2, 3) if m == 0 else (4, 5)
                    nkeys = 32 + 4 * j + 4
                    base = sbase
                    sbase += nkeys
                    for g in range(nkeys):
                        if g == 12 and pending:
                            pending.pop(0)()
                        if base + g + LOOK < len(stream):
                            emit_S(base + g + LOOK)
                        sbank = SB[(base + g) % 3]
                        pb = pidx % 4
                        pidx += 1
                        tile_id = None
                        qb_min = 0
                        if g == 31 and j == 0:
                            tile_id = 5
                        elif j > 0 and g == 32 + 4 * j - 1:
                            tile_id = 4
                        elif g >= 32 + 4 * j:
                            tile_id = g - (32 + 4 * j)
                            qb_min = tile_id
                        if tile_id is None:
                            bcol, h_bcol = (cP, h_cP) if g < 32 else (cF, h_cF)
                            act(PT[pb], PS[sbank], AF.Exp, r=[h_bcol], w=[PH[sbank], h_PT[pb]],
                                bias=bcol[:, hm:hm + 1])
                        else:
                            st_, h_st = stmp[g % 2], h_stmp[g % 2]
                            k.op("dve", lambda e: e.scalar_tensor_tensor(out=st_, in0=PS[sbank], scalar=-SHIFT,
                                                                         in1=bt[:, m, tile_id, :], op0=ALU.add, op1=ALU.add),
                                 r=[h_bt], w=[PH[sbank], h_st])
                            act(PT[pb], st_, AF.Exp, r=[h_st], w=[h_PT[pb]])
                        def pv_closure(g=g, pb=pb, qb_min=qb_min, ob=ob, nkeys=nkeys):
                            for qb in range(qb_min, 4):
                                bnk = ob[0] if qb < 3 else ob[1]
                                c0 = (qb % 3) * 130
                                first_in_bank = (g == 0) and (qb == 0 or qb == 3)
                                mm(PS[bnk][:, c0:c0 + 129], PT[pb][:, qb * 128:(qb + 1) * 128], Vg[:, g, 0:129],
                                   start=first_in_bank, stop=(g == nkeys - 1), r=[h_PT[pb], h_V], bank=bnk, skip=True)

                        if prev_pv[0] is not None:
                            prev_pv[0]()
                        prev_pv[0] = pv_closure
                        if g == nkeys - 1:
                            prev_pv[0]()
                            prev_pv[0] = None
                    for qb in range(4):
                        bnk = ob[0] if qb < 3 else ob[1]
                        c0 = (qb % 3) * 130
                        k.op("dve", lambda e: e.reciprocal(rl[:, qb:qb + 1], PS[bnk][:, c0 + 128:c0 + 129]),
                             w=[PH[bnk], h_rl])
                        if m == 0:
                            k.op("dve", lambda e: e.tensor_scalar(on0[:, qb, :], PS[bnk][:, c0:c0 + 128],
                                                                  rl[:, qb:qb + 1], None, ALU.mult),
                                 r=[h_rl], w=[PH[bnk], h_on0])
                        else:
                            k.op("dve", lambda e: e.tensor_scalar(nrl[:, qb:qb + 1], rl[:, qb:qb + 1], neglam, None,
                                                                  ALU.mult), r=[h_rl, h_neglam], w=[h_nrl])
                            k.op("dve", lambda e: e.scalar_tensor_tensor(out=od2[j % 2][:, qb, :], in0=PS[bnk][:, c0:c0 + 128],
                                                                         scalar=nrl[:, qb:qb + 1], in1=on0[:, qb, :],
                                                                         op0=ALU.mult, op1=ALU.add),
                                 r=[h_nrl, h_on0], w=[PH[bnk], h_od2[j % 2]])
                    if m == 1:
                        def epilogue(j=j, hd=hd):
                            for qb in range(4):
                                k.op("dve", lambda e: e.tensor_tensor(out=sqd, in0=od2[j % 2][:, qb, :], in1=od2[j % 2][:, qb, :], op=ALU.mult),
                                     r=[h_od2[j % 2]], w=[h_sqd])
                                k.op("dve", lambda e: e.tensor_reduce(out=ssd, in_=sqd, axis=AX.X, op=ALU.add),
                                     r=[h_sqd], w=[h_ssd])
                                rstd_from_ss(rsd, ssd, 1.0 / 128.0, h_ssd, h_rsd, sdd, h_sdd)
                                k.op("dve", lambda e: e.scalar_tensor_tensor(out=yb4[qb], in0=od2[j % 2][:, qb, :], scalar=rsd, in1=gsub8,
                                                                             op0=ALU.mult, op1=ALU.mult),
                                     r=[h_od2[j % 2], h_rsd, h_gsub8], w=[h_yb4[qb]])
                            pv = PS[7].bitcast(BF16)
                            for qb in range(4):
                                k.op("pe", lambda e: e.transpose(pv[:, qb * 128:(qb + 1) * 128], yb4[qb], ident_b),
                                     r=[h_yb4[qb], h_ident_b], w=[PH[7]])
                            q0 = j * 512
                            k.op("dve", lambda e: e.tensor_copy(mixT[:, 4 + hd, q0:q0 + 512], pv[:, 0:512]),
                                 w=[PH[7], h_mixT])
                        pending.append(epilogue)
            while pending:
                pending.pop(0)()
        k.barrier()

    with ExitStack() as es:
        wo_f = sb(es, "wo_f", [128, 8, D]); h_wo_f = H()
        wo = sb(es, "wo", [128, 8, D], BF16); h_wo = H()
        wr_f = sb(es, "wr_f", [128, 8, E]); h_wr_f = H()
        gt1_bc = sb(es, "gt1_bc", [128, D]); h_gt1 = H()
        a2_bc = sb(es, "a2_bc", [128, D]); h_a2bc = H()
        sh2_bc = sb(es, "sh2_bc", [128, D]); h_sh2bc = H()
        k.dma("sp", gt1_bc, modbc_d[0], w=[h_gt1])
        k.dma("sp", a2_bc, modbc_d[1], w=[h_a2bc])
        k.dma("sp", sh2_bc, modbc_d[2], w=[h_sh2bc])
        k.dma("sp", wo_f, w_out_d.rearrange("(kc p) n -> p kc n", p=128), w=[h_wo_f])
        k.dma("sp", wr_f, w_router_d.rearrange("(kc p) n -> p kc n", p=128), w=[h_wr_f])
        for kc in range(8):
            k.op("dve", lambda e: e.tensor_tensor(out=wo[:, kc, :], in0=wo_f[:, kc, :], in1=gt1_bc, op=ALU.mult),
                 r=[h_wo_f, h_gt1], w=[h_wo])
        xb = [sb(es, f"p4_x{i}", [128, D]) for i in range(2)]; h_xb = [H(), H()]
        x1 = [sb(es, f"p4_x1{i}", [128, D]) for i in range(2)]; h_x1 = [H(), H()]
        sq = sb(es, "p4_sq", [128, D], BF16); h_sq = H()
        ss = sb(es, "p4_ss", [128, 1]); h_ss = H()
        sd = sb(es, "p4_sd", [128, 1]); h_sd = H()
        rs = sb(es, "p4_rs", [128, 1]); h_rs = H()
        h2t = sb(es, "p4_h2t", [128, D]); h_h2t = H()
        h2tb = [sb(es, f"p4_h2tb{i}", [128, D], BF16) for i in range(2)]; h_h2tb = [H(), H()]
        h2f = sb(es, "p4_h2f", [128, 8, 128]); h_h2f = H()
        nmx = sb(es, "p4_nmx", [128, 1]); h_nmx = H()
        ex = sb(es, "p4_ex", [128, E]); h_ex = H()
        exm = sb(es, "p4_exm", [128, E]); h_exm = H()
        den = sb(es, "p4_den", [128, 1]); h_den = H()
        rden = sb(es, "p4_rden", [128, 1]); h_rden = H()
        def p4_stage1(t):
            b2 = t % 2
            tok = slice(t * 128, (t + 1) * 128)
            k.dma("sp", xb[b2], x_own[tok, :], w=[h_xb[b2]])
            for half in range(2):
                for kc in range(8):
                    mm(PS[half], mixT[:, kc, tok], wo[:, kc, half * 512:(half + 1) * 512], start=(kc == 0), stop=(kc == 7),
                       r=[h_mixT, h_wo], bank=half)
                k.op("dve", lambda e: e.tensor_tensor(out=x1[b2][:, half * 512:(half + 1) * 512], in0=PS[half],
                                                      in1=xb[b2][:, half * 512:(half + 1) * 512], op=ALU.add),
                     r=[h_xb[b2]], w=[PH[half], h_x1[b2]])
            k.dma("pool", x1_d[tok, :], x1[b2], r=[h_x1[b2]])

        h2t2 = [h2t, sb(es, "p4_h2t_b", [128, D])]; h_h2t2 = [h_h2t, H()]

        def p4_stage2a(t):
            b2 = t % 2
            tok = slice(t * 128, (t + 1) * 128)
            ht, h_ht = h2t2[b2], h_h2t2[b2]
            act(sq, x1[b2], AF.Square, r=[h_x1[b2]], w=[h_sq])
            k.op("dve", lambda e: e.tensor_reduce(out=ss, in_=sq, axis=AX.X, op=ALU.add), r=[h_sq], w=[h_ss])
            rstd_from_ss(rs, ss, 1.0 / D, h_ss, h_rs, sd, h_sd)
            k.op("dve", lambda e: e.scalar_tensor_tensor(out=ht, in0=x1[b2], scalar=rs, in1=a2_bc, op0=ALU.mult,
                                                         op1=ALU.mult), r=[h_x1[b2], h_rs, h_a2bc], w=[h_ht])
            k.op("dve", lambda e: e.tensor_tensor(out=ht, in0=ht, in1=sh2_bc, op=ALU.add),
                 r=[h_ht, h_sh2bc], w=[h_ht])
            act(h2tb[b2], ht, AF.Copy, r=[h_ht], w=[h_h2tb[b2]])
            k.dma("pool", h2tm_d[tok, :], h2tb[b2], r=[h_h2tb[b2]])

        def p4_stage2b(t):
            b2 = t % 2
            ht, h_ht = h2t2[b2], h_h2t2[b2]
            for kc in range(8):
                bank = 2 + kc // 4
                k.op("pe", lambda e: e.transpose(PS[bank][:, (kc % 4) * 128:(kc % 4 + 1) * 128],
                                                 ht[:, kc * 128:(kc + 1) * 128], ident_f),
                     r=[h_ht, h_ident_f], w=[PH[bank]])
            for hf in range(2):
                act(h2f[:, hf * 4:(hf + 1) * 4, :], PS[2 + hf].rearrange("p (k t) -> p k t", k=4), AF.Copy, r=[],
                    w=[PH[2 + hf], h_h2f])
            for kc in range(8):
                mm(PS[4][:, 0:E], h2f[:, kc, :], wr_f[:, kc, :], start=(kc == 0), stop=(kc == 7), r=[h_h2f, h_wr_f], bank=4)
            lg = lg_all[:, t, :]
            top8 = top8_all[:, t, :]
            msk = msk_all[:, t, :]
            k.op("dve", lambda e: e.tensor_tensor(out=lg, in0=PS[4][:, 0:E], in1=b_router, op=ALU.add),
                 r=[h_b_router], w=[PH[4], h_lg_all])
            k.op("dve", lambda e: e.max(out=top8, in_=lg), r=[h_lg_all], w=[h_top8_all])
            k.op("dve", lambda e: e.tensor_scalar(nmx, top8[:, 0:1], -1.0, None, ALU.mult), r=[h_top8_all], w=[h_nmx])
            k.op("dve", lambda e: e.tensor_scalar(msk, lg, top8[:, 3:4], None, ALU.is_ge), r=[h_lg_all, h_top8_all],
                 w=[h_msk_all])
            act(ex, lg, AF.Exp, r=[h_lg_all, h_nmx], w=[h_ex], bias=nmx)
            k.op("dve", lambda e: e.tensor_tensor(out=exm, in0=ex, in1=msk, op=ALU.mult), r=[h_ex, h_msk_all], w=[h_exm])
            k.op("dve", lambda e: e.tensor_reduce(out=den, in_=exm, axis=AX.X, op=ALU.add), r=[h_exm], w=[h_den])
            k.op("dve", lambda e: e.reciprocal(rden, den), r=[h_den], w=[h_rden])
            k.op("dve", lambda e: e.tensor_scalar(wts[:, t, :], exm, rden, None, ALU.mult), r=[h_exm, h_rden], w=[h_wts])

        p4_stage1(0)
        p4_stage2a(0)
        for t in range(NT_OWN):
            if t + 1 < NT_OWN:
                p4_stage1(t + 1)
            p4_stage2b(t)
            if t + 1 < NT_OWN:
                p4_stage2a(t + 1)
        k.barrier()
    mix_es.close()

    with ExitStack() as es:
        cnt = sb(es, "r_cnt", [128, E]); h_cnt = H()
        nblk = sb(es, "r_nblk", [128, E]); h_nblk = H()
        ones32 = sb(es, "r_ones32", [128, E]); h_ones32 = H()
        cs = sb(es, "r_cs", [128, E]); h_cs = H()
        pstart = sb(es, "r_pstart", [128, E]); h_pstart = H()
        be = sb(es, "r_be", [128, NBLK]); h_be = H()
        bidxf = sb(es, "r_bidxf", [128, 3, NBLK]); h_bidxf = H()
        dest = sb(es, "r_dest", [128, E]); h_dest = H()
        eq = sb(es, "r_eq", [128, E]); h_eq = H()
        pr = sb(es, "r_pr", [128, E]); h_pr = H()
        h2r = [sb(es, f"r_h2r{i}", [128, D], BF16) for i in range(3)]; h_h2r = [H() for _ in range(3)]
        for t in range(NT_OWN):
            mm(PS[0][:, 0:E], ones_f, msk_all[:, t, :], start=(t == 0), stop=(t == NT_OWN - 1), r=[h_ones_f, h_msk_all], bank=0)
        k.op("dve", lambda e: e.tensor_copy(cnt, PS[0][:, 0:E]), w=[PH[0], h_cnt])
        k.op("dve", lambda e: e.memset(nblk, 0.0), w=[h_nblk])
        k.op("dve", lambda e: e.memset(ones32, 1.0), w=[h_ones32])
        for j in range(HALF // MOE_B):
            k.op("dve", lambda e: e.scalar_tensor_tensor(out=nblk, in0=cnt, scalar=float(j * MOE_B), in1=nblk,
                                                         op0=ALU.is_gt, op1=ALU.add), r=[h_cnt, h_nblk], w=[h_nblk])
        k.op("dve", lambda e: e.tensor_tensor_scan(out=cs, data0=ones32, data1=nblk, initial=0.0, op0=ALU.mult,
                                                   op1=ALU.add), r=[h_ones32, h_nblk], w=[h_cs])
        k.op("dve", lambda e: e.tensor_tensor(out=pstart, in0=cs, in1=nblk, op=ALU.subtract), r=[h_cs, h_nblk], w=[h_pstart])
        k.op("dve", lambda e: e.tensor_scalar(pstart, pstart, float(MOE_B), None, ALU.mult), r=[h_pstart], w=[h_pstart])
        k.op("dve", lambda e: e.memset(be, 0.0), w=[h_be])
        for ex_ in range(E):
            k.op("dve", lambda e: e.scalar_tensor_tensor(out=be, in0=iota64, scalar=cs[:, ex_:ex_ + 1], in1=be,
                                                         op0=ALU.is_ge, op1=ALU.add), r=[h_iota64, h_cs, h_be], w=[h_be])
        k.op("dve", lambda e: e.tensor_scalar(bidxf[:, 0, :], be, 1024.0, lane_id, ALU.mult, ALU.add), r=[h_be, h_pidx], w=[h_bidxf])
        k.op("dve", lambda e: e.tensor_scalar(bidxf[:, 1, :], be, 128.0, lane_id, ALU.mult, ALU.add), r=[h_be, h_pidx], w=[h_bidxf])
        k.op("dve", lambda e: e.tensor_copy(bidxf[:, 2, :], be), r=[h_be], w=[h_bidxf])
        k.op("dve", lambda e: e.tensor_copy(blk_idx, bidxf.rearrange("p a b -> p (a b)")), r=[h_bidxf], w=[h_blk_idx])
        dest_all = sb(es, "r_dest_all", [128, NT_OWN, E]); h_dest_all = H()
        eq_all = sb(es, "r_eq_all", [128, NT_OWN * 4, E]); h_eq_all = H()
        pr_all = sb(es, "r_pr_all", [128, NT_OWN * 4, E]); h_pr_all = H()
        base_all = sb(es, "r_base_all", [128, NT_OWN, E]); h_base_all = H()
        for t in range(NT_OWN):
            mm(PS[1 + t // 16][:, (t % 16) * E:(t % 16 + 1) * E], ustrict, msk_all[:, t, :], start=(t % 16 == 0), stop=True,
               r=[h_ustrict, h_msk_all], bank=1 + t // 16, skip=True)
        for t in range(NT_OWN):
            mm(PS[3 + t // 16][:, (t % 16) * E:(t % 16 + 1) * E], ones_f, msk_all[:, t, :], start=(t % 16 == 0), stop=True,
               r=[h_ones_f, h_msk_all], bank=3 + t // 16, skip=True)
        k.op("dve", lambda e: e.tensor_copy(base_all[:, 0, :], pstart), r=[h_pstart], w=[h_base_all])
        for t in range(1, NT_OWN):
            tp = t - 1
            k.op("dve", lambda e: e.tensor_tensor(out=base_all[:, t, :], in0=PS[3 + tp // 16][:, (tp % 16) * E:(tp % 16 + 1) * E],
                                                  in1=base_all[:, t - 1, :], op=ALU.add), r=[h_base_all],
                 w=[PH[3 + tp // 16], h_base_all])
        for hf in range(2):
            k.op("dve", lambda e: e.tensor_tensor(out=dest_all[:, hf * 16:(hf + 1) * 16, :],
                                                  in0=PS[1 + hf].rearrange("p (t e) -> p t e", e=E),
                                                  in1=base_all[:, hf * 16:(hf + 1) * 16, :], op=ALU.add),
                 r=[h_base_all], w=[PH[1 + hf], h_dest_all])
        for t in range(NT_OWN):
            for kk in range(4):
                k.op("dve", lambda e: e.tensor_scalar(eq_all[:, 4 * t + kk, :], lg_all[:, t, :], top8_all[:, t, kk:kk + 1], None,
                                                      ALU.is_equal), r=[h_lg_all, h_top8_all], w=[h_eq_all])
        for t in range(NT_OWN):
            for kk in range(4):
                k.op("dve", lambda e: e.tensor_tensor(out=pr_all[:, 4 * t + kk, :], in0=eq_all[:, 4 * t + kk, :],
                                                      in1=dest_all[:, t, :], op=ALU.mult), r=[h_eq_all, h_dest_all], w=[h_pr_all])
        k.op("dve", lambda e: e.tensor_reduce(out=idxf_all.rearrange("p a b -> p (a b)"), in_=pr_all, axis=AX.X, op=ALU.add),
             r=[h_pr_all], w=[h_idxf_all])
        for t in range(NT_OWN):
            for kk in range(4):
                k.op("dve", lambda e: e.tensor_tensor(out=pr_all[:, 4 * t + kk, :], in0=eq_all[:, 4 * t + kk, :],
                                                      in1=wts[:, t, :], op=ALU.mult), r=[h_eq_all, h_wts, h_idxf_all], w=[h_pr_all])
        k.op("dve", lambda e: e.tensor_reduce(out=wk_all.rearrange("p a b -> p (a b)"), in_=pr_all, axis=AX.X, op=ALU.add),
             r=[h_pr_all], w=[h_wk_all])
        k.op("dve", lambda e: e.tensor_copy(idx_all, idxf_all.rearrange("p a b -> p (a b)")), r=[h_idxf_all], w=[h_idx_all])
        if DEBUG:
            k.dma("sp", dbg["cnt"], cnt, r=[h_cnt])
            k.dma("sp", dbg["cs"], cs, r=[h_cs])
            k.dma("sp", dbg["pstart"], pstart, r=[h_pstart])
            k.dma("sp", dbg["be"], be, r=[h_be])
            k.dma("sp", dbg["idxf"], idxf_all.rearrange("p a b -> p (a b)"), r=[h_idxf_all])
            k.dma("sp", dbg["wk"], wk_all.rearrange("p a b -> p (a b)"), r=[h_wk_all])
            k.dma("sp", dbg["lg"], lg_all.rearrange("p a b -> p (a b)"), r=[h_lg_all])
            k.dma("sp", dbg["wts"], wts.rearrange("p a b -> p (a b)"), r=[h_wts])
            k.dma("sp", dbg["msk"], msk_all.rearrange("p a b -> p (a b)"), r=[h_msk_all])
        h_xs = H()
        reg_bounds = nc.gpsimd.to_reg(PSLOTS - 1)
        reg_bw = nc.gpsimd.to_reg(E * D - 1)
        reg_bb = nc.gpsimd.to_reg(E * 128 - 1)
        reg_be = nc.gpsimd.to_reg(E - 1)
        for t in range(NT_OWN):
            b3 = t % 3
            k.dma("sp", h2r[b3], h2tm_d[t * 128:(t + 1) * 128, :], w=[h_h2r[b3]])
            for kk in range(4):
                k._wait("pool", [], [h_h2r[b3].w, h_idx_all.w])
                d = k.dq["pool"]
                i = d["rr"]; d["rr"] = (i + 1) % NQ
                if d["cnt"][i] > 0:
                    k._wait("pool", [(d["sems"][i], 16 * d["cnt"][i])])
                inst = nc.gpsimd.indirect_dma_start(
                    out=xs_d, out_offset=bass.IndirectOffsetOnAxis(ap=idx_all[:, t * 4 + kk:t * 4 + kk + 1], axis=0),
                    in_=h2r[b3], in_offset=None, bounds_check=reg_bounds, oob_is_err=False)
                d["cnt"][i] += 1
                inst.then_inc(k.sems[d["sems"][i]], 16)
                ev = (d["sems"][i], 16 * d["cnt"][i])
                k._mark(ev, [h_h2r[b3], h_idx_all], [h_xs])
        k.barrier()

    def igather(out, src, idx_ap, r, w, element_offset=0, bounds=None):
        d = k.dq["pool"]
        i = d["rr"]; d["rr"] = (i + 1) % NQ
        evs = k._deps(r, w, "pool")
        if d["cnt"][i] > 0:
            evs.append((d["sems"][i], 16 * d["cnt"][i]))
        k._wait("pool", evs, k._raw(r))
        kw = {}
        if bounds is not None:
            kw = {"bounds_check": bounds, "oob_is_err": False}
        inst = nc.gpsimd.indirect_dma_start(out=out, out_offset=None, in_=src,
                                            in_offset=bass.IndirectOffsetOnAxis(ap=idx_ap, axis=0),
                                            element_offset=element_offset, **kw)
        d["cnt"][i] += 1
        inst.then_inc(k.sems[d["sems"][i]], 16)
        ev = (d["sems"][i], 16 * d["cnt"][i])
        k._mark(ev, r, w)
        return ev

    w_gu_rows = wgu_bf_d
    w_dn_rows = wdn_bf_d

    NSUB = MOE_B // 128
    with ExitStack() as es:
        wgu = [sb(es, f"m_wgu{i}", [128, 8, 2 * D], BF16) for i in range(2)]; h_wgu = [[H() for _ in range(8)] for _ in range(2)]
        wdn = [sb(es, f"m_wdn{i}", [128, 8, D], BF16) for i in range(2)]; h_wdn = [[H() for _ in range(8)] for _ in range(2)]
        bgu = [sb(es, f"m_bgu{i}", [128, 16]) for i in range(2)]; h_bgu = [H(), H()]
        bgu1 = [sb(es, f"m_bgu1{i}", [128, 8]) for i in range(2)]; h_bgu1 = [H(), H()]
        bdn = [sb(es, f"m_bdn{i}", [128, D]) for i in range(2)]; h_bdn = [H(), H()]
        xsb = sb(es, "m_xs", [128, NSUB, D], BF16); h_xsb = H()
        xT = [sb(es, f"m_xT{i}", [128, 8, MOE_B], BF16) for i in range(2)]; h_xT = [H(), H()]; h_xTd = [H(), H()]
        yT = [sb(es, f"m_yT{i}", [128, 8, MOE_B], BF16) for i in range(2)]; h_yT = [H(), H()]
        gate = [sb(es, f"m_gate{i}", [128, MOE_B]) for i in range(2)]; h_gate = [H(), H()]
        sig = [sb(es, f"m_sig{i}", [128, MOE_B]) for i in range(2)]; h_sig = [H(), H()]
        upc = [sb(es, f"m_upc{i}", [128, MOE_B]) for i in range(2)]; h_upc = [H(), H()]
        gs = [sb(es, f"m_gs{i}", [128, MOE_B]) for i in range(2)]; h_gs = [H(), H()]
        ysb = [sb(es, f"m_ys{i}", [128, D]) for i in range(2)]; h_ysb = [H(), H()]
        h_ys = H()

        def load_block_weights(bi):
            sl = bi % 2
            ib = blk_idx[:, NBLK + bi:NBLK + bi + 1]
            igather(wgu[sl].rearrange("p k f -> p (k f)"), w_gu_rows, ib, r=[h_blk_idx], w=h_wgu[sl], bounds=reg_bb)
            igather(wdn[sl].rearrange("p k f -> p (k f)"), w_dn_rows, ib, r=[h_blk_idx], w=h_wdn[sl], bounds=reg_bb)
            igather(bgu[sl], b_gu_rows_d, blk_idx[:, NBLK + bi:NBLK + bi + 1], r=[h_blk_idx], w=[h_bgu[sl]], bounds=reg_bb)
            igather(bdn[sl], b_dn_d, blk_idx[:, 2 * NBLK + bi:2 * NBLK + bi + 1], r=[h_blk_idx], w=[h_bdn[sl]], bounds=reg_be)

        xsb2 = [xsb, sb(es, "m_xs2", [128, NSUB, D], BF16)]; h_xsb2 = [h_xsb, H()]

        def load_xs(bi):
            sl = bi % 2
            k.dma("pool", xsb2[sl], xs_d[bi * MOE_B:(bi + 1) * MOE_B, :].rearrange("(s p) d -> p s d", p=128), w=[h_xsb2[sl]])

        def transposes(bi):
            sl = bi % 2
            for s4 in range(NSUB):
                bank = 4 + s4 % 2
                pv = PS[bank].bitcast(BF16)
                for kc in range(8):
                    k.op("pe", lambda e: e.transpose(pv[:, kc * 128:(kc + 1) * 128], xsb2[sl][:, s4, kc * 128:(kc + 1) * 128], ident_b),
                         r=[h_xsb2[sl], h_ident_b], w=[PH[bank]])
                if s4 % 2 == 0:
                    act(xT[sl][:, :, s4 * 128:(s4 + 1) * 128], pv.rearrange("p (k t) -> p k t", k=8), AF.Copy, r=[],
                        w=[PH[bank], h_xT[sl]])
                else:
                    k.op("dve", lambda e: e.tensor_copy(xT[sl][:, :, s4 * 128:(s4 + 1) * 128], pv.rearrange("p (k t) -> p k t", k=8)),
                         w=[PH[bank], h_xTd[sl]])

        load_block_weights(0)
        load_xs(0)
        transposes(0)
        it = 0
        ysi = 0
        for bi in range(NBLK):
            sl = bi % 2
            if bi + 1 < NBLK:
                load_block_weights(bi + 1)
                load_xs(bi + 1)
            k.op("dve", lambda e: e.tensor_scalar(bgu1[sl], bgu[sl][:, 8:16], 1.0, None, ALU.add), r=[h_bgu[sl]], w=[h_bgu1[sl]])
            for ft in range(8):
                i2 = it % 2
                it += 1
                gb, ub = (0, 1) if i2 == 0 else (2, 3)
                for kc in range(8):
                    mm(PS[gb][:, 0:MOE_B], wgu[sl][:, kc, ft * 128:(ft + 1) * 128], xT[sl][:, kc, :], start=(kc == 0), stop=(kc == 7),
                       r=[h_wgu[sl][kc], h_xT[sl], h_xTd[sl]], bank=gb)
                for kc in range(8):
                    mm(PS[ub][:, 0:MOE_B], wgu[sl][:, kc, D + ft * 128:D + (ft + 1) * 128], xT[sl][:, kc, :], start=(kc == 0),
                       stop=(kc == 7), r=[h_wgu[sl][kc], h_xT[sl], h_xTd[sl]], bank=ub)
                k.op("dve", lambda e: e.tensor_scalar(gate[i2], PS[gb][:, 0:MOE_B], bgu[sl][:, ft:ft + 1], 7.0, ALU.add, ALU.min),
                     r=[h_bgu[sl]], w=[PH[gb], h_gate[i2]])
                act(sig[i2], gate[i2], AF.Sigmoid, r=[h_gate[i2]], w=[h_sig[i2]], scale=1.702)
                k.op("dve", lambda e: e.tensor_scalar(upc[i2], PS[ub][:, 0:MOE_B], bgu1[sl][:, ft:ft + 1], 8.0, ALU.add, ALU.min),
                     r=[h_bgu1[sl]], w=[PH[ub], h_upc[i2]])
                k.op("dve", lambda e: e.tensor_tensor(out=gs[i2], in0=gate[i2], in1=sig[i2], op=ALU.mult),
                     r=[h_gate[i2], h_sig[i2]], w=[h_gs[i2]])
                k.op("dve", lambda e: e.scalar_tensor_tensor(out=yT[sl][:, ft, :], in0=upc[i2], scalar=-6.0, in1=gs[i2],
                                                             op0=ALU.max, op1=ALU.mult), r=[h_gs[i2], h_upc[i2]], w=[h_yT[sl]])
            if bi + 1 < NBLK:
                transposes(bi + 1)
            for s4 in range(NSUB):
                yb2 = ysi % 2
                ysi += 1
                for half in range(2):
                    bank = 6 + half
                    for fc in range(8):
                        mm(PS[bank], yT[sl][:, fc, s4 * 128:(s4 + 1) * 128], wdn[sl][:, fc, half * 512:(half + 1) * 512],
                           start=(fc == 0), stop=False, r=[h_yT[sl], h_wdn[sl][fc]], bank=bank)
                    mm(PS[bank], ones_row, bdn[sl][0:1, half * 512:(half + 1) * 512], start=False, stop=True,
                       r=[h_ones_row, h_bdn[sl]], bank=bank)
                    act(ysb[yb2][:, half * 512:(half + 1) * 512], PS[bank], AF.Copy, r=[], w=[PH[bank], h_ysb[yb2]])
                r0 = bi * MOE_B + s4 * 128
                k.dma("sp", ys_d[r0:r0 + 128, :], ysb[yb2], r=[h_ysb[yb2]], w=[h_ys])
        k.barrier()

    with ExitStack() as es:
        gr = [[sb(es, f"c_g{i}_{kk}", [128, D]) for kk in range(4)] for i in range(2)]
        h_gr = [[H() for _ in range(4)] for _ in range(2)]
        x1b = [sb(es, f"c_x1{i}", [128, D]) for i in range(2)]; h_x1b = [H(), H()]
        acc = [sb(es, f"c_acc{i}", [128, D]) for i in range(2)]; h_acc = [H(), H()]
        for t in range(NT_OWN):
            b2 = t % 2
            tok = slice(t * 128, (t + 1) * 128)
            k.dma("sp", x1b[b2], x1_d[tok, :], w=[h_x1b[b2]])
            for kk in range(4):
                igather(gr[b2][kk], ys_d, idx_all[:, t * 4 + kk:t * 4 + kk + 1], r=[h_idx_all, h_ys], w=[h_gr[b2][kk]], bounds=reg_bounds)
            k.op("dve", lambda e: e.tensor_scalar(acc[b2], gr[b2][0], wk_all[:, t, 0:1], None, ALU.mult),
                 r=[h_gr[b2][0], h_wk_all], w=[h_acc[b2]])
            for kk in range(1, 4):
                k.op("dve", lambda e: e.scalar_tensor_tensor(out=acc[b2], in0=gr[b2][kk], scalar=wk_all[:, t, kk:kk + 1],
                                                             in1=acc[b2], op0=ALU.mult, op1=ALU.add),
                     r=[h_gr[b2][kk], h_wk_all, h_acc[b2]], w=[h_acc[b2]])
            k.op("dve", lambda e: e.tensor_tensor(out=acc[b2], in0=acc[b2], in1=gt2_bc, op=ALU.mult),
                 r=[h_acc[b2], h_gt2], w=[h_acc[b2]])
            k.op("dve", lambda e: e.tensor_tensor(out=acc[b2], in0=acc[b2], in1=x1b[b2], op=ALU.add),
                 r=[h_acc[b2], h_x1b[b2]], w=[h_acc[b2]])
            k.dma("sp", out_d[tok, :], acc[b2], r=[h_acc[b2]])
        k.barrier()
    k.final_wait()
    top.close()
    return nc


def _t5_bucket(n):
    max_exact = 16
    nf = np.maximum(n, 1).astype(np.float32)
    large = max_exact + (np.log(nf / np.float32(max_exact)) / np.float32(math.log(128 / 16))
                         * np.float32(32 - max_exact)).astype(np.int32)
    large = np.minimum(large, 31)
    return np.where(n < max_exact, n, large)


def _bias_tiles(table, half):
    tiles = np.empty((8, 6, 128, 512), np.float32)
    kk = np.arange(128)[:, None]
    qq = np.arange(512)[None, :]
    for i in range(4):
        d = qq - (i * 128 + kk)
        b = _t5_bucket(np.maximum(d, 0))
        for hm in range(8):
            tiles[hm, i] = np.where(d >= 0, table[b, hm], np.float32(NEG))
    d = qq + 128 - kk
    b = _t5_bucket(d)
    for hm in range(8):
        tiles[hm, 4] = table[b, hm]
        tiles[hm, 5] = table[b, hm] if half == 1 else np.float32(NEG)
    return tiles


_NC_CACHE = {}


def kernel(x, c, rel_bias_table, w_ada, b_ada, g_norm1, w_in, w_gk_up, b_gk_up, g_gla_out, g_qnorm, g_knorm,
           lambda_q1, lambda_k1, lambda_q2, lambda_k2, g_subln, w_out, g_norm2, w_router, b_router,
           w_gate_up, b_gate_up, w_down, b_down):
    f = lambda a: np.ascontiguousarray(np.asarray(a, dtype=np.float32))
    x, c, table = f(x), f(c), f(rel_bias_table)
    w_ada, b_ada, w_in = f(w_ada)[0], f(b_ada)[0], f(w_in)[0]
    w_gk_up, b_gk_up = f(w_gk_up)[0], f(b_gk_up)[0]
    w_out, w_router, b_router = f(w_out)[0], f(w_router)[0], f(b_router)[0]
    w_gate_up, b_gate_up, w_down, b_down = f(w_gate_up)[0], f(b_gate_up)[0], f(w_down)[0], f(b_down)[0]
    colT = lambda v: f(v.reshape(-1, 128).T)
    rep = lambda v, n=128: f(np.broadcast_to(v[None, :], (n, v.shape[0])))

    ii = np.arange(128)
    shared = {
        "b_adaT": colT(b_ada), "b_ada_row": f(b_ada[None, :]),
        "g1T": colT(f(g_norm1)[0]), "g2T": colT(f(g_norm2)[0]),
        "w_ada": w_ada, "w_in": w_in, "w_gk_up": w_gk_up,
        "b_gkT": colT(b_gk_up), "b_gk_bc": rep(b_gk_up),
        "ggla_col": f(f(g_gla_out)[0][:, None]),
        "gq_col": f(np.tile(f(g_qnorm)[0], 2)[:, None]), "gk_col": f(np.tile(f(g_knorm)[0], 2)[:, None]),
        "lam4": f(np.broadcast_to(np.stack([f(lambda_q1)[0], f(lambda_k1)[0], f(lambda_q2)[0], f(lambda_k2)[0]])[None],
                                  (128, 4, 64))),
        "gsub_bc": rep(f(g_subln)[0]),
        "w_out": w_out, "w_router": w_router, "b_router_bc": rep(b_router),
        "w_gate_up": w_gate_up, "b_gu_rows": f(b_gate_up.reshape(E, 16, 128).transpose(0, 2, 1).reshape(E * 128, 16)),
        "g2_bc": rep(f(g_norm2)[0]),
        "ustrict": (ii[:, None] < ii[None, :]).astype(np.float32),
        "ones_f": np.ones((128, 128), np.float32),
        "iota64": f(np.broadcast_to(np.arange(NBLK, dtype=np.float32)[None, :], (128, NBLK))),
        "pidx": f(ii.astype(np.float32)[:, None]),
        "w_down": w_down, "b_down": b_down,
        "cF": rep(table[31]),
        "ident": np.eye(128, dtype=np.float32),
        "maskT": (ii[:, None] <= ii[None, :]).astype(np.float32),
        "Lmat": np.where(ii[:, None] > ii[None, :], np.float32(-1.0 / 16.0), np.float32(0.0)).astype(np.float32),
        "keep": f(np.tile(np.where(ii == 0, 0.0, 1.0).astype(np.float32), 4)[None, :].repeat(128, 0)),
        "blk64": f(np.kron(np.eye(2, dtype=np.float32), np.full((64, 64), 1.0 / 64.0, np.float32))),
        "ones128": np.full((128, 128), 1.0 / 128.0, np.float32),
        "ones_row": np.ones((1, 128), np.float32),
    }
    tiles = [_bias_tiles(table, 0), _bias_tiles(table, 1)]
    in_maps = []
    for core in range(8):
        b, half = core // 2, core % 2
        m = dict(shared)
        m["x_pre"] = f(x[b, 0:HALF])
        m["x_own"] = f(x[b, half * HALF:(half + 1) * HALF])
        m["cT"] = colT(c[b])
        m["bias_tiles"] = tiles[half]
        m["cP"] = shared["cF"] if half == 1 else np.full((128, 8), NEG, np.float32)
        m["flag"] = np.full((128, 1), float(half), np.float32)
        in_maps.append(m)
    if "nc" not in _NC_CACHE:
        _NC_CACHE["nc"] = build_program()
    nc = _NC_CACHE["nc"]
    res = run_bass_kernel_spmd(nc, in_maps, core_ids=list(range(8)))
    out = np.empty((4, S, D), np.float32)
    for core in range(8):
        b, half = core // 2, core % 2
        out[b, half * HALF:(half + 1) * HALF] = res.results[core]["out"]
    kernel.last_results = res.results
    return out
```

```python
import math
from contextlib import ExitStack
import numpy as np
import concourse.bass as bass
import concourse.mybir as mybir
from concourse.bass_utils import run_bass_kernel_spmd
from concourse.alu_op_type import AluOpType as ALU

F32 = mybir.dt.float32
BF16 = mybir.dt.bfloat16
AF = mybir.ActivationFunctionType
AX = mybir.AxisListType

D = 1024
S = 8192
HALF = 4096
NT_OWN = 32
NT_K = 64
E = 32
IN_W = 3088
OFF_QG, OFF_KG, OFF_VG, OFF_RG, OFF_GK, OFF_QD, OFF_KD, OFF_VD = 0, 256, 512, 1024, 1536, 1552, 2064, 2576
EPS = 1e-6
NEG = -30000.0
LAMBDA_INIT = 0.8 - 0.6 * math.exp(-0.3 * 0)
NQ = 12
MOE_B = 256
NBLK = (HALF * 4 - E) // MOE_B + E
PSLOTS = NBLK * MOE_B
I32 = mybir.dt.int32

import os
DEBUG = bool(os.environ.get('KDEBUG'))


class H:
    __slots__ = ("w", "r", "psum")

    def __init__(self, psum=False):
        self.w = None
        self.r = {}
        self.psum = psum


class KB:
    def __init__(self, nc):
        self.nc = nc
        self.E = {"pe": nc.tensor, "act": nc.scalar, "dve": nc.vector, "pool": nc.gpsimd, "sp": nc.sync}
        self.sems = []
        self.esem = {}
        self.ecnt = {}
        for n in self.E:
            self.esem[n] = self._newsem("e_" + n)
            self.ecnt[n] = 0
        self.waited = {n: {} for n in self.E}
        self.dq = {}
        for q in ("sp", "pool"):
            self.dq[q] = {"sems": [self._newsem(f"d_{q}{i}") for i in range(NQ)], "cnt": [0] * NQ, "rr": 0}

    def _newsem(self, name):
        s = self.nc.alloc_semaphore(name)
        self.sems.append(s)
        return len(self.sems) - 1

    def _wait(self, en, evs, raw=()):
        need = {}
        own = self.esem[en]
        for ev in list(raw) + list(evs):
            if ev is None:
                continue
            k, v = ev
            if k == own and en == "pe":
                continue
            if need.get(k, 0) < v:
                need[k] = v
        wd = self.waited[en]
        for k, v in need.items():
            if wd.get(k, 0) >= v:
                continue
            self.E[en].wait_ge(self.sems[k], v)
            wd[k] = v

    def _deps(self, r, w, en=None):
        evs = []
        own = self.esem.get(en) if en is not None else None
        for h in w:
            cand = [h.w] + list(h.r.values())
            if h.psum and own is not None:
                cand = [ev for ev in cand if ev is not None and ev[0] != own]
            evs.extend(cand)
        return evs

    @staticmethod
    def _raw(r):
        return [h.w for h in r]

    @staticmethod
    def _mark(ev, r, w):
        for h in r:
            h.r[ev[0]] = ev
        for h in w:
            h.w = ev
            h.r = {}

    def op(self, en, fn, r=(), w=()):
        self._wait(en, self._deps(r, w, en), self._raw(r))
        inst = fn(self.E[en])
        self.ecnt[en] += 1
        inst.then_inc(self.sems[self.esem[en]], 1)
        ev = (self.esem[en], self.ecnt[en])
        self._mark(ev, r, w)
        return ev

    def dma(self, q, out, in_, r=(), w=()):
        d = self.dq[q]
        i = d["rr"]
        d["rr"] = (i + 1) % NQ
        evs = self._deps(r, w)
        if d["cnt"][i] > 0:
            evs.append((d["sems"][i], 16 * d["cnt"][i]))
        self._wait(q, evs, self._raw(r))
        inst = self.E[q].dma_start(out=out, in_=in_)
        d["cnt"][i] += 1
        inst.then_inc(self.sems[d["sems"][i]], 16)
        ev = (d["sems"][i], 16 * d["cnt"][i])
        self._mark(ev, r, w)
        return ev

    def all_events(self):
        evs = [(self.esem[n], self.ecnt[n]) for n in self.E if self.ecnt[n] > 0]
        for q, d in self.dq.items():
            for i in range(NQ):
                if d["cnt"][i] > 0:
                    evs.append((d["sems"][i], 16 * d["cnt"][i]))
        return evs

    def barrier(self):
        evs = self.all_events()
        for en in self.E:
            need = [e for e in evs if e[0] != self.esem[en]]
            self._wait(en, need)

    def final_wait(self):
        self._wait("sp", self.all_events())


def build_program():
    nc = bass.Bass("TRN2", target_bir_lowering=False)
    k = KB(nc)

    def din(name, shape, dt=F32):
        return nc.dram_tensor(name, list(shape), dt, kind="ExternalInput").ap()

    def dscratch(name, shape, dt):
        kind = "ExternalOutput" if (DEBUG and name in ("h2tm_scr", "xs_scr", "ys_scr", "x1_scr")) else "Internal"
        return nc.dram_tensor(name, list(shape), dt, kind=kind).ap()

    x_pre = din("x_pre", [HALF, D])
    x_own = din("x_own", [HALF, D])
    cT_d = din("cT", [128, 8])
    b_adaT_d = din("b_adaT", [128, 48])
    b_ada_row_d = din("b_ada_row", [1, 6 * D])
    g1T_d = din("g1T", [128, 8])
    g2T_d = din("g2T", [128, 8])
    w_ada_d = din("w_ada", [D, 6 * D])
    w_in_d = din("w_in", [D, IN_W])
    w_gk_up_d = din("w_gk_up", [16, 256])
    b_gkT_d = din("b_gkT", [128, 2])
    b_gk_bc_d = din("b_gk_bc", [128, 256])
    ggla_d = din("ggla_col", [128, 1])
    gq_d = din("gq_col", [128, 1])
    gkn_d = din("gk_col", [128, 1])
    lam4_d = din("lam4", [128, 4, 64])
    gsub_d = din("gsub_bc", [128, 128])
    w_out_d = din("w_out", [D, D])
    w_router_d = din("w_router", [D, E])
    b_router_d = din("b_router_bc", [128, E])
    w_gu_d = din("w_gate_up", [E, D, 2 * D])
    b_gu_rows_d = din("b_gu_rows", [E * 128, 16])
    g2_bc_d = din("g2_bc", [128, D])
    ustrict_d = din("ustrict", [128, 128])
    ones_f_d = din("ones_f", [128, 128])
    iota64_d = din("iota64", [128, NBLK])
    pidx_d = din("pidx", [128, 1])
    w_dn_d = din("w_down", [E, D, D])
    b_dn_d = din("b_down", [E, D])
    bias_d = din("bias_tiles", [8, 6, 128, 512])
    cF_d = din("cF", [128, 8])
    cP_d = din("cP", [128, 8])
    flag_d = din("flag", [128, 1])
    ident_d = din("ident", [128, 128])
    maskT_d = din("maskT", [128, 128])
    Lmat_d = din("Lmat", [128, 128])
    keep_d = din("keep", [128, 512])
    blk64_d = din("blk64", [128, 128])
    ones128_d = din("ones128", [128, 128])
    ones_row_d = din("ones_row", [1, 128])

    out_d = nc.dram_tensor("out", [HALF, D], F32, kind="ExternalOutput").ap()
    dbg = {}
    if DEBUG:
        for nm, shp in (("cnt", [128, E]), ("cs", [128, E]), ("pstart", [128, E]), ("be", [128, NBLK]),
                        ("idxf", [128, NT_OWN * 4]), ("wk", [128, NT_OWN * 4]), ("lg", [128, NT_OWN * E]),
                        ("wts", [128, NT_OWN * E]), ("msk", [128, NT_OWN * E])):
            dbg[nm] = nc.dram_tensor("dbg_" + nm, shp, F32, kind="ExternalOutput").ap()
        for nm, shp, dt_ in (("xT", [128, 8, 512], BF16), ("wgu", [128, 8, 2 * D], BF16), ("wdn", [128, 8, D], BF16),
                             ("bgu", [128, 16], F32), ("bdn", [128, D], F32), ("yT", [128, 8, 512], BF16),
                             ("bidx", [128, 3 * NBLK], I32)):
            dbg[nm] = nc.dram_tensor("dbg_" + nm, shp, dt_, kind="ExternalOutput").ap()

    hT_d = dscratch("hT_scr", [8, 128, S], BF16)
    h2tm_d = dscratch("h2tm_scr", [HALF, D], BF16)
    xs_d = dscratch("xs_scr", [PSLOTS, D], BF16)
    ys_d = dscratch("ys_scr", [PSLOTS, D], F32)
    wgu_bf_d = dscratch("wgu_bf_scr", [E * 128, 8 * 2 * D], BF16)
    wdn_bf_d = dscratch("wdn_bf_scr", [E * 128, 8 * D], BF16)
    x1_d = dscratch("x1_scr", [HALF, D], F32)

    top = ExitStack()

    uid = [0]

    def sb(es, name, shape, dt=F32):
        uid[0] += 1
        return es.enter_context(nc.sbuf_tensor(f"s{uid[0]}_{name}", list(shape), dt)).ap()

    PS = [top.enter_context(nc.psum_tensor(f"ps{i}", [128, 512], F32)).ap() for i in range(8)]
    PH = [H(psum=True) for _ in range(8)]

    def const(name, src, shape, dt=F32, q="sp"):
        t = sb(top, name, shape, dt)
        h = H()
        k.dma(q, t, src, w=[h])
        return t, h

    ident_f, h_ident_f = const("ident_f", ident_d, [128, 128])
    ident_b, h_ident_b = const("ident_b", ident_d, [128, 128], BF16, q="pool")
    maskT, h_maskT = const("maskT", maskT_d, [128, 128])
    Lmat, h_Lmat = const("Lmat", Lmat_d, [128, 128])
    keep, h_keep = const("keep", keep_d, [128, 512])
    blk64, h_blk64 = const("blk64", blk64_d, [128, 128], BF16, q="pool")
    ones128, h_ones128 = const("ones128", ones128_d, [128, 128], BF16, q="pool")
    ones_row, h_ones_row = const("ones_row", ones_row_d, [1, 128])
    cT, h_cT = const("cT", cT_d, [128, 8])
    b_adaT, h_b_adaT = const("b_adaT", b_adaT_d, [128, 48])
    g1T, h_g1T = const("g1T", g1T_d, [128, 8])
    g2T, h_g2T = const("g2T", g2T_d, [128, 8])
    w_gk_up = sb(top, "w_gk_up_pad", [128, 256]); h_w_gk_up = H()
    k.op("dve", lambda e: e.memset(w_gk_up, 0.0), w=[h_w_gk_up])
    k.dma("sp", w_gk_up[0:16, :], w_gk_up_d, w=[h_w_gk_up])
    b_gkT, h_b_gkT = const("b_gkT", b_gkT_d, [128, 2])
    b_gk_bc, h_b_gk_bc = const("b_gk_bc", b_gk_bc_d, [128, 256])
    ggla, h_ggla = const("ggla", ggla_d, [128, 1])
    gq, h_gq = const("gq", gq_d, [128, 1])
    gkn, h_gkn = const("gkn", gkn_d, [128, 1])
    lam4, h_lam4 = const("lam4", lam4_d, [128, 4, 64])
    gsub, h_gsub = const("gsub", gsub_d, [128, 128])
    b_router, h_b_router = const("b_router", b_router_d, [128, E])
    cF0, h_cF0 = const("cF", cF_d, [128, 8])
    cP0, h_cP0 = const("cP", cP_d, [128, 8])
    SHIFT = 8.0
    cF = sb(top, "cF_sh", [128, 8]); h_cF = H()
    cP = sb(top, "cP_sh", [128, 8]); h_cP = H()
    k.op("dve", lambda e: e.tensor_scalar(cF, cF0, -SHIFT, None, ALU.add), r=[h_cF0], w=[h_cF])
    k.op("dve", lambda e: e.tensor_scalar(cP, cP0, -SHIFT, None, ALU.add), r=[h_cP0], w=[h_cP])
    flag, h_flag = const("flag", flag_d, [128, 1])
    ustrict, h_ustrict = const("ustrict", ustrict_d, [128, 128])
    ones_f, h_ones_f = const("ones_f", ones_f_d, [128, 128])
    iota64, h_iota64 = const("iota64", iota64_d, [128, NBLK])
    lane_id, h_pidx = const("lane_id", pidx_d, [128, 1])

    a1T = sb(top, "a1T", [128, 8]); h_a1T = H()
    sh1T = sb(top, "sh1T", [128, 8]); h_sh1T = H()
    gt2_bc = sb(top, "gt2_bc", [128, D]); h_gt2 = H()
    wts = sb(top, "wts", [128, NT_OWN, E]); h_wts = H()
    lg_all = sb(top, "lg_all", [128, NT_OWN, E]); h_lg_all = H()
    top8_all = sb(top, "top8_all", [128, NT_OWN, 8]); h_top8_all = H()
    msk_all = sb(top, "msk_all", [128, NT_OWN, E]); h_msk_all = H()
    idxf_all = sb(top, "idxf_all", [128, NT_OWN, 4]); h_idxf_all = H()
    wk_all = sb(top, "wk_all", [128, NT_OWN, 4]); h_wk_all = H()
    idx_all = sb(top, "idx_all", [128, NT_OWN * 4], I32); h_idx_all = H()
    blk_idx = sb(top, "blk_idx", [128, 3 * NBLK], I32); h_blk_idx = H()
    modbc_d = dscratch("modbc_scr", [3, 128, D], F32)
    neg_bgkT = sb(top, "neg_bgkT", [128, 2]); h_neg_bgkT = H()
    gq8 = sb(top, "gq8", [128, 1]); h_gq8 = H()
    neglam = sb(top, "neglam", [128, 1]); h_neglam = H()
    gsub8 = sb(top, "gsub8", [128, 128]); h_gsub8 = H()
    eps_col = sb(top, "eps_col", [128, 1]); h_eps = H()
    one_col = sb(top, "one_col", [128, 1]); h_one = H()
    k.op("dve", lambda e: e.memset(eps_col, EPS), w=[h_eps])
    k.op("dve", lambda e: e.memset(one_col, 1.0), w=[h_one])

    def act(out, in_, func, r, w, bias=None, scale=None):
        kw = {}
        if bias is not None:
            kw["bias"] = bias
        if scale is not None:
            kw["scale"] = scale
        return k.op("act", lambda e: e.activation(out=out, in_=in_, func=func, **kw), r=r, w=w)

    def mm(out, lhsT, rhs, start, stop, r, bank, skip=False):
        kw = {"skip_group_check": True} if skip else {}
        return k.op("pe", lambda e: e.matmul(out, lhsT, rhs, start=start, stop=stop, **kw), r=r, w=[PH[bank]])

    def rstd_from_ss(out_col, ss_col, scale, r_h, w_h, tmp, h_tmp):
        act(tmp, ss_col, AF.Ln, r=[r_h, h_eps], w=[h_tmp], bias=eps_col[0:tmp.shape[0], :], scale=scale)
        act(out_col, tmp, AF.Exp, r=[h_tmp], w=[w_h], scale=-0.5)

    with ExitStack() as es:
        cond = sb(es, "cond", [128, 8]); h_cond = H()
        act(cond, cT, AF.Silu, r=[h_cT], w=[h_cond])
        wa = [sb(es, f"wa{i}", [128, 8, D]) for i in range(2)]
        h_wa = [H(), H()]
        modT = sb(es, "modT", [128, 2, 8]); h_modT = H()
        rows = sb(es, "rows", [1, 4, D]); h_rows = H()
        brow = sb(es, "brow", [1, 4, D]); h_brow = H()
        g2bc = sb(es, "g2bc", [128, D]); h_g2bc = H()
        gt1_bc = sb(es, "gt1_bc0", [128, D]); h_gt1 = H()
        a2_bc = sb(es, "a2_bc0", [128, D]); h_a2bc = H()
        sh2_bc = sb(es, "sh2_bc0", [128, D]); h_sh2bc = H()
        k.dma("sp", g2bc, g2_bc_d, w=[h_g2bc])
        row_slot = {2: 0, 5: 1, 3: 2, 4: 3}
        for s6_, ri_ in row_slot.items():
            k.dma("sp", brow[:, ri_, :], b_ada_row_d[:, s6_ * D:(s6_ + 1) * D], w=[h_brow])
        fm_slot = {0: 0, 1: 1}
        for s6 in range(6):
            wb, hb = wa[s6 % 2], h_wa[s6 % 2]
            k.dma("sp", wb, w_ada_d[:, s6 * D:(s6 + 1) * D].rearrange("(kc p) n -> p kc n", p=128), w=[hb])
            if s6 in fm_slot:
                sl = fm_slot[s6]
                for nt in range(8):
                    for kc in range(8):
                        mm(PS[0][:, nt:nt + 1], wb[:, kc, nt * 128:(nt + 1) * 128], cond[:, kc:kc + 1],
                           start=(kc == 0), stop=(kc == 7), r=[hb, h_cond], bank=0, skip=True)
                k.op("dve", lambda e: e.tensor_tensor(out=modT[:, sl, :], in0=PS[0][:, 0:8],
                                                      in1=b_adaT[:, s6 * 8:(s6 + 1) * 8], op=ALU.add),
                     r=[h_b_adaT], w=[PH[0], h_modT])
            else:
                ri = row_slot[s6]
                for half in range(2):
                    for kc in range(8):
                        mm(PS[1][0:1, :], cond[:, kc:kc + 1], wb[:, kc, half * 512:(half + 1) * 512],
                           start=(kc == 0), stop=(kc == 7), r=[hb, h_cond], bank=1)
                    k.op("dve", lambda e: e.tensor_tensor(out=rows[:, ri, half * 512:(half + 1) * 512],
                                                          in0=PS[1][0:1, :],
                                                          in1=brow[:, ri, half * 512:(half + 1) * 512], op=ALU.add),
                         r=[h_brow], w=[PH[1], h_rows])
                dst, hd_ = {2: (gt1_bc, h_gt1), 5: (gt2_bc, h_gt2), 3: (sh2_bc, h_sh2bc), 4: (a2_bc, h_a2bc)}[s6]
                for half in range(2):
                    mm(PS[2], ones_row, rows[:, ri, half * 512:(half + 1) * 512], start=True, stop=True,
                       r=[h_ones_row, h_rows], bank=2)
                    k.op("dve", lambda e: e.tensor_copy(dst[:, half * 512:(half + 1) * 512], PS[2]),
                         w=[PH[2], hd_])
        k.op("dve", lambda e: e.scalar_tensor_tensor(out=a1T, in0=modT[:, 1, :], scalar=1.0, in1=g1T,
                                                     op0=ALU.add, op1=ALU.mult), r=[h_modT, h_g1T], w=[h_a1T])
        k.op("dve", lambda e: e.tensor_copy(sh1T, modT[:, 0, :]), r=[h_modT], w=[h_sh1T])
        k.op("dve", lambda e: e.scalar_tensor_tensor(out=a2_bc, in0=a2_bc, scalar=1.0, in1=g2bc,
                                                     op0=ALU.add, op1=ALU.mult), r=[h_a2bc, h_g2bc], w=[h_a2bc])
        k.dma("sp", modbc_d[0], gt1_bc, r=[h_gt1])
        k.dma("sp", modbc_d[1], a2_bc, r=[h_a2bc])
        k.dma("sp", modbc_d[2], sh2_bc, r=[h_sh2bc])
        k.op("dve", lambda e: e.tensor_scalar(neg_bgkT, b_gkT, -1.0, None, ALU.mult), r=[h_b_gkT], w=[h_neg_bgkT])
        k.op("dve", lambda e: e.tensor_scalar(gq8, gq, 0.125, None, ALU.mult), r=[h_gq], w=[h_gq8])
        k.op("dve", lambda e: e.tensor_scalar(gsub8, gsub, 1.0 - LAMBDA_INIT, None, ALU.mult), r=[h_gsub], w=[h_gsub8])
        lp = sb(es, "lp", [128, 2, 64]); h_lp = H()
        ls = sb(es, "ls", [128, 2]); h_ls = H()
        le = sb(es, "le", [128, 2]); h_le = H()
        k.op("dve", lambda e: e.tensor_tensor(out=lp[:, 0, :], in0=lam4[:, 0, :], in1=lam4[:, 1, :], op=ALU.mult),
             r=[h_lam4], w=[h_lp])
        k.op("dve", lambda e: e.tensor_tensor(out=lp[:, 1, :], in0=lam4[:, 2, :], in1=lam4[:, 3, :], op=ALU.mult),
             r=[h_lam4], w=[h_lp])
        k.op("dve", lambda e: e.tensor_reduce(out=ls, in_=lp, axis=AX.X, op=ALU.add), r=[h_lp], w=[h_ls])
        act(le, ls, AF.Exp, r=[h_ls], w=[h_le])
        k.op("dve", lambda e: e.scalar_tensor_tensor(out=neglam, in0=le[:, 1:2], scalar=-LAMBDA_INIT, in1=le[:, 0:1],
                                                     op0=ALU.add, op1=ALU.subtract), r=[h_le], w=[h_neglam])
        k.barrier()

    all_casts = []
    for ex_ in range(E):
        all_casts.append((wgu_bf_d[ex_ * 128:(ex_ + 1) * 128, :].rearrange("p (kc f) -> p kc f", kc=8),
                          w_gu_d[ex_].rearrange("(kc p) f -> p kc f", p=128)))
        all_casts.append((wdn_bf_d[ex_ * 128:(ex_ + 1) * 128, :].rearrange("p (kc f) -> p kc f", kc=8),
                          w_dn_d[ex_].rearrange("(kc p) f -> p kc f", p=128)))

    def issue_casts(n):
        for _ in range(min(n, len(all_casts))):
            dst_, src_ = all_casts.pop(0)
            k.dma("pool", dst_, src_)

    with ExitStack() as es:
        xb = [sb(es, f"xb{i}", [128, D]) for i in range(3)]
        h_xb = [H() for _ in range(3)]
        sq = sb(es, "sq_junk", [128, D], BF16); h_sq = H()
        ss = [sb(es, f"ss{i}", [128, 1]) for i in range(2)]; h_ss = [H(), H()]
        sd = [sb(es, f"sd{i}", [128, 1]) for i in range(2)]; h_sd = [H(), H()]
        rs = [sb(es, f"rs{i}", [128, 1]) for i in range(2)]; h_rs = [H(), H()]
        xn = [sb(es, f"xn{i}", [128, D], BF16) for i in range(2)]; h_xn = [H(), H()]
        hT = [sb(es, f"hTst{i}", [128, 8, 512], BF16) for i in range(2)]; h_hT = [H(), H()]; h_hTa = [H(), H()]
        psb = [PS[0].bitcast(BF16), PS[1].bitcast(BF16)]
        def p1_stage1(i):
            src = x_pre if i < 32 else x_own
            ti = i % 32
            b3 = i % 3
            b2 = i % 2
            k.dma("sp", xb[b3], src[ti * 128:(ti + 1) * 128, :], w=[h_xb[b3]])
            act(sq, xb[b3], AF.Square, r=[h_xb[b3]], w=[h_sq])
            k.op("dve", lambda e: e.tensor_reduce(out=ss[b2], in_=sq, axis=AX.X, op=ALU.add), r=[h_sq], w=[h_ss[b2]])
            rstd_from_ss(rs[b2], ss[b2], 1.0 / D, h_ss[b2], h_rs[b2], sd[b2], h_sd[b2])
            k.op("dve", lambda e: e.tensor_scalar(xn[b2], xb[b3], rs[b2], None, ALU.mult),
                 r=[h_xb[b3], h_rs[b2]], w=[h_xn[b2]])

        def p1_stage2(i):
            b2 = i % 2
            sbi = i // 4
            hb = (sbi % 2)
            bd, ba = 2 * b2, 2 * b2 + 1
            pvd = PS[bd].bitcast(BF16)
            pva = PS[ba].bitcast(BF16)
            for kc in range(8):
                pv_, bank_ = (pvd, bd) if kc < 4 else (pva, ba)
                k.op("pe", lambda e: e.transpose(pv_[:, (kc % 4) * 128:(kc % 4 + 1) * 128], xn[b2][:, kc * 128:(kc + 1) * 128],
                                                 ident_b), r=[h_xn[b2], h_ident_b], w=[PH[bank_]])
            for kc in range(4):
                dst = hT[hb][:, kc, (i % 4) * 128:(i % 4 + 1) * 128]
                k.op("dve", lambda e: e.tensor_scalar(dst, pvd[:, kc * 128:(kc + 1) * 128], a1T[:, kc:kc + 1],
                                                      sh1T[:, kc:kc + 1], ALU.mult, ALU.add),
                     r=[h_a1T, h_sh1T], w=[PH[bd], h_hT[hb]])
            for kc in range(4, 8):
                dst = hT[hb][:, kc, (i % 4) * 128:(i % 4 + 1) * 128]
                act(dst, pva[:, (kc - 4) * 128:(kc - 3) * 128], AF.Identity, r=[h_a1T, h_sh1T], w=[PH[ba], h_hTa[hb]],
                    bias=sh1T[:, kc:kc + 1], scale=a1T[:, kc:kc + 1])
            if i % 4 == 3:
                k.dma("pool", hT_d[:, :, sbi * 512:(sbi + 1) * 512].rearrange("k p t -> p k t"), hT[hb], r=[h_hT[hb], h_hTa[hb]])

        p1_stage1(0)
        for i in range(NT_K):
            if i + 1 < NT_K:
                p1_stage1(i + 1)
            p1_stage2(i)
            if i % 8 == 7:
                issue_casts(1)
        k.barrier()

    mix_es = ExitStack()
    mixT = sb(mix_es, "mixT", [128, 8, HALF], BF16)
    h_mixT = H()

    def load_w(es, name, col0, ncols, q="pool"):
        t = sb(es, name, [128, 8, ncols], BF16)
        h = H()
        k.dma(q, t, w_in_d[:, col0:col0 + ncols].rearrange("(kc p) n -> p kc n", p=128), w=[h])
        return t, h

    for P in range(2):
        with ExitStack() as es:
            wq, h_wq = load_w(es, "g_wq", OFF_QG + P * 128, 128)
            wk, h_wk = load_w(es, "g_wk", OFF_KG + P * 128, 128)
            wv, h_wv = load_w(es, "g_wv", OFF_VG + P * 256, 256)
            wr, h_wr = load_w(es, "g_wr", OFF_RG + P * 256, 256)
            wgk = sb(es, "g_wgk_pad", [128, 8, 128], BF16); h_wgk = H()
            k.op("dve", lambda e: e.memset(wgk, 0.0), w=[h_wgk])
            k.dma("pool", wgk[:, :, 0:16], w_in_d[:, OFF_GK:OFF_GK + 16].rearrange("(kc p) n -> p kc n", p=128), w=[h_wgk])
            hTb = [sb(es, f"g_hT{i}", [128, 8, 512], BF16) for i in range(2)]; h_hTb = [H(), H()]
            gkT2 = [sb(es, f"g_gkT{i}", [128, 512]) for i in range(2)]; h_gkT2 = [H(), H()]
            e1 = sb(es, "g_e1", [128, 512]); h_e1 = H()
            spf = sb(es, "g_spf", [128, 512]); h_spf = H()
            cum = sb(es, "g_cum", [128, 512]); h_cum = H()
            Eq2 = [sb(es, f"g_Eq{i}", [128, 512]) for i in range(2)]; h_Eq2 = [H(), H()]
            Ek = sb(es, "g_Ek", [128, 512]); h_Ek = H()
            keT2 = [sb(es, f"g_keT{i}", [128, 512], BF16) for i in range(2)]; h_keT2 = [H(), H()]
            qeTz2 = [[sb(es, f"g_qeTz{j}_{i}", [128, 512], BF16) for i in range(2)] for j in range(2)]; h_qeT2 = [H(), H()]
            for j_ in range(2):
                k.op("dve", lambda e: e.memset(qeTz2[j_][0][64:128, :], 0.0), w=[h_qeT2[j_]])
                k.op("dve", lambda e: e.memset(qeTz2[j_][1][0:64, :], 0.0), w=[h_qeT2[j_]])
            rsT2 = [sb(es, f"g_rsT{i}", [128, 2, 512]) for i in range(2)]; h_rsT2 = [H(), H()]
            vtm2 = [sb(es, f"g_vtm{i}", [128, 256], BF16) for i in range(2)]; h_vtm2 = [H(), H()]
            ks2 = [sb(es, f"g_ks{i}", [128, 128], BF16) for i in range(2)]; h_ks2 = [H(), H()]
            AT2 = [sb(es, f"g_AT{i}", [128, 128], BF16) for i in range(2)]; h_AT2 = [H(), H()]
            sqo2 = [sb(es, f"g_sqo{i}", [128, 128], BF16) for i in range(2)]; h_sqo2 = [H(), H()]
            sdo2 = [sb(es, f"g_sdo{i}", [128, 128]) for i in range(2)]; h_sdo2 = [H(), H()]
            rso2 = [sb(es, f"g_rso{i}", [128, 128]) for i in range(2)]; h_rso2 = [H(), H()]
            t12 = [sb(es, f"g_t1{i}", [128, 128]) for i in range(2)]; h_t12 = [H(), H()]
            gtm = sb(es, "g_gtm", [128, 128]); h_gtm = H()
            e1t = sb(es, "g_e1t", [128, 128]); h_e1t = H()
            spt = sb(es, "g_spt", [128, 128]); h_spt = H()
            Es = sb(es, "g_Es", [128, 128]); h_Es = H()
            ks = sb(es, "g_ks", [128, 128], BF16); h_ks = H()
            AT = sb(es, "g_AT", [128, 128], BF16); h_AT = H()
            Sf = sb(es, "g_Sf", [128, 128]); h_Sf = H()
            Sb = sb(es, "g_Sb", [128, 128], BF16); h_Sb = H()
            sqo = sb(es, "g_sqo", [128, 128], BF16); h_sqo = H()
            sdo = sb(es, "g_sdo", [128, 128]); h_sdo = H()
            rso = sb(es, "g_rso", [128, 128]); h_rso = H()
            t1 = sb(es, "g_t1", [128, 128]); h_t1 = H()
            k.op("dve", lambda e: e.memset(Sf, 0.0), w=[h_Sf])
            k.op("dve", lambda e: e.memset(Sb, 0.0), w=[h_Sb])
            def fm(sbi):
                own = sbi >= 8
                fb = sbi % 2
                hTc, h_hTc = hTb[fb], h_hTb[fb]
                k.dma("sp", hTc, hT_d[:, :, sbi * 512:(sbi + 1) * 512].rearrange("k p t -> p k t"), w=[h_hTc])
                for kc in range(8):
                    mm(PS[0], wgk[:, kc, :], hTc[:, kc, :], start=(kc == 0), stop=(kc == 7), r=[h_wgk, h_hTc], bank=0)
                k.op("dve", lambda e: e.tensor_copy(gkT2[fb], PS[0]), w=[PH[0], h_gkT2[fb]])
                mm(PS[1], w_gk_up[:, P * 128:(P + 1) * 128], gkT2[fb], start=True, stop=True, r=[h_w_gk_up, h_gkT2[fb]], bank=1)
                act(e1, PS[1], AF.Exp, r=[h_neg_bgkT], w=[PH[1], h_e1], bias=neg_bgkT[:, P:P + 1], scale=-1.0)
                act(spf, e1, AF.Ln, r=[h_e1, h_one], w=[h_spf], bias=one_col)
                k.op("dve", lambda e: e.tensor_tensor_scan(out=cum, data0=keep, data1=spf, initial=0.0,
                                                           op0=ALU.mult, op1=ALU.add), r=[h_keep, h_spf], w=[h_cum])
                act(Eq2[fb], cum, AF.Exp, r=[h_cum], w=[h_Eq2[fb]], scale=-1.0 / 16.0)
                act(Ek, cum, AF.Exp, r=[h_cum], w=[h_Ek], scale=1.0 / 16.0)
                for kc in range(8):
                    mm(PS[2], wk[:, kc, :], hTc[:, kc, :], start=(kc == 0), stop=(kc == 7), r=[h_wk, h_hTc], bank=2)
                k.op("dve", lambda e: e.tensor_tensor(out=keT2[fb], in0=PS[2], in1=Ek, op=ALU.mult),
                     r=[h_Ek], w=[PH[2], h_keT2[fb]])
                if own:
                    for kc in range(8):
                        mm(PS[0], wq[:, kc, :], hTc[:, kc, :], start=(kc == 0), stop=(kc == 7), r=[h_wq, h_hTc], bank=0)
                    for hq in range(2):
                        pr = slice(hq * 64, (hq + 1) * 64)
                        k.op("dve", lambda e: e.scalar_tensor_tensor(out=qeTz2[fb][hq][pr, :], in0=PS[0][pr, :], scalar=0.125,
                                                                     in1=Eq2[fb][pr, :], op0=ALU.mult, op1=ALU.mult),
                             r=[h_Eq2[fb]], w=[PH[0], h_qeT2[fb]])
                    for hd in range(2):
                        for kc in range(8):
                            mm(PS[1 + hd], wr[:, kc, hd * 128:(hd + 1) * 128], hTc[:, kc, :], start=(kc == 0), stop=(kc == 7),
                               r=[h_wr, h_hTc], bank=1 + hd)
                        act(rsT2[fb][:, hd, :], PS[1 + hd], AF.Silu, r=[], w=[PH[1 + hd], h_rsT2[fb]])

            fm(0)
            for sbi in range(16):
                issue_casts(1)
                own = sbi >= 8
                fb = sbi % 2
                hTc, h_hTc = hTb[fb], h_hTb[fb]
                gkT, h_gkT = gkT2[fb], h_gkT2[fb]
                Eq, h_Eq = Eq2[fb], h_Eq2[fb]
                keT, h_keT = keT2[fb], h_keT2[fb]
                qeTz, h_qeT = qeTz2[fb], h_qeT2[fb]
                rsT, h_rsT = rsT2[fb], h_rsT2[fb]
                if sbi == 8:
                    k.op("dve", lambda e: e.tensor_scalar(Sf, Sf, flag, None, ALU.mult), r=[h_flag, h_Sf], w=[h_Sf])
                    act(Sb, Sf, AF.Copy, r=[h_Sf], w=[h_Sb])
                BB = (7, 3)

                def stage_a1(t):
                    tok = slice(t * 128, (t + 1) * 128)
                    ab = t % 2
                    for kc in range(8):
                        mm(PS[4][:, 0:128], hTc[:, kc, tok], wk[:, kc, :], start=(kc == 0), stop=(kc == 7),
                           r=[h_wk, h_hTc], bank=4)
                    for kc in range(8):
                        mm(PS[5][:, 0:256], hTc[:, kc, tok], wv[:, kc, :], start=(kc == 0), stop=(kc == 7),
                           r=[h_wv, h_hTc], bank=5)
                    act(vtm2[ab], PS[5][:, 0:256], AF.Copy, r=[], w=[PH[5], h_vtm2[ab]])
                    mm(PS[6][:, 0:128], gkT[:, tok], w_gk_up[:, P * 128:(P + 1) * 128], start=True, stop=True,
                       r=[h_gkT, h_w_gk_up], bank=6)
                    k.op("dve", lambda e: e.tensor_tensor(out=gtm, in0=PS[6][:, 0:128],
                                                          in1=b_gk_bc[:, P * 128:(P + 1) * 128], op=ALU.add),
                         r=[h_b_gk_bc], w=[PH[6], h_gtm])
                    act(e1t, gtm, AF.Exp, r=[h_gtm], w=[h_e1t], scale=-1.0)
                    act(spt, e1t, AF.Ln, r=[h_e1t, h_one], w=[h_spt], bias=one_col)

                def stage_a2(t):
                    ab = t % 2
                    mm(PS[6][:, 128:256], Lmat, spt, start=True, stop=True, r=[h_Lmat, h_spt], bank=6)
                    act(Es, PS[6][:, 128:256], AF.Exp, r=[], w=[PH[6], h_Es])
                    k.op("dve", lambda e: e.tensor_tensor(out=ks2[ab], in0=PS[4][:, 0:128], in1=Es, op=ALU.mult),
                         r=[h_Es], w=[PH[4], h_ks2[ab]])

                def stage_b(t):
                    tok = slice(t * 128, (t + 1) * 128)
                    ab = t % 2
                    vt, h_vt, kst, h_kst = vtm2[ab], h_vtm2[ab], ks2[ab], h_ks2[ab]
                    if own:
                        gt = (sbi - 8) * 512 + t * 128
                        R = [slice(0, 64), slice(64, 128)]
                        for hd in range(2):
                            mm(PS[BB[hd]][:, 0:128], keT[:, tok], qeTz[hd][:, tok], start=True, stop=True,
                               r=[h_keT, h_qeT], bank=BB[hd])
                        for hd in range(2):
                            k.op("dve", lambda e: e.tensor_tensor(out=AT2[hd], in0=PS[BB[hd]][:, 0:128], in1=maskT, op=ALU.mult),
                                 r=[h_maskT], w=[PH[BB[hd]], h_AT2[hd]])
                        for hd in range(2):
                            mm(PS[BB[hd]][:, 128:256], vt[:, hd * 128:(hd + 1) * 128], AT2[hd], start=True, stop=False,
                               r=[h_vt, h_AT2[hd]], bank=BB[hd], skip=True)
                            mm(PS[BB[hd]][:, 128:256], Sb, qeTz[hd][:, tok], start=False, stop=True,
                               r=[h_Sb, h_qeT], bank=BB[hd], skip=True)
                        for hd in range(2):
                            act(sqo2[hd], PS[BB[hd]][:, 128:256], AF.Square, r=[], w=[PH[BB[hd]], h_sqo2[hd]])
                        for hd in range(2):
                            mm(PS[BB[hd]][:, 256:384], ones128, sqo2[hd], start=True, stop=True, r=[h_ones128, h_sqo2[hd]],
                               bank=BB[hd], skip=True)
                        for hd in range(2):
                            act(sdo2[hd], PS[BB[hd]][:, 256:384], AF.Ln, r=[h_eps], w=[PH[BB[hd]], h_sdo2[hd]], bias=eps_col)
                        for hd in range(2):
                            act(rso2[hd], sdo2[hd], AF.Exp, r=[h_sdo2[hd]], w=[h_rso2[hd]], scale=-0.5)
                        for hd in range(2):
                            k.op("dve", lambda e: e.scalar_tensor_tensor(out=t12[hd], in0=PS[BB[hd]][:, 128:256], scalar=ggla,
                                                                         in1=rso2[hd], op0=ALU.mult, op1=ALU.mult),
                                 r=[h_ggla, h_rso2[hd]], w=[PH[BB[hd]], h_t12[hd]])
                        for hd in range(2):
                            k.op("dve", lambda e: e.tensor_tensor(out=mixT[:, 2 * P + hd, gt:gt + 128], in0=t12[hd],
                                                                  in1=rsT[:, hd, tok], op=ALU.mult),
                                 r=[h_t12[hd], h_rsT], w=[h_mixT])
                    mm(PS[6][:, 256:512], kst, vt, start=True, stop=True, r=[h_kst, h_vt], bank=6, skip=True)
                    for hd in range(2):
                        pr = slice(hd * 64, (hd + 1) * 64)
                        dcol = Eq[pr, t * 128 + 127:t * 128 + 128]
                        k.op("dve", lambda e: e.scalar_tensor_tensor(out=Sf[pr, :], in0=Sf[pr, :], scalar=dcol,
                                                                     in1=PS[6][pr, 256 + hd * 128:256 + (hd + 1) * 128],
                                                                     op0=ALU.mult, op1=ALU.add), r=[h_Eq, h_Sf], w=[PH[6], h_Sf])
                    act(Sb, Sf, AF.Copy, r=[h_Sf], w=[h_Sb])

                stage_a1(0)
                stage_a2(0)
                for t in range(4):
                    if t + 1 < 4:
                        stage_a1(t + 1)
                    if t == 2 and sbi + 1 < 16:
                        fm(sbi + 1)
                    stage_b(t)
                    if t + 1 < 4:
                        stage_a2(t + 1)
            k.barrier()

    with ExitStack() as es:
        KT = sb(es, "a_KT", [128, S], BF16); h_KT = H()
        QTz = [sb(es, f"a_QT{i}", [128, HALF], BF16) for i in range(2)]; h_QT = H()
        k.op("dve", lambda e: e.memset(QTz[0][64:128, :], 0.0), w=[h_QT])
        k.op("dve", lambda e: e.memset(QTz[1][0:64, :], 0.0), w=[h_QT])
        Vg = sb(es, "a_V", [128, NT_K, 130], BF16); h_V = H()
        k.op("dve", lambda e: e.memset(Vg, 1.0), w=[h_V])
        hTb = [sb(es, f"a_hT{i}", [128, 8, 512], BF16) for i in range(2)]; h_hTb = [H(), H()]
        bt = sb(es, "a_bias", [128, 2, 6, 512]); h_bt = H()
        sqk2 = [sb(es, f"a_sqk{i}", [128, 512], BF16) for i in range(2)]; h_sqk2 = [H(), H()]
        sdk2 = [sb(es, f"a_sdk{i}", [128, 512]) for i in range(2)]; h_sdk2 = [H(), H()]
        PT = [sb(es, f"a_PT{i}", [128, 512], BF16) for i in range(4)]; h_PT = [H() for _ in range(4)]
        stmp0 = sb(es, "a_stmp0", [128, 512]); h_stmp0 = H()
        stmp = [stmp0, stmp0]; h_stmp = [h_stmp0, h_stmp0]
        vT_sb = sb(es, "a_vT", [128, 512], BF16); h_vT_sb = H()
        on0 = sb(es, "a_on0", [128, 4, 128]); h_on0 = H()
        od_single = sb(es, "a_od", [128, 4, 128]); h_od_single = H()
        od2 = [od_single, od_single]; h_od2 = [h_od_single, h_od_single]
        yb4 = [sb(es, f"a_yb{i}", [128, 128], BF16) for i in range(4)]; h_yb4 = [H() for _ in range(4)]
        pending = []
        rl = sb(es, "a_rl", [128, 4]); h_rl = H()
        nrl = sb(es, "a_nrl", [128, 4]); h_nrl = H()
        sqd = sb(es, "a_sqd", [128, 128]); h_sqd = H()
        ssd = sb(es, "a_ssd", [128, 1]); h_ssd = H()
        sdd = sb(es, "a_sdd", [128, 1]); h_sdd = H()
        rsd = sb(es, "a_rsd", [128, 1]); h_rsd = H()
        wqd = sb(es, "a_wq", [128, 8, 128], BF16); h_wqd = H()
        wkd = sb(es, "a_wk", [128, 8, 128], BF16); h_wkd = H()
        wvd = sb(es, "a_wv", [128, 8, 128], BF16); h_wvd = H()
        for hd in range(4):
            for wt_, hw_, off_ in ((wqd, h_wqd, OFF_QD), (wkd, h_wkd, OFF_KD), (wvd, h_wvd, OFF_VD)):
                k.dma("pool", wt_, w_in_d[:, off_ + hd * 128:off_ + (hd + 1) * 128].rearrange("(kc p) n -> p kc n", p=128),
                      w=[hw_])
            k.dma("sp", bt, bias_d[2 * hd:2 * hd + 2].rearrange("m s p q -> p m s q"), w=[h_bt])
            cast_jobs = [all_casts.pop(0) for _ in range(min(6, len(all_casts)))]
            if hd == 3:
                cast_jobs += all_casts
                all_casts = []
            units_p = []
            for sbi in range(16):
                units_p.append((sbi, 0))
                if sbi >= 8:
                    units_p.append((sbi, 1))
            loaded = set()

            def ensure_hT(sbi):
                if sbi not in loaded:
                    loaded.add(sbi)
                    hb_ = sbi % 2
                    k.dma("sp", hTb[hb_], hT_d[:, :, sbi * 512:(sbi + 1) * 512].rearrange("k p t -> p k t"), w=[h_hTb[hb_]])

            def proj_a(u):
                sbi, which = units_p[u]
                ensure_hT(sbi)
                hTc, h_hTc = hTb[sbi % 2], h_hTb[sbi % 2]
                wsel, h_wsel = (wkd, h_wkd) if which == 0 else (wqd, h_wqd)
                pa = u % 3
                for kc in range(8):
                    mm(PS[pa], wsel[:, kc, :], hTc[:, kc, :], start=(kc == 0), stop=(kc == 7), r=[h_wsel, h_hTc], bank=pa)
                act(sqk2[u % 2], PS[pa], AF.Square, r=[], w=[PH[pa], h_sqk2[u % 2]])

            def proj_b(u):
                sbi, which = units_p[u]
                gcol, h_gcol = (gkn, h_gkn) if which == 0 else (gq8, h_gq8)
                pa = u % 3
                pm = 3 if u % 2 == 0 else 6
                sq_, h_sq_ = sqk2[u % 2], h_sqk2[u % 2]
                sd_, h_sd_ = sdk2[u % 2], h_sdk2[u % 2]
                mm(PS[pm], blk64, sq_, start=True, stop=True, r=[h_blk64, h_sq_], bank=pm)
                act(sd_, PS[pm], AF.Ln, r=[h_eps], w=[PH[pm], h_sd_], bias=eps_col)
                act(sd_, sd_, AF.Exp, r=[h_sd_], w=[h_sd_], scale=-0.5)
                if which == 0:
                    dst = KT[:, sbi * 512:(sbi + 1) * 512]
                    k.op("dve", lambda e: e.scalar_tensor_tensor(out=dst, in0=PS[pa], scalar=gcol, in1=sd_,
                                                                 op0=ALU.mult, op1=ALU.mult),
                         r=[h_gcol, h_sd_], w=[PH[pa], h_KT])
                else:
                    for mq in range(2):
                        pr = slice(mq * 64, (mq + 1) * 64)
                        dst = QTz[mq][pr, (sbi - 8) * 512:(sbi - 7) * 512]
                        k.op("dve", lambda e: e.scalar_tensor_tensor(out=dst, in0=PS[pa][pr, :], scalar=gcol[pr, :],
                                                                     in1=sd_[pr, :], op0=ALU.mult, op1=ALU.mult),
                             r=[h_gcol, h_sd_], w=[PH[pa], h_QT])

            def proj_v(sbi):
                hTc, h_hTc = hTb[sbi % 2], h_hTb[sbi % 2]
                for kc in range(8):
                    mm(PS[4], wvd[:, kc, :], hTc[:, kc, :], start=(kc == 0), stop=(kc == 7), r=[h_wvd, h_hTc], bank=4)
                act(vT_sb, PS[4], AF.Copy, r=[], w=[PH[4], h_vT_sb])
                pvv = PS[5].bitcast(BF16)
                for t in range(4):
                    k.op("pe", lambda e: e.transpose(pvv[:, t * 128:(t + 1) * 128], vT_sb[:, t * 128:(t + 1) * 128], ident_b),
                         r=[h_vT_sb, h_ident_b], w=[PH[5]])
                k.op("dve", lambda e: e.tensor_copy(Vg[:, sbi * 4:sbi * 4 + 4, 0:128],
                                                    pvv[:, 0:512].rearrange("p (t e) -> p t e", t=4)), w=[PH[5], h_V])

            proj_a(0)
            for u in range(len(units_p)):
                sbi_u, which_u = units_p[u]
                last_of_sb = (u + 1 == len(units_p)) or (units_p[u + 1][0] != sbi_u)
                if last_of_sb:
                    proj_v(sbi_u)
                if u + 1 < len(units_p):
                    proj_a(u + 1)
                proj_b(u)
            k._wait("pool", [], [h_QT.w, h_V.w])
            for dst_, src_ in cast_jobs:
                k.dma("pool", dst_, src_)
            SB = (0, 1, 6)
            LOOK = 2
            pidx = 0
            stream = []
            for j_ in range(8):
                for m_ in range(2):
                    for g_ in range(32 + 4 * j_ + 4):
                        stream.append((j_, m_, g_))

            def emit_S(si):
                j_, m_, g_ = stream[si]
                sbank_ = SB[si % 3]
                mm(PS[sbank_], KT[:, g_ * 128:(g_ + 1) * 128], QTz[m_][:, j_ * 512:(j_ + 1) * 512],
                   start=True, stop=True, r=[h_KT, h_QT], bank=sbank_)

            for si in range(LOOK):
                emit_S(si)
            sbase = 0
            prev_pv = [None]
            for j in range(8):
                for m in range(2):
                    hm = 2 * hd + m
                    ob = (2, 3) if m == 0 else (4, 5)
                    nkeys = 32 + 4 * j + 4
                    base = sbase
                    sbase += nkeys
                    for g in range(nkeys):
                        if g == 12 and pending:
                            pending.pop(0)()
                        if base + g + LOOK < len(stream):
                            emit_S(base + g + LOOK)
                        sbank = SB[(base + g) % 3]
                        pb = pidx % 4
                        pidx += 1
                        tile_id = None
                        qb_min = 0
                        if g == 31 and j == 0:
                            tile_id = 5
                        elif j > 0 and g == 32 + 4 * j - 1:
                            tile_id = 4
                        elif g >= 32 + 4 * j:
                            tile_id = g - (32 + 4 * j)
                            qb_min = tile_id
                        if tile_id is None:
                            bcol, h_bcol = (cP, h_cP) if g < 32 else (cF, h_cF)
                            act(PT[pb], PS[sbank], AF.Exp, r=[h_bcol], w=[PH[sbank], h_PT[pb]],
                                bias=bcol[:, hm:hm + 1])
                        else:
                            st_, h_st = stmp[g % 2], h_stmp[g % 2]
                            k.op("dve", lambda e: e.scalar_tensor_tensor(out=st_, in0=PS[sbank], scalar=-SHIFT,
                                                                         in1=bt[:, m, tile_id, :], op0=ALU.add, op1=ALU.add),
                                 r=[h_bt], w=[PH[sbank], h_st])
                            act(PT[pb], st_, AF.Exp, r=[h_st], w=[h_PT[pb]])
                        def pv_closure(g=g, pb=pb, qb_min=qb_min, ob=ob, nkeys=nkeys):
                            for qb in range(qb_min, 4):
                                bnk = ob[0] if qb < 3 else ob[1]
                                c0 = (qb % 3) * 130
                                first_in_bank = (g == 0) and (qb == 0 or qb == 3)
                                mm(PS[bnk][:, c0:c0 + 129], PT[pb][:, qb * 128:(qb + 1) * 128], Vg[:, g, 0:129],
                                   start=first_in_bank, stop=(g == nkeys - 1), r=[h_PT[pb], h_V], bank=bnk, skip=True)

                        if prev_pv[0] is not None:
                            prev_pv[0]()
                        prev_pv[0] = pv_closure
                        if g == nkeys - 1:
                            prev_pv[0]()
                            prev_pv[0] = None
                    for qb in range(4):
                        bnk = ob[0] if qb < 3 else ob[1]
                        c0 = (qb % 3) * 130
                        k.op("dve", lambda e: e.reciprocal(rl[:, qb:qb + 1], PS[bnk][:, c0 + 128:c0 + 129]),
                             w=[PH[bnk], h_rl])
                        if m == 0:
                            k.op("dve", lambda e: e.tensor_scalar(on0[:, qb, :], PS[bnk][:, c0:c0 + 128],
                                                                  rl[:, qb:qb + 1], None, ALU.mult),
                                 r=[h_rl], w=[PH[bnk], h_on0])
                        else:
                            k.op("dve", lambda e: e.tensor_scalar(nrl[:, qb:qb + 1], rl[:, qb:qb + 1], neglam, None,
                                                                  ALU.mult), r=[h_rl, h_neglam], w=[h_nrl])
                            k.op("dve", lambda e: e.scalar_tensor_tensor(out=od2[j % 2][:, qb, :], in0=PS[bnk][:, c0:c0 + 128],
                                                                         scalar=nrl[:, qb:qb + 1], in1=on0[:, qb, :],
                                                                         op0=ALU.mult, op1=ALU.add),
                                 r=[h_nrl, h_on0], w=[PH[bnk], h_od2[j % 2]])
                    if m == 1:
                        def epilogue(j=j, hd=hd):
                            for qb in range(4):
                                k.op("dve", lambda e: e.tensor_tensor(out=sqd, in0=od2[j % 2][:, qb, :], in1=od2[j % 2][:, qb, :], op=ALU.mult),
                                     r=[h_od2[j % 2]], w=[h_sqd])
                                k.op("dve", lambda e: e.tensor_reduce(out=ssd, in_=sqd, axis=AX.X, op=ALU.add),
                                     r=[h_sqd], w=[h_ssd])
                                rstd_from_ss(rsd, ssd, 1.0 / 128.0, h_ssd, h_rsd, sdd, h_sdd)
                                k.op("dve", lambda e: e.scalar_tensor_tensor(out=yb4[qb], in0=od2[j % 2][:, qb, :], scalar=rsd, in1=gsub8,
                                                                             op0=ALU.mult, op1=ALU.mult),
                                     r=[h_od2[j % 2], h_rsd, h_gsub8], w=[h_yb4[qb]])
                            pv = PS[7].bitcast(BF16)
                            for qb in range(4):
                                k.op("pe", lambda e: e.transpose(pv[:, qb * 128:(qb + 1) * 128], yb4[qb], ident_b),
                                     r=[h_yb4[qb], h_ident_b], w=[PH[7]])
                            q0 = j * 512
                            k.op("dve", lambda e: e.tensor_copy(mixT[:, 4 + hd, q0:q0 + 512], pv[:, 0:512]),
                                 w=[PH[7], h_mixT])
                        pending.append(epilogue)
            while pending:
                pending.pop(0)()
        k.barrier()

    with ExitStack() as es:
        wo_f = sb(es, "wo_f", [128, 8, D]); h_wo_f = H()
        wo = sb(es, "wo", [128, 8, D], BF16); h_wo = H()
        wr_f = sb(es, "wr_f", [128, 8, E]); h_wr_f = H()
        gt1_bc = sb(es, "gt1_bc", [128, D]); h_gt1 = H()
        a2_bc = sb(es, "a2_bc", [128, D]); h_a2bc = H()
        sh2_bc = sb(es, "sh2_bc", [128, D]); h_sh2bc = H()
        k.dma("sp", gt1_bc, modbc_d[0], w=[h_gt1])
        k.dma("sp", a2_bc, modbc_d[1], w=[h_a2bc])
        k.dma("sp", sh2_bc, modbc_d[2], w=[h_sh2bc])
        k.dma("sp", wo_f, w_out_d.rearrange("(kc p) n -> p kc n", p=128), w=[h_wo_f])
        k.dma("sp", wr_f, w_router_d.rearrange("(kc p) n -> p kc n", p=128), w=[h_wr_f])
        for kc in range(8):
            k.op("dve", lambda e: e.tensor_tensor(out=wo[:, kc, :], in0=wo_f[:, kc, :], in1=gt1_bc, op=ALU.mult),
                 r=[h_wo_f, h_gt1], w=[h_wo])
        xb = [sb(es, f"p4_x{i}", [128, D]) for i in range(2)]; h_xb = [H(), H()]
        x1 = [sb(es, f"p4_x1{i}", [128, D]) for i in range(2)]; h_x1 = [H(), H()]
        sq = sb(es, "p4_sq", [128, D], BF16); h_sq = H()
        ss = sb(es, "p4_ss", [128, 1]); h_ss = H()
        sd = sb(es, "p4_sd", [128, 1]); h_sd = H()
        rs = sb(es, "p4_rs", [128, 1]); h_rs = H()
        h2t = sb(es, "p4_h2t", [128, D]); h_h2t = H()
        h2tb = [sb(es, f"p4_h2tb{i}", [128, D], BF16) for i in range(2)]; h_h2tb = [H(), H()]
        h2f = sb(es, "p4_h2f", [128, 8, 128]); h_h2f = H()
        nmx = sb(es, "p4_nmx", [128, 1]); h_nmx = H()
        ex = sb(es, "p4_ex", [128, E]); h_ex = H()
        exm = sb(es, "p4_exm", [128, E]); h_exm = H()
        den = sb(es, "p4_den", [128, 1]); h_den = H()
        rden = sb(es, "p4_rden", [128, 1]); h_rden = H()
        def p4_stage1(t):
            b2 = t % 2
            tok = slice(t * 128, (t + 1) * 128)
            k.dma("sp", xb[b2], x_own[tok, :], w=[h_xb[b2]])
            for half in range(2):
                for kc in range(8):
                    mm(PS[half], mixT[:, kc, tok], wo[:, kc, half * 512:(half + 1) * 512], start=(kc == 0), stop=(kc == 7),
                       r=[h_mixT, h_wo], bank=half)
                k.op("dve", lambda e: e.tensor_tensor(out=x1[b2][:, half * 512:(half + 1) * 512], in0=PS[half],
                                                      in1=xb[b2][:, half * 512:(half + 1) * 512], op=ALU.add),
                     r=[h_xb[b2]], w=[PH[half], h_x1[b2]])
            k.dma("pool", x1_d[tok, :], x1[b2], r=[h_x1[b2]])

        h2t2 = [h2t, sb(es, "p4_h2t_b", [128, D])]; h_h2t2 = [h_h2t, H()]

        def p4_stage2a(t):
            b2 = t % 2
            tok = slice(t * 128, (t + 1) * 128)
            ht, h_ht = h2t2[b2], h_h2t2[b2]
            act(sq, x1[b2], AF.Square, r=[h_x1[b2]], w=[h_sq])
            k.op("dve", lambda e: e.tensor_reduce(out=ss, in_=sq, axis=AX.X, op=ALU.add), r=[h_sq], w=[h_ss])
            rstd_from_ss(rs, ss, 1.0 / D, h_ss, h_rs, sd, h_sd)
            k.op("dve", lambda e: e.scalar_tensor_tensor(out=ht, in0=x1[b2], scalar=rs, in1=a2_bc, op0=ALU.mult,
                                                         op1=ALU.mult), r=[h_x1[b2], h_rs, h_a2bc], w=[h_ht])
            k.op("dve", lambda e: e.tensor_tensor(out=ht, in0=ht, in1=sh2_bc, op=ALU.add),
                 r=[h_ht, h_sh2bc], w=[h_ht])
            act(h2tb[b2], ht, AF.Copy, r=[h_ht], w=[h_h2tb[b2]])
            k.dma("pool", h2tm_d[tok, :], h2tb[b2], r=[h_h2tb[b2]])

        def p4_stage2b(t):
            b2 = t % 2
            ht, h_ht = h2t2[b2], h_h2t2[b2]
            for kc in range(8):
                bank = 2 + kc // 4
                k.op("pe", lambda e: e.transpose(PS[bank][:, (kc % 4) * 128:(kc % 4 + 1) * 128],
                                                 ht[:, kc * 128:(kc + 1) * 128], ident_f),
                     r=[h_ht, h_ident_f], w=[PH[bank]])
            for hf in range(2):
                act(h2f[:, hf * 4:(hf + 1) * 4, :], PS[2 + hf].rearrange("p (k t) -> p k t", k=4), AF.Copy, r=[],
                    w=[PH[2 + hf], h_h2f])
            for kc in range(8):
                mm(PS[4][:, 0:E], h2f[:, kc, :], wr_f[:, kc, :], start=(kc == 0), stop=(kc == 7), r=[h_h2f, h_wr_f], bank=4)
            lg = lg_all[:, t, :]
            top8 = top8_all[:, t, :]
            msk = msk_all[:, t, :]
            k.op("dve", lambda e: e.tensor_tensor(out=lg, in0=PS[4][:, 0:E], in1=b_router, op=ALU.add),
                 r=[h_b_router], w=[PH[4], h_lg_all])
            k.op("dve", lambda e: e.max(out=top8, in_=lg), r=[h_lg_all], w=[h_top8_all])
            k.op("dve", lambda e: e.tensor_scalar(nmx, top8[:, 0:1], -1.0, None, ALU.mult), r=[h_top8_all], w=[h_nmx])
            k.op("dve", lambda e: e.tensor_scalar(msk, lg, top8[:, 3:4], None, ALU.is_ge), r=[h_lg_all, h_top8_all],
                 w=[h_msk_all])
            act(ex, lg, AF.Exp, r=[h_lg_all, h_nmx], w=[h_ex], bias=nmx)
            k.op("dve", lambda e: e.tensor_tensor(out=exm, in0=ex, in1=msk, op=ALU.mult), r=[h_ex, h_msk_all], w=[h_exm])
            k.op("dve", lambda e: e.tensor_reduce(out=den, in_=exm, axis=AX.X, op=ALU.add), r=[h_exm], w=[h_den])
            k.op("dve", lambda e: e.reciprocal(rden, den), r=[h_den], w=[h_rden])
            k.op("dve", lambda e: e.tensor_scalar(wts[:, t, :], exm, rden, None, ALU.mult), r=[h_exm, h_rden], w=[h_wts])

        p4_stage1(0)
        p4_stage2a(0)
        for t in range(NT_OWN):
            if t + 1 < NT_OWN:
                p4_stage1(t + 1)
            p4_stage2b(t)
            if t + 1 < NT_OWN:
                p4_stage2a(t + 1)
        k.barrier()
    mix_es.close()

    with ExitStack() as es:
        cnt = sb(es, "r_cnt", [128, E]); h_cnt = H()
        nblk = sb(es, "r_nblk", [128, E]); h_nblk = H()
        ones32 = sb(es, "r_ones32", [128, E]); h_ones32 = H()
        cs = sb(es, "r_cs", [128, E]); h_cs = H()
        pstart = sb(es, "r_pstart", [128, E]); h_pstart = H()
        be = sb(es, "r_be", [128, NBLK]); h_be = H()
        bidxf = sb(es, "r_bidxf", [128, 3, NBLK]); h_bidxf = H()
        dest = sb(es, "r_dest", [128, E]); h_dest = H()
        eq = sb(es, "r_eq", [128, E]); h_eq = H()
        pr = sb(es, "r_pr", [128, E]); h_pr = H()
        h2r = [sb(es, f"r_h2r{i}", [128, D], BF16) for i in range(3)]; h_h2r = [H() for _ in range(3)]
        for t in range(NT_OWN):
            mm(PS[0][:, 0:E], ones_f, msk_all[:, t, :], start=(t == 0), stop=(t == NT_OWN - 1), r=[h_ones_f, h_msk_all], bank=0)
        k.op("dve", lambda e: e.tensor_copy(cnt, PS[0][:, 0:E]), w=[PH[0], h_cnt])
        k.op("dve", lambda e: e.memset(nblk, 0.0), w=[h_nblk])
        k.op("dve", lambda e: e.memset(ones32, 1.0), w=[h_ones32])
        for j in range(HALF // MOE_B):
            k.op("dve", lambda e: e.scalar_tensor_tensor(out=nblk, in0=cnt, scalar=float(j * MOE_B), in1=nblk,
                                                         op0=ALU.is_gt, op1=ALU.add), r=[h_cnt, h_nblk], w=[h_nblk])
        k.op("dve", lambda e: e.tensor_tensor_scan(out=cs, data0=ones32, data1=nblk, initial=0.0, op0=ALU.mult,
                                                   op1=ALU.add), r=[h_ones32, h_nblk], w=[h_cs])
        k.op("dve", lambda e: e.tensor_tensor(out=pstart, in0=cs, in1=nblk, op=ALU.subtract), r=[h_cs, h_nblk], w=[h_pstart])
        k.op("dve", lambda e: e.tensor_scalar(pstart, pstart, float(MOE_B), None, ALU.mult), r=[h_pstart], w=[h_pstart])
        k.op("dve", lambda e: e.memset(be, 0.0), w=[h_be])
        for ex_ in range(E):
            k.op("dve", lambda e: e.scalar_tensor_tensor(out=be, in0=iota64, scalar=cs[:, ex_:ex_ + 1], in1=be,
                                                         op0=ALU.is_ge, op1=ALU.add), r=[h_iota64, h_cs, h_be], w=[h_be])
        k.op("dve", lambda e: e.tensor_scalar(bidxf[:, 0, :], be, 1024.0, lane_id, ALU.mult, ALU.add), r=[h_be, h_pidx], w=[h_bidxf])
        k.op("dve", lambda e: e.tensor_scalar(bidxf[:, 1, :], be, 128.0, lane_id, ALU.mult, ALU.add), r=[h_be, h_pidx], w=[h_bidxf])
        k.op("dve", lambda e: e.tensor_copy(bidxf[:, 2, :], be), r=[h_be], w=[h_bidxf])
        k.op("dve", lambda e: e.tensor_copy(blk_idx, bidxf.rearrange("p a b -> p (a b)")), r=[h_bidxf], w=[h_blk_idx])
        dest_all = sb(es, "r_dest_all", [128, NT_OWN, E]); h_dest_all = H()
        eq_all = sb(es, "r_eq_all", [128, NT_OWN * 4, E]); h_eq_all = H()
        pr_all = sb(es, "r_pr_all", [128, NT_OWN * 4, E]); h_pr_all = H()
        base_all = sb(es, "r_base_all", [128, NT_OWN, E]); h_base_all = H()
        for t in range(NT_OWN):
            mm(PS[1 + t // 16][:, (t % 16) * E:(t % 16 + 1) * E], ustrict, msk_all[:, t, :], start=(t % 16 == 0), stop=True,
               r=[h_ustrict, h_msk_all], bank=1 + t // 16, skip=True)
        for t in range(NT_OWN):
            mm(PS[3 + t // 16][:, (t % 16) * E:(t % 16 + 1) * E], ones_f, msk_all[:, t, :], start=(t % 16 == 0), stop=True,
               r=[h_ones_f, h_msk_all], bank=3 + t // 16, skip=True)
        k.op("dve", lambda e: e.tensor_copy(base_all[:, 0, :], pstart), r=[h_pstart], w=[h_base_all])
        for t in range(1, NT_OWN):
            tp = t - 1
            k.op("dve", lambda e: e.tensor_tensor(out=base_all[:, t, :], in0=PS[3 + tp // 16][:, (tp % 16) * E:(tp % 16 + 1) * E],
                                                  in1=base_all[:, t - 1, :], op=ALU.add), r=[h_base_all],
                 w=[PH[3 + tp // 16], h_base_all])
        for hf in range(2):
            k.op("dve", lambda e: e.tensor_tensor(out=dest_all[:, hf * 16:(hf + 1) * 16, :],
                                                  in0=PS[1 + hf].rearrange("p (t e) -> p t e", e=E),
                                                  in1=base_all[:, hf * 16:(hf + 1) * 16, :], op=ALU.add),
                 r=[h_base_all], w=[PH[1 + hf], h_dest_all])
        for t in range(NT_OWN):
            for kk in range(4):
                k.op("dve", lambda e: e.tensor_scalar(eq_all[:, 4 * t + kk, :], lg_all[:, t, :], top8_all[:, t, kk:kk + 1], None,
                                                      ALU.is_equal), r=[h_lg_all, h_top8_all], w=[h_eq_all])
        for t in range(NT_OWN):
            for kk in range(4):
                k.op("dve", lambda e: e.tensor_tensor(out=pr_all[:, 4 * t + kk, :], in0=eq_all[:, 4 * t + kk, :],
                                                      in1=dest_all[:, t, :], op=ALU.mult), r=[h_eq_all, h_dest_all], w=[h_pr_all])
        k.op("dve", lambda e: e.tensor_reduce(out=idxf_all.rearrange("p a b -> p (a b)"), in_=pr_all, axis=AX.X, op=ALU.add),
             r=[h_pr_all], w=[h_idxf_all])
        for t in range(NT_OWN):
            for kk in range(4):
                k.op("dve", lambda e: e.tensor_tensor(out=pr_all[:, 4 * t + kk, :], in0=eq_all[:, 4 * t + kk, :],
                                                      in1=wts[:, t, :], op=ALU.mult), r=[h_eq_all, h_wts, h_idxf_all], w=[h_pr_all])
        k.op("dve", lambda e: e.tensor_reduce(out=wk_all.rearrange("p a b -> p (a b)"), in_=pr_all, axis=AX.X, op=ALU.add),
             r=[h_pr_all], w=[h_wk_all])
        k.op("dve", lambda e: e.tensor_copy(idx_all, idxf_all.rearrange("p a b -> p (a b)")), r=[h_idxf_all], w=[h_idx_all])
        if DEBUG:
            k.dma("sp", dbg["cnt"], cnt, r=[h_cnt])
            k.dma("sp", dbg["cs"], cs, r=[h_cs])
            k.dma("sp", dbg["pstart"], pstart, r=[h_pstart])
            k.dma("sp", dbg["be"], be, r=[h_be])
            k.dma("sp", dbg["idxf"], idxf_all.rearrange("p a b -> p (a b)"), r=[h_idxf_all])
            k.dma("sp", dbg["wk"], wk_all.rearrange("p a b -> p (a b)"), r=[h_wk_all])
            k.dma("sp", dbg["lg"], lg_all.rearrange("p a b -> p (a b)"), r=[h_lg_all])
            k.dma("sp", dbg["wts"], wts.rearrange("p a b -> p (a b)"), r=[h_wts])
            k.dma("sp", dbg["msk"], msk_all.rearrange("p a b -> p (a b)"), r=[h_msk_all])
        h_xs = H()
        reg_bounds = nc.gpsimd.to_reg(PSLOTS - 1)
        reg_bw = nc.gpsimd.to_reg(E * D - 1)
        reg_bb = nc.gpsimd.to_reg(E * 128 - 1)
        reg_be = nc.gpsimd.to_reg(E - 1)
        for t in range(NT_OWN):
            b3 = t % 3
            k.dma("sp", h2r[b3], h2tm_d[t * 128:(t + 1) * 128, :], w=[h_h2r[b3]])
            for kk in range(4):
                k._wait("pool", [], [h_h2r[b3].w, h_idx_all.w])
                d = k.dq["pool"]
                i = d["rr"]; d["rr"] = (i + 1) % NQ
                if d["cnt"][i] > 0:
                    k._wait("pool", [(d["sems"][i], 16 * d["cnt"][i])])
                inst = nc.gpsimd.indirect_dma_start(
                    out=xs_d, out_offset=bass.IndirectOffsetOnAxis(ap=idx_all[:, t * 4 + kk:t * 4 + kk + 1], axis=0),
                    in_=h2r[b3], in_offset=None, bounds_check=reg_bounds, oob_is_err=False)
                d["cnt"][i] += 1
                inst.then_inc(k.sems[d["sems"][i]], 16)
                ev = (d["sems"][i], 16 * d["cnt"][i])
                k._mark(ev, [h_h2r[b3], h_idx_all], [h_xs])
        k.barrier()

    def igather(out, src, idx_ap, r, w, element_offset=0, bounds=None):
        d = k.dq["pool"]
        i = d["rr"]; d["rr"] = (i + 1) % NQ
        evs = k._deps(r, w, "pool")
        if d["cnt"][i] > 0:
            evs.append((d["sems"][i], 16 * d["cnt"][i]))
        k._wait("pool", evs, k._raw(r))
        kw = {}
        if bounds is not None:
            kw = {"bounds_check": bounds, "oob_is_err": False}
        inst = nc.gpsimd.indirect_dma_start(out=out, out_offset=None, in_=src,
                                            in_offset=bass.IndirectOffsetOnAxis(ap=idx_ap, axis=0),
                                            element_offset=element_offset, **kw)
        d["cnt"][i] += 1
        inst.then_inc(k.sems[d["sems"][i]], 16)
        ev = (d["sems"][i], 16 * d["cnt"][i])
        k._mark(ev, r, w)
        return ev

    w_gu_rows = wgu_bf_d
    w_dn_rows = wdn_bf_d

    NSUB = MOE_B // 128
    with ExitStack() as es:
        wgu = [sb(es, f"m_wgu{i}", [128, 8, 2 * D], BF16) for i in range(2)]; h_wgu = [[H() for _ in range(8)] for _ in range(2)]
        wdn = [sb(es, f"m_wdn{i}", [128, 8, D], BF16) for i in range(2)]; h_wdn = [[H() for _ in range(8)] for _ in range(2)]
        bgu = [sb(es, f"m_bgu{i}", [128, 16]) for i in range(2)]; h_bgu = [H(), H()]
        bgu1 = [sb(es, f"m_bgu1{i}", [128, 8]) for i in range(2)]; h_bgu1 = [H(), H()]
        bdn = [sb(es, f"m_bdn{i}", [128, D]) for i in range(2)]; h_bdn = [H(), H()]
        xsb = sb(es, "m_xs", [128, NSUB, D], BF16); h_xsb = H()
        xT = [sb(es, f"m_xT{i}", [128, 8, MOE_B], BF16) for i in range(2)]; h_xT = [H(), H()]; h_xTd = [H(), H()]
        yT = [sb(es, f"m_yT{i}", [128, 8, MOE_B], BF16) for i in range(2)]; h_yT = [H(), H()]
        gate = [sb(es, f"m_gate{i}", [128, MOE_B]) for i in range(2)]; h_gate = [H(), H()]
        sig = [sb(es, f"m_sig{i}", [128, MOE_B]) for i in range(2)]; h_sig = [H(), H()]
        upc = [sb(es, f"m_upc{i}", [128, MOE_B]) for i in range(2)]; h_upc = [H(), H()]
        gs = [sb(es, f"m_gs{i}", [128, MOE_B]) for i in range(2)]; h_gs = [H(), H()]
        ysb = [sb(es, f"m_ys{i}", [128, D]) for i in range(2)]; h_ysb = [H(), H()]
        h_ys = H()

        def load_block_weights(bi):
            sl = bi % 2
            ib = blk_idx[:, NBLK + bi:NBLK + bi + 1]
            igather(wgu[sl].rearrange("p k f -> p (k f)"), w_gu_rows, ib, r=[h_blk_idx], w=h_wgu[sl], bounds=reg_bb)
            igather(wdn[sl].rearrange("p k f -> p (k f)"), w_dn_rows, ib, r=[h_blk_idx], w=h_wdn[sl], bounds=reg_bb)
            igather(bgu[sl], b_gu_rows_d, blk_idx[:, NBLK + bi:NBLK + bi + 1], r=[h_blk_idx], w=[h_bgu[sl]], bounds=reg_bb)
            igather(bdn[sl], b_dn_d, blk_idx[:, 2 * NBLK + bi:2 * NBLK + bi + 1], r=[h_blk_idx], w=[h_bdn[sl]], bounds=reg_be)

        xsb2 = [xsb, sb(es, "m_xs2", [128, NSUB, D], BF16)]; h_xsb2 = [h_xsb, H()]

        def load_xs(bi):
            sl = bi % 2
            k.dma("pool", xsb2[sl], xs_d[bi * MOE_B:(bi + 1) * MOE_B, :].rearrange("(s p) d -> p s d", p=128), w=[h_xsb2[sl]])

        def transposes(bi):
            sl = bi % 2
            for s4 in range(NSUB):
                bank = 4 + s4 % 2
                pv = PS[bank].bitcast(BF16)
                for kc in range(8):
                    k.op("pe", lambda e: e.transpose(pv[:, kc * 128:(kc + 1) * 128], xsb2[sl][:, s4, kc * 128:(kc + 1) * 128], ident_b),
                         r=[h_xsb2[sl], h_ident_b], w=[PH[bank]])
                if s4 % 2 == 0:
                    act(xT[sl][:, :, s4 * 128:(s4 + 1) * 128], pv.rearrange("p (k t) -> p k t", k=8), AF.Copy, r=[],
                        w=[PH[bank], h_xT[sl]])
                else:
                    k.op("dve", lambda e: e.tensor_copy(xT[sl][:, :, s4 * 128:(s4 + 1) * 128], pv.rearrange("p (k t) -> p k t", k=8)),
                         w=[PH[bank], h_xTd[sl]])

        load_block_weights(0)
        load_xs(0)
        transposes(0)
        it = 0
        ysi = 0
        for bi in range(NBLK):
            sl = bi % 2
            if bi + 1 < NBLK:
                load_block_weights(bi + 1)
                load_xs(bi + 1)
            k.op("dve", lambda e: e.tensor_scalar(bgu1[sl], bgu[sl][:, 8:16], 1.0, None, ALU.add), r=[h_bgu[sl]], w=[h_bgu1[sl]])
            for ft in range(8):
                i2 = it % 2
                it += 1
                gb, ub = (0, 1) if i2 == 0 else (2, 3)
                for kc in range(8):
                    mm(PS[gb][:, 0:MOE_B], wgu[sl][:, kc, ft * 128:(ft + 1) * 128], xT[sl][:, kc, :], start=(kc == 0), stop=(kc == 7),
                       r=[h_wgu[sl][kc], h_xT[sl], h_xTd[sl]], bank=gb)
                for kc in range(8):
                    mm(PS[ub][:, 0:MOE_B], wgu[sl][:, kc, D + ft * 128:D + (ft + 1) * 128], xT[sl][:, kc, :], start=(kc == 0),
                       stop=(kc == 7), r=[h_wgu[sl][kc], h_xT[sl], h_xTd[sl]], bank=ub)
                k.op("dve", lambda e: e.tensor_scalar(gate[i2], PS[gb][:, 0:MOE_B], bgu[sl][:, ft:ft + 1], 7.0, ALU.add, ALU.min),
                     r=[h_bgu[sl]], w=[PH[gb], h_gate[i2]])
                act(sig[i2], gate[i2], AF.Sigmoid, r=[h_gate[i2]], w=[h_sig[i2]], scale=1.702)
                k.op("dve", lambda e: e.tensor_scalar(upc[i2], PS[ub][:, 0:MOE_B], bgu1[sl][:, ft:ft + 1], 8.0, ALU.add, ALU.min),
                     r=[h_bgu1[sl]], w=[PH[ub], h_upc[i2]])
                k.op("dve", lambda e: e.tensor_tensor(out=gs[i2], in0=gate[i2], in1=sig[i2], op=ALU.mult),
                     r=[h_gate[i2], h_sig[i2]], w=[h_gs[i2]])
                k.op("dve", lambda e: e.scalar_tensor_tensor(out=yT[sl][:, ft, :], in0=upc[i2], scalar=-6.0, in1=gs[i2],
                                                             op0=ALU.max, op1=ALU.mult), r=[h_gs[i2], h_upc[i2]], w=[h_yT[sl]])
            if bi + 1 < NBLK:
                transposes(bi + 1)
            for s4 in range(NSUB):
                yb2 = ysi % 2
                ysi += 1
                for half in range(2):
                    bank = 6 + half
                    for fc in range(8):
                        mm(PS[bank], yT[sl][:, fc, s4 * 128:(s4 + 1) * 128], wdn[sl][:, fc, half * 512:(half + 1) * 512],
                           start=(fc == 0), stop=False, r=[h_yT[sl], h_wdn[sl][fc]], bank=bank)
                    mm(PS[bank], ones_row, bdn[sl][0:1, half * 512:(half + 1) * 512], start=False, stop=True,
                       r=[h_ones_row, h_bdn[sl]], bank=bank)
                    act(ysb[yb2][:, half * 512:(half + 1) * 512], PS[bank], AF.Copy, r=[], w=[PH[bank], h_ysb[yb2]])
                r0 = bi * MOE_B + s4 * 128
                k.dma("sp", ys_d[r0:r0 + 128, :], ysb[yb2], r=[h_ysb[yb2]], w=[h_ys])
        k.barrier()

    with ExitStack() as es:
        gr = [[sb(es, f"c_g{i}_{kk}", [128, D]) for kk in range(4)] for i in range(2)]
        h_gr = [[H() for _ in range(4)] for _ in range(2)]
        x1b = [sb(es, f"c_x1{i}", [128, D]) for i in range(2)]; h_x1b = [H(), H()]
        acc = [sb(es, f"c_acc{i}", [128, D]) for i in range(2)]; h_acc = [H(), H()]
        for t in range(NT_OWN):
            b2 = t % 2
            tok = slice(t * 128, (t + 1) * 128)
            k.dma("sp", x1b[b2], x1_d[tok, :], w=[h_x1b[b2]])
            for kk in range(4):
                igather(gr[b2][kk], ys_d, idx_all[:, t * 4 + kk:t * 4 + kk + 1], r=[h_idx_all, h_ys], w=[h_gr[b2][kk]], bounds=reg_bounds)
            k.op("dve", lambda e: e.tensor_scalar(acc[b2], gr[b2][0], wk_all[:, t, 0:1], None, ALU.mult),
                 r=[h_gr[b2][0], h_wk_all], w=[h_acc[b2]])
            for kk in range(1, 4):
                k.op("dve", lambda e: e.scalar_tensor_tensor(out=acc[b2], in0=gr[b2][kk], scalar=wk_all[:, t, kk:kk + 1],
                                                             in1=acc[b2], op0=ALU.mult, op1=ALU.add),
                     r=[h_gr[b2][kk], h_wk_all, h_acc[b2]], w=[h_acc[b2]])
            k.op("dve", lambda e: e.tensor_tensor(out=acc[b2], in0=acc[b2], in1=gt2_bc, op=ALU.mult),
                 r=[h_acc[b2], h_gt2], w=[h_acc[b2]])
            k.op("dve", lambda e: e.tensor_tensor(out=acc[b2], in0=acc[b2], in1=x1b[b2], op=ALU.add),
                 r=[h_acc[b2], h_x1b[b2]], w=[h_acc[b2]])
            k.dma("sp", out_d[tok, :], acc[b2], r=[h_acc[b2]])
        k.barrier()
    k.final_wait()
    top.close()
    return nc


def _t5_bucket(n):
    max_exact = 16
    nf = np.maximum(n, 1).astype(np.float32)
    large = max_exact + (np.log(nf / np.float32(max_exact)) / np.float32(math.log(128 / 16))
                         * np.float32(32 - max_exact)).astype(np.int32)
    large = np.minimum(large, 31)
    return np.where(n < max_exact, n, large)


def _bias_tiles(table, half):
    tiles = np.empty((8, 6, 128, 512), np.float32)
    kk = np.arange(128)[:, None]
    qq = np.arange(512)[None, :]
    for i in range(4):
        d = qq - (i * 128 + kk)
        b = _t5_bucket(np.maximum(d, 0))
        for hm in range(8):
            tiles[hm, i] = np.where(d >= 0, table[b, hm], np.float32(NEG))
    d = qq + 128 - kk
    b = _t5_bucket(d)
    for hm in range(8):
        tiles[hm, 4] = table[b, hm]
        tiles[hm, 5] = table[b, hm] if half == 1 else np.float32(NEG)
    return tiles


_NC_CACHE = {}


def kernel(x, c, rel_bias_table, w_ada, b_ada, g_norm1, w_in, w_gk_up, b_gk_up, g_gla_out, g_qnorm, g_knorm,
           lambda_q1, lambda_k1, lambda_q2, lambda_k2, g_subln, w_out, g_norm2, w_router, b_router,
           w_gate_up, b_gate_up, w_down, b_down):
    f = lambda a: np.ascontiguousarray(np.asarray(a, dtype=np.float32))
    x, c, table = f(x), f(c), f(rel_bias_table)
    w_ada, b_ada, w_in = f(w_ada)[0], f(b_ada)[0], f(w_in)[0]
    w_gk_up, b_gk_up = f(w_gk_up)[0], f(b_gk_up)[0]
    w_out, w_router, b_router = f(w_out)[0], f(w_router)[0], f(b_router)[0]
    w_gate_up, b_gate_up, w_down, b_down = f(w_gate_up)[0], f(b_gate_up)[0], f(w_down)[0], f(b_down)[0]
    colT = lambda v: f(v.reshape(-1, 128).T)
    rep = lambda v, n=128: f(np.broadcast_to(v[None, :], (n, v.shape[0])))

    ii = np.arange(128)
    shared = {
        "b_adaT": colT(b_ada), "b_ada_row": f(b_ada[None, :]),
        "g1T": colT(f(g_norm1)[0]), "g2T": colT(f(g_norm2)[0]),
        "w_ada": w_ada, "w_in": w_in, "w_gk_up": w_gk_up,
        "b_gkT": colT(b_gk_up), "b_gk_bc": rep(b_gk_up),
        "ggla_col": f(f(g_gla_out)[0][:, None]),
        "gq_col": f(np.tile(f(g_qnorm)[0], 2)[:, None]), "gk_col": f(np.tile(f(g_knorm)[0], 2)[:, None]),
        "lam4": f(np.broadcast_to(np.stack([f(lambda_q1)[0], f(lambda_k1)[0], f(lambda_q2)[0], f(lambda_k2)[0]])[None],
                                  (128, 4, 64))),
        "gsub_bc": rep(f(g_subln)[0]),
        "w_out": w_out, "w_router": w_router, "b_router_bc": rep(b_router),
        "w_gate_up": w_gate_up, "b_gu_rows": f(b_gate_up.reshape(E, 16, 128).transpose(0, 2, 1).reshape(E * 128, 16)),
        "g2_bc": rep(f(g_norm2)[0]),
        "ustrict": (ii[:, None] < ii[None, :]).astype(np.float32),
        "ones_f": np.ones((128, 128), np.float32),
        "iota64": f(np.broadcast_to(np.arange(NBLK, dtype=np.float32)[None, :], (128, NBLK))),
        "pidx": f(ii.astype(np.float32)[:, None]),
        "w_down": w_down, "b_down": b_down,
        "cF": rep(table[31]),
        "ident": np.eye(128, dtype=np.float32),
        "maskT": (ii[:, None] <= ii[None, :]).astype(np.float32),
        "Lmat": np.where(ii[:, None] > ii[None, :], np.float32(-1.0 / 16.0), np.float32(0.0)).astype(np.float32),
        "keep": f(np.tile(np.where(ii == 0, 0.0, 1.0).astype(np.float32), 4)[None, :].repeat(128, 0)),
        "blk64": f(np.kron(np.eye(2, dtype=np.float32), np.full((64, 64), 1.0 / 64.0, np.float32))),
        "ones128": np.full((128, 128), 1.0 / 128.0, np.float32),
        "ones_row": np.ones((1, 128), np.float32),
    }
    tiles = [_bias_tiles(table, 0), _bias_tiles(table, 1)]
    in_maps = []
    for core in range(8):
        b, half = core // 2, core % 2
        m = dict(shared)
        m["x_pre"] = f(x[b, 0:HALF])
        m["x_own"] = f(x[b, half * HALF:(half + 1) * HALF])
        m["cT"] = colT(c[b])
        m["bias_tiles"] = tiles[half]
        m["cP"] = shared["cF"] if half == 1 else np.full((128, 8), NEG, np.float32)
        m["flag"] = np.full((128, 1), float(half), np.float32)
        in_maps.append(m)
    if "nc" not in _NC_CACHE:
        _NC_CACHE["nc"] = build_program()
    nc = _NC_CACHE["nc"]
    res = run_bass_kernel_spmd(nc, in_maps, core_ids=list(range(8)))
    out = np.empty((4, S, D), np.float32)
    for core in range(8):
        b, half = core // 2, core % 2
        out[b, half * HALF:(half + 1) * HALF] = res.results[core]["out"]
    kernel.last_results = res.results
    return out
```
